# Optimizing a Trainium2 kernel written in Bass

```python
import math
import jax, jax.numpy as jnp
from jax import lax
import numpy as np

D_MODEL = 1024
BATCH = 16
SEQ = 4096
DEPTH = 4

CHUNK = 64
Q_BLOCK = 128
RMS_EPS = 1e-6
ROPE_THETA = 500000.0
MLA_ROPE_THETA = 10000.0
MACARON_WEIGHT = 0.5
N_MOD = 9
D_FF = 2816

FOX_HEADS = 8
FOX_HD = 64
FOX_GATE_BIAS = 3.0
DSA_HEADS = 8
DSA_HD = 64
DSA_ROT = DSA_HD // 4
IDX_HEADS = 4
IDX_HD = 64
IDX_ROT = IDX_HD // 4
DSA_TOPK_MAX = 256
MLA_HEADS = 8
MLA_NOPE = 128
MLA_ROPE = 64
MLA_V = 128
MLA_Q_LORA = 384
MLA_KV_LORA = 256
MLA_DOWN = MLA_Q_LORA + MLA_KV_LORA + MLA_ROPE

HYB_SIZES = (FOX_HEADS * FOX_HD, FOX_HEADS * FOX_HD, FOX_HEADS * FOX_HD, FOX_HEADS,
             DSA_HEADS * DSA_HD, DSA_HEADS * DSA_HD, DSA_HEADS * DSA_HD,
             IDX_HEADS * IDX_HD, IDX_HEADS, IDX_HD)
HYB_IN = sum(HYB_SIZES)
HYB_OUT = FOX_HEADS * FOX_HD + DSA_HEADS * DSA_HD
N_EVEN = (DEPTH + 1) // 2
N_ODD = DEPTH // 2

kernel_name = 'chunk_causal_hybrid_fox_dsa_mla_trunk'


def _rmsnorm(x, g):
    x32 = x.astype(jnp.float32)
    y = x32 * lax.rsqrt(jnp.mean(x32 * x32, axis=-1, keepdims=True) + RMS_EPS)
    return (y * g.astype(jnp.float32)).astype(x.dtype)


def _rope(x, pos, rot_dim, theta):
    half = rot_dim // 2
    inv_freq = jnp.exp(-math.log(theta) * 2.0 * jnp.arange(half, dtype=jnp.float32) / rot_dim)
    ang = pos.astype(jnp.float32)[:, :, None, None] * inv_freq
    cos, sin = jnp.cos(ang), jnp.sin(ang)
    xr = x[..., :rot_dim].astype(jnp.float32)
    x1, x2 = xr[..., :half], xr[..., half:]
    rot = jnp.concatenate([x1 * cos - x2 * sin, x2 * cos + x1 * sin], axis=-1).astype(x.dtype)
    return jnp.concatenate([rot, x[..., rot_dim:]], axis=-1)


def _sweep_query_blocks(fn, *q_args):
    b, s = q_args[0].shape[:2]
    nblk = s // Q_BLOCK
    blocked = tuple(jnp.moveaxis(a.reshape(b, nblk, Q_BLOCK, *a.shape[2:]), 1, 0) for a in q_args)
    out = lax.map(lambda xs: fn(xs[0] * Q_BLOCK + jnp.arange(Q_BLOCK, dtype=jnp.int32), *xs[1:]),
                  (jnp.arange(nblk, dtype=jnp.int32),) + blocked)
    out = jnp.moveaxis(out, 0, 1)
    return out.reshape(b, s, *out.shape[3:])


def _fox_attention(q, k, v, log_f):
    s = k.shape[1]
    cum = jnp.moveaxis(jnp.cumsum(log_f, axis=1), 1, 2)
    s_idx = jnp.arange(s, dtype=jnp.int32)
    scale = q.shape[-1] ** -0.5

    def block(t_idx, qb, cum_q):
        logits = jnp.einsum('bqhd,bshd->bhqs', qb, k, preferred_element_type=jnp.float32) * scale
        logits = logits + (jnp.moveaxis(cum_q, 1, 2)[..., :, None] - cum[..., None, :])
        mask = s_idx[None, :] <= t_idx[:, None]
        logits = jnp.where(mask, logits, -jnp.inf)
        p = jax.nn.softmax(logits, axis=-1).astype(v.dtype)
        return jnp.einsum('bhqs,bshd->bqhd', p, v)

    return _sweep_query_blocks(block, q, jnp.moveaxis(cum, 1, 2))


def _dsa_attention(q, k, v, iq, iw, ik):
    s = k.shape[1]
    top_k = min(DSA_TOPK_MAX, s // 4)
    s_chunk = jnp.arange(s, dtype=jnp.int32) // CHUNK
    scale = q.shape[-1] ** -0.5
    idx_scale = iq.shape[-1] ** -0.5
    gather = jax.vmap(lambda table, ix: table[ix])

    def block(t_idx, qb, iqb, iwb):
        dots = jnp.einsum('bqhd,bsd->bqhs', iqb, ik, preferred_element_type=jnp.float32) * idx_scale
        score = jnp.einsum('bqhs,bqh->bqs', jax.nn.relu(dots), iwb.astype(jnp.float32))
        admissible = s_chunk[None, :] <= (t_idx // CHUNK)[:, None]
        score = jnp.where(admissible[None], score, -jnp.inf)
        sel_score, sel = lax.top_k(score, top_k)
        kg = gather(k, sel)
        vg = gather(v, sel)
        logits = jnp.einsum('bqhd,bqkhd->bhqk', qb, kg, preferred_element_type=jnp.float32) * scale
        valid = jnp.isfinite(sel_score)[:, None]
        logits = jnp.where(valid, logits, -jnp.inf)
        p = jax.nn.softmax(logits, axis=-1).astype(v.dtype)
        return jnp.einsum('bhqk,bqkhd->bqhd', p, vg)

    return _sweep_query_blocks(block, q, iq, iw)


def _mla_attention(q_nope, q_rope, k_nope, k_rope, v):
    s = k_nope.shape[1]
    s_chunk = jnp.arange(s, dtype=jnp.int32) // CHUNK
    scale = (q_nope.shape[-1] + q_rope.shape[-1]) ** -0.5

    def block(t_idx, qn, qr):
        logits = (jnp.einsum('bqhd,bshd->bhqs', qn, k_nope, preferred_element_type=jnp.float32)
                  + jnp.einsum('bqhr,bsr->bhqs', qr, k_rope, preferred_element_type=jnp.float32)) * scale
        mask = s_chunk[None, :] <= (t_idx // CHUNK)[:, None]
        logits = jnp.where(mask, logits, -jnp.inf)
        p = jax.nn.softmax(logits, axis=-1).astype(v.dtype)
        return jnp.einsum('bhqs,bshd->bqhd', p, v)

    return _sweep_query_blocks(block, q_nope, q_rope)


def _hybrid_mixer(u, positions, w_in, b_f, w_out):
    b, s, _ = u.shape
    offsets = np.cumsum(HYB_SIZES)[:-1].tolist()
    fq, fk, fv, ff, dq, dk, dv, iq, iw, ik = jnp.split(u @ w_in, offsets, axis=-1)
    log_f = jax.nn.log_sigmoid((ff + b_f).astype(jnp.float32))
    out_a = _fox_attention(fq.reshape(b, s, FOX_HEADS, FOX_HD), fk.reshape(b, s, FOX_HEADS, FOX_HD),
                           fv.reshape(b, s, FOX_HEADS, FOX_HD), log_f)
    dq = _rope(dq.reshape(b, s, DSA_HEADS, DSA_HD), positions, DSA_ROT, ROPE_THETA)
    dk = _rope(dk.reshape(b, s, DSA_HEADS, DSA_HD), positions, DSA_ROT, ROPE_THETA)
    iq = _rope(iq.reshape(b, s, IDX_HEADS, IDX_HD), positions, IDX_ROT, ROPE_THETA)
    ik = _rope(ik[:, :, None, :], positions, IDX_ROT, ROPE_THETA)[:, :, 0, :]
    iw = iw * IDX_HEADS ** -0.5
    out_b = _dsa_attention(dq, dk, dv.reshape(b, s, DSA_HEADS, DSA_HD), iq, iw, ik)
    mixed = jnp.concatenate([out_a.reshape(b, s, -1), out_b.reshape(b, s, -1)], axis=-1)
    return mixed @ w_out


def _mla_mixer(u, positions, w_down, q_norm, kv_norm, w_uq, w_ukv, w_out):
    b, s, _ = u.shape
    cq, ckv, k_rope = jnp.split(u @ w_down, [MLA_Q_LORA, MLA_Q_LORA + MLA_KV_LORA], axis=-1)
    q = (_rmsnorm(cq, q_norm) @ w_uq).reshape(b, s, MLA_HEADS, MLA_NOPE + MLA_ROPE)
    kv = (_rmsnorm(ckv, kv_norm) @ w_ukv).reshape(b, s, MLA_HEADS, MLA_NOPE + MLA_V)
    q_nope = q[..., :MLA_NOPE]
    q_rope = _rope(q[..., MLA_NOPE:], positions, MLA_ROPE, MLA_ROPE_THETA)
    k_nope, v = kv[..., :MLA_NOPE], kv[..., MLA_NOPE:]
    k_rope = _rope(k_rope[:, :, None, :], positions, MLA_ROPE, MLA_ROPE_THETA)[:, :, 0, :]
    out = _mla_attention(q_nope, q_rope, k_nope, k_rope, v)
    return out.reshape(b, s, -1) @ w_out


def _swiglu(u, w_gate, w_up, w_down):
    return (jax.nn.silu(u @ w_gate) * (u @ w_up)) @ w_down


def setup_inputs(seed: int = 0) -> dict:
    key = jax.random.key(seed)
    ks = jax.random.split(key, 20)
    f32 = jnp.float32

    def nrm(k, shape, fan_in):
        return jax.random.normal(k, shape, f32) * fan_in ** -0.5

    x = jax.random.normal(ks[0], (BATCH, SEQ, D_MODEL), f32)
    c = jax.random.normal(ks[1], (BATCH, D_MODEL), f32)
    start = jax.random.randint(ks[2], (BATCH, 1), 0, 4096, dtype=jnp.int32)
    positions = start + jnp.arange(SEQ, dtype=jnp.int32)[None, :]
    ada_w = 0.5 * nrm(ks[3], (DEPTH, D_MODEL, N_MOD * D_MODEL), D_MODEL)
    ada_b = 0.02 * jax.random.normal(ks[4], (DEPTH, N_MOD * D_MODEL), f32)
    norm_g = 1.0 + 0.02 * jax.random.normal(ks[5], (DEPTH, 3, D_MODEL), f32)
    ffn_w_gate = nrm(ks[6], (DEPTH, 2, D_MODEL, D_FF), D_MODEL)
    ffn_w_up = nrm(ks[7], (DEPTH, 2, D_MODEL, D_FF), D_MODEL)
    ffn_w_down = nrm(ks[8], (DEPTH, 2, D_FF, D_MODEL), D_FF)
    hyb_w_in = nrm(ks[9], (N_EVEN, D_MODEL, HYB_IN), D_MODEL)
    fox_b_f = FOX_GATE_BIAS + 0.5 * jax.random.normal(ks[10], (N_EVEN, FOX_HEADS), f32)
    hyb_w_out = nrm(ks[11], (N_EVEN, HYB_OUT, D_MODEL), HYB_OUT)
    mla_w_down = nrm(ks[12], (N_ODD, D_MODEL, MLA_DOWN), D_MODEL)
    mla_q_norm = 1.0 + 0.02 * jax.random.normal(ks[13], (N_ODD, MLA_Q_LORA), f32)
    mla_kv_norm = 1.0 + 0.02 * jax.random.normal(ks[14], (N_ODD, MLA_KV_LORA), f32)
    mla_w_uq = nrm(ks[15], (N_ODD, MLA_Q_LORA, MLA_HEADS * (MLA_NOPE + MLA_ROPE)), MLA_Q_LORA)
    mla_w_ukv = nrm(ks[16], (N_ODD, MLA_KV_LORA, MLA_HEADS * (MLA_NOPE + MLA_V)), MLA_KV_LORA)
    mla_w_out = nrm(ks[17], (N_ODD, MLA_HEADS * MLA_V, D_MODEL), MLA_HEADS * MLA_V)
    final_g = 1.0 + 0.02 * jax.random.normal(ks[18], (D_MODEL,), f32)
    return {'x': x, 'c': c, 'positions': positions, 'ada_w': ada_w, 'ada_b': ada_b,
            'norm_g': norm_g, 'ffn_w_gate': ffn_w_gate, 'ffn_w_up': ffn_w_up,
            'ffn_w_down': ffn_w_down, 'hyb_w_in': hyb_w_in, 'fox_b_f': fox_b_f,
            'hyb_w_out': hyb_w_out, 'mla_w_down': mla_w_down, 'mla_q_norm': mla_q_norm,
            'mla_kv_norm': mla_kv_norm, 'mla_w_uq': mla_w_uq, 'mla_w_ukv': mla_w_ukv,
            'mla_w_out': mla_w_out, 'final_g': final_g}


def reference(x, c, positions, ada_w, ada_b, norm_g, ffn_w_gate, ffn_w_up, ffn_w_down,
              hyb_w_in, fox_b_f, hyb_w_out, mla_w_down, mla_q_norm, mla_kv_norm,
              mla_w_uq, mla_w_ukv, mla_w_out, final_g):
    h = x
    cond = jax.nn.silu(c)
    for i in range(DEPTH):
        mod = (cond @ ada_w[i] + ada_b[i]).reshape(c.shape[0], N_MOD, D_MODEL)[:, :, None, :]
        sh1, sc1, g1, sh2, sc2, g2, sh3, sc3, g3 = [mod[:, j] for j in range(N_MOD)]
        u = _rmsnorm(h, norm_g[i, 0]) * (1 + sc1) + sh1
        h = h + MACARON_WEIGHT * g1 * _swiglu(u, ffn_w_gate[i, 0], ffn_w_up[i, 0], ffn_w_down[i, 0])
        u = _rmsnorm(h, norm_g[i, 1]) * (1 + sc2) + sh2
        j = i // 2
        if i % 2 == 0:
            mix = _hybrid_mixer(u, positions, hyb_w_in[j], fox_b_f[j], hyb_w_out[j])
        else:
            mix = _mla_mixer(u, positions, mla_w_down[j], mla_q_norm[j], mla_kv_norm[j],
                             mla_w_uq[j], mla_w_ukv[j], mla_w_out[j])
        h = h + g2 * mix
        u = _rmsnorm(h, norm_g[i, 2]) * (1 + sc3) + sh3
        h = h + MACARON_WEIGHT * g3 * _swiglu(u, ffn_w_gate[i, 1], ffn_w_up[i, 1], ffn_w_down[i, 1])
    return _rmsnorm(h, final_g)
```

```python
import contextlib
import numpy as np
import ml_dtypes
import concourse.bass as bass
import concourse.mybir as mybir
from concourse.bass_utils import run_bass_kernel_spmd

F32 = mybir.dt.float32
BF16 = mybir.dt.bfloat16
I32 = mybir.dt.int32
ALU = mybir.AluOpType
AF = mybir.ActivationFunctionType
AX = mybir.AxisListType

STREAMS = ("pe", "act", "dve", "pool", "sp")
N_DMA_SEMS = 12
EPOCH = 20000

D = 1024
DFF = 2816
NFC = 22
NEGM = -30000.0


class Buf:
    __slots__ = ("name", "w", "r", "excl")

    def __init__(self, name, excl=False):
        self.name = name
        self.w = None
        self.r = []
        self.excl = excl


class Sched:
    def __init__(self, nc):
        self.nc = nc
        self.ops = []
        self.cnt = {(s, k): 0 for s in STREAMS for k in "cd"}
        self.seen = {s: {} for s in STREAMS}
        self.fence_deps = {s: set() for s in STREAMS}
        self.out_dma = []

    def fence(self):
        deps = set()
        for s in STREAMS:
            n = self.cnt[(s, "c")]
            if n > 0:
                deps.add((s, "c", n - 1))
            nd = self.cnt[(s, "d")]
            for j in range(max(0, nd - N_DMA_SEMS), nd):
                deps.add((s, "d", j))
        for s in STREAMS:
            self.fence_deps[s] = set(deps)
            self.seen[s] = {k: v for k, v in self.seen[s].items() if not isinstance(k, tuple)}

    def op(self, stream, fn, reads=(), writes=(), dma=False):
        kind = "d" if dma else "c"
        idx = self.cnt[(stream, kind)]
        self.cnt[(stream, kind)] += 1
        me = (stream, kind, idx)
        deps = set()
        if self.fence_deps[stream]:
            deps |= self.fence_deps[stream]
            self.fence_deps[stream] = set()
        for b in reads:
            if not b.excl and b.w is not None:
                deps.add(b.w)
        wl = list(writes) + [b for b in reads if b.excl]
        for b in wl:
            if b.w is not None:
                deps.add(b.w)
            deps.update(b.r)
        for b in reads:
            if not b.excl:
                if dma:
                    b.r.append(me)
                else:
                    b.r = [x for x in b.r if not (x[0] == stream and x[1] == "c")] + [me]
        for b in wl:
            b.w = me
            b.r = []
        fd = []
        seen = self.seen[stream]
        best = {}
        for d in deps:
            ps, pk, pi = d
            if d == me:
                continue
            if pk == "d":
                if d in seen:
                    continue
                seen[d] = True
                fd.append(d)
            else:
                if ps == stream and stream == "pe" and not dma:
                    continue
                if ps == stream and pi >= idx and not dma:
                    continue
                if seen.get(ps, -1) >= pi:
                    continue
                if best.get(ps, -1) < pi:
                    best[ps] = pi
        for ps, pi in best.items():
            seen[ps] = pi
            fd.append((ps, "c", pi))
        self.ops.append((stream, kind, idx, fn, fd))
        return me

    def emit(self):
        nc = self.nc
        needs = {s: [False] * self.cnt[(s, "c")] for s in STREAMS}
        for stream, kind, idx, fn, fd in self.ops:
            for (ps, pk, pi) in fd:
                if pk == "c":
                    needs[ps][pi] = True
        val = {}
        for s in STREAMS:
            c = 0
            v = []
            for n in needs[s]:
                if n:
                    c += 1
                v.append(c)
            val[s] = v
        per = {s: [] for s in STREAMS}
        for o in self.ops:
            per[o[0]].append(o)
        self.nwaits = 0
        with contextlib.ExitStack() as st:
            sems = {}
            for s in STREAMS:
                tot = val[s][-1] if val[s] else 0
                sems[s] = [st.enter_context(nc.semaphore("s_%s_%d" % (s, i))) for i in range(tot // EPOCH + 1)]
            dsems = {s: [st.enter_context(nc.semaphore("d_%s_%d" % (s, i))) for i in range(N_DMA_SEMS)]
                     for s in STREAMS if self.cnt[(s, "d")] > 0}
            block = st.enter_context(nc.Block())

            def semv(ps, c):
                ep = (c - 1) // EPOCH
                return sems[ps][ep], c - ep * EPOCH

            def run(s, e):
                for (stream, kind, idx, fn, fd) in per[s]:
                    for (ps, pk, pi) in fd:
                        self.nwaits += 1
                        if pk == "d":
                            e.wait_ge(dsems[ps][pi % N_DMA_SEMS], 16 * (pi // N_DMA_SEMS + 1))
                        else:
                            sm, v = semv(ps, val[ps][pi])
                            e.wait_ge(sm, v)
                    if kind == "d":
                        if idx >= N_DMA_SEMS:
                            e.wait_ge(dsems[s][idx % N_DMA_SEMS], 16 * (idx // N_DMA_SEMS))
                        fn(e).then_inc(dsems[s][idx % N_DMA_SEMS], 16)
                    else:
                        ins = fn(e)
                        if needs[s][idx]:
                            sm, v = semv(s, val[s][idx])
                            ins.then_inc(sm, 1)
                n = self.cnt[(s, "d")]
                for j in range(max(0, n - N_DMA_SEMS), n):
                    e.wait_ge(dsems[s][j % N_DMA_SEMS], 16 * (j // N_DMA_SEMS + 1))

            @block.tensor
            def _(e):
                run("pe", e)

            @block.scalar
            def _(e):
                run("act", e)

            @block.vector
            def _(e):
                run("dve", e)

            @block.gpsimd
            def _(e):
                run("pool", e)

            @block.sync
            def _(e):
                run("sp", e)


class Tl:
    __slots__ = ("ap", "b")

    def __init__(self, ap, b):
        self.ap = ap
        self.b = b


class KB:
    def __init__(self, nc, S, cfg):
        self.nc = nc
        self.S = S
        self.cfg = cfg
        self.st = contextlib.ExitStack()
        self.uid = 0
        self.rr = 0

    def mm(self, out, lhsT, rhs, start, stop, reads, writes):
        self.S.op("pe", lambda e: e.matmul(out, lhsT, rhs, start=start, stop=stop, skip_group_check=True), reads, writes)

    def act(self, out, in_, func, reads, writes, bias=0.0, scale=1.0, accum_out=None):
        if accum_out is None:
            self.S.op("act", lambda e: e.activation(out, in_, func, bias=bias, scale=scale), reads, writes)
        else:
            self.S.op("act", lambda e: e.activation(out, in_, func, bias=bias, scale=scale, accum_out=accum_out), reads, writes)

    def tt(self, eng, out, in0, in1, op, reads, writes):
        self.S.op(eng, lambda e: e.tensor_tensor(out, in0, in1, op), reads, writes)

    def ts(self, eng, out, in0, s1, s2, op0, op1, reads, writes, accum_out=None):
        if accum_out is None:
            if op1 is None:
                self.S.op(eng, lambda e: e.tensor_single_scalar(out, in0, s1, op0), reads, writes)
            else:
                self.S.op(eng, lambda e: e.tensor_scalar(out, in0, s1, s2, op0, op1), reads, writes)
        else:
            self.S.op(eng, lambda e: e.tensor_scalar(out, in0, s1, s2, op0, op1, accum_out=accum_out), reads, writes)

    def stt(self, eng, out, in0, scalar, in1, op0, op1, reads, writes):
        self.S.op(eng, lambda e: e.scalar_tensor_tensor(out, in0, scalar, in1, op0, op1), reads, writes)

    def cp(self, eng, out, in_, reads, writes):
        if eng == "act":
            self.S.op("act", lambda e: e.copy(out, in_), reads, writes)
        else:
            self.S.op(eng, lambda e: e.tensor_copy(out, in_), reads, writes)

    def memset(self, eng, ap, v, writes):
        self.S.op(eng, lambda e: e.memset(ap, v), (), writes)

    def dma(self, out, in_, reads, writes, slow=False):
        if slow:
            self.S.op("sp", lambda e: e.dma_start(out=out, in_=in_, allow_slow_non_contiguous=True), reads, writes, dma=True)
        else:
            self.S.op("sp", lambda e: e.dma_start(out=out, in_=in_), reads, writes, dma=True)

    def recip(self, out, in_, reads, writes):
        self.S.op("dve", lambda e: e.reciprocal(out, in_), reads, writes)

    def sb(self, name, shape, dt):
        t = self.st.enter_context(self.nc.sbuf_tensor(name, list(shape), dt))
        return t

    def psum(self, name, shape, dt):
        return self.st.enter_context(self.nc.psum_tensor(name, list(shape), dt))

    def dram(self, name, shape, dt, kind="Internal"):
        return self.nc.dram_tensor(name, list(shape), dt, kind=kind)


class Arena:
    def __init__(self, t_f32, nwords):
        self.t = t_f32
        self.n = nwords
        self.off = 0

    def reset(self):
        self.off = 0

    def f32(self, n, name="a"):
        o = self.off
        self.off += n
        assert self.off <= self.n, ("arena overflow", name, self.off, self.n)
        return Tl(self.t[:, o:o + n], Buf(name))

    def bf(self, n, name="a"):
        w = (n + 1) // 2
        o = self.off
        self.off += w
        assert self.off <= self.n, ("arena overflow", name, self.off, self.n)
        return Tl(self.t[:, o:o + w].bitcast(BF16), Buf(name))


def build_program(cfg):
    SQ = cfg["S"]
    NSEQ = cfg["NSEQ"]
    DEPTH = cfg["DEPTH"]
    NG = SQ // 512
    NKT = SQ // 128
    nc = bass.Bass("TRN2", target_bir_lowering=False)
    S = Sched(nc)
    kb = KB(nc, S, cfg)
    n_even = (DEPTH + 1) // 2
    n_odd = DEPTH // 2

    def din(name, shape, dt=F32):
        return nc.dram_tensor(name, list(shape), dt, kind="ExternalInput")

    xT = din("xT", [NSEQ, D, SQ])
    cT = din("cT", [128, 8 * NSEQ])
    pos = din("pos", [NSEQ, SQ], I32)
    consts = din("consts", [128, 640])
    ada_w = din("ada_w", [DEPTH, 128, 8 * 9216])
    ada_b = din("ada_b", [DEPTH, 1, 9216])
    norm_gT = din("norm_gT", [DEPTH * 3, 128, 8])
    final_gT = din("final_gT", [128, 8])
    w_gate = din("w_gate", [DEPTH * 2, 128, 8 * DFF])
    w_up = din("w_up", [DEPTH * 2, 128, 8 * DFF])
    w_down = din("w_down", [DEPTH * 2, 128, NFC * D])
    NHF = 3456
    NHT = 1032
    hyb_fm = din("hyb_fm", [n_even, 128, 8 * NHF])
    hyb_tm = din("hyb_tm", [n_even, 128, 8 * NHT])
    hyb_out = din("hyb_out", [n_even, 128, 8 * D])
    fox_b = din("fox_b", [n_even, 128, 8])
    if n_odd:
        mla_down = din("mla_down", [n_odd, 128, 8 * 768])
        mla_qn = din("mla_qn", [n_odd, 128, 3])
        mla_kvn = din("mla_kvn", [n_odd, 128, 2])
        mla_uq = din("mla_uq", [n_odd, 128, 3 * 1536])
        mla_ukv = din("mla_ukv", [n_odd, 128, 2 * 2048])
        mla_out = din("mla_out", [n_odd, 128, 8 * D])
    outT = nc.dram_tensor("outT", [NSEQ, D, SQ], F32, kind="ExternalOutput")

    hT = [nc.dram_tensor("hT%d" % s, [D, SQ], F32, kind=("ExternalOutput" if cfg.get("dbg_h") else "Internal")) for s in range(NSEQ)]
    hT_b = [[Buf("hT%d_%d" % (s, g)) for g in range(NG)] for s in range(NSEQ)]
    xT_b = [Buf("xT%d" % g) for g in range(NG)]
    scr = {}

    def scratch(name, shape, dt=BF16):
        kind = "ExternalOutput" if name in cfg.get("dbg", []) else "Internal"
        scr[name] = (nc.dram_tensor("scr_" + name, list(shape), dt, kind=kind), Buf("scr_" + name))
        return scr[name]

    for h in range(8):
        scratch("QA%d" % h, [128, SQ])
        scratch("KA%d" % h, [128, SQ])
    scratch("VF", [SQ, 512])
    for c in range(4):
        scratch("DQ%d" % c, [128, SQ])
        scratch("DK%d" % c, [128, SQ])
    scratch("DV", [SQ, 512])
    for c in range(2):
        scratch("IQ%d" % c, [128, SQ])
    scratch("IK", [128, SQ])
    scratch("MIX", [D, SQ])
    if n_odd:
        for h in range(8):
            scratch("QN%d" % h, [128, SQ])
            scratch("QR%d" % h, [64, SQ])
            scratch("KN%d" % h, [128, SQ])
        scratch("KR", [64, SQ])
        scratch("MV", [SQ, 1024])
    with kb.st:
        ARENA_WORDS = 49400
        arena_t = kb.sb("arena", [128, ARENA_WORDS], F32)
        A = Arena(arena_t, ARENA_WORDS)
        cst = kb.sb("cst", [128, 640], F32)
        cst_b = Buf("cst")
        ident_bf = kb.sb("ident_bf", [128, 128], BF16)
        ones_bf = kb.sb("ones_bf", [128, 128], BF16)
        zeros_f = kb.sb("zeros_f", [128, 512], F32)
        misc_b = Buf("misc")
        condT = kb.sb("condT", [128, 8 * NSEQ], F32)
        cond_b = Buf("cond")
        modT = kb.sb("modT", [128, NSEQ * 72], F32)
        mod_b = Buf("mod")
        ngT = kb.sb("ngT", [128, DEPTH * 3 * 8 + 8], F32)
        ng_b = Buf("ng")
        ABG = kb.sb("ABG", [128, NSEQ * 3 * 24], F32)
        abg_b = Buf("abg")
        stat = kb.sb("stat", [128, 64], F32)
        stat_b = Buf("stat")
        negB = kb.sb("negB", [128, 16], F32)
        negB_b = Buf("negB")
        iw_all = kb.sb("iw_all", [128, NKT * 4], F32)
        iw_b = Buf("iw")
        iwabs = kb.sb("iwabs", [128, NKT * 4], F32)
        iwsgn = kb.sb("iwsgn", [128, NKT * 4], F32)
        smallf = kb.sb("smallf", [128, 64], F32)
        fb_t = kb.sb("fb_t", [128, 8], F32)
        fb_b = Buf("fb")
        qn_t = kb.sb("qn_t", [128, 8], F32)
        qn_b = Buf("qn")
        PS = [kb.psum("ps%d" % i, [128, 512], F32) for i in range(8)]
        PB = [Buf("ps%d" % i, excl=True) for i in range(8)]

        kb.dma(cst[:], consts[:, :], [], [cst_b])
        IOTA = cst[:, 0:512]
        kb.memset("pool", zeros_f[:], 0.0, [misc_b])
        kb.memset("pool", ones_bf[:], 1.0, [misc_b])
        kb.ts("dve", ident_bf[:], cst[:, 0:128], cst[:, 512:513], None, ALU.is_equal, None, [cst_b], [misc_b])

        def build_mask(kind):
            m = A.bf(4 * 512, "mask%d" % kind)
            for a in range(4):
                if kind == 0:
                    kb.ts("dve", m.ap[:, a * 512:(a + 1) * 512], IOTA, cst[:, 513 + a:514 + a], NEGM, ALU.is_lt, ALU.mult, [cst_b], [m.b])
                elif kind == 1:
                    kb.ts("dve", m.ap[:, a * 512:(a + 1) * 512], IOTA, cst[:, 517 + a:518 + a], NEGM, ALU.is_lt, ALU.mult, [cst_b], [m.b])
                else:
                    kb.ts("dve", m.ap[:, a * 512:(a + 1) * 512], IOTA, cst[:, 521 + a:522 + a], -1e30, ALU.is_ge, ALU.mult, [cst_b], [m.b])
            return m
        kb.dma(condT[:], cT[:, :], [], [cond_b])
        kb.act(condT[:], condT[:], AF.Silu, [cond_b], [cond_b])
        for i in range(DEPTH * 3):
            kb.dma(ngT[:, i * 8:(i + 1) * 8], norm_gT[i, :, :], [], [ng_b])
        kb.dma(ngT[:, DEPTH * 24:DEPTH * 24 + 8], final_gT[:, :], [], [ng_b])

        def load_w_bf(dst_bf_ap, src_dram_ap, ncols, name):
            CH = 2048
            nst = 2
            stg = [A.f32(CH, "stg%d" % i) for i in range(nst)]
            dstb = Buf(name)
            i = 0
            engs = ("pool", "act", "dve")
            for o in range(0, ncols, CH):
                n = min(CH, ncols - o)
                s_ = stg[i % nst]
                kb.dma(s_.ap[:, 0:n], src_dram_ap[:, o:o + n], [], [s_.b])
                kb.cp("pool", dst_bf_ap[:, o:o + n], s_.ap[:, 0:n], [s_.b], [dstb])
                i += 1
            S.fence()
            return dstb

        def compute_mod(layer):
            A.reset()
            PIECE = 512
            wst = [A.f32(8 * PIECE, "adaw%d" % i) for i in range(2)]
            bst = [A.f32(PIECE, "adab%d" % i) for i in range(2)]
            onesf = A.f32(8, "onesf")
            kb.memset("pool", onesf.ap[0:1, 0:NSEQ], 1.0, [onesf.b])
            npiece = 9216 // PIECE
            for pi in range(npiece):
                w_ = wst[pi % 2]
                b_ = bst[pi % 2]
                src = ada_w[layer].rearrange("p (k n) -> p k n", k=8)[:, :, pi * PIECE:(pi + 1) * PIECE]
                kb.dma(w_.ap.rearrange("p (k n) -> p k n", k=8), src, [], [w_.b])
                kb.dma(b_.ap[0:1, :], ada_b[layer, :, pi * PIECE:(pi + 1) * PIECE], [], [b_.b])
                for cc in range(PIECE // 128):
                    col = pi * (PIECE // 128) + cc
                    for k in range(8):
                        kb.mm(PS[0][:, col * NSEQ:(col + 1) * NSEQ], w_.ap[:, k * PIECE + cc * 128:k * PIECE + (cc + 1) * 128],
                              condT[:, k * NSEQ:(k + 1) * NSEQ], k == 0, False, [w_.b, cond_b], [PB[0]])
                    kb.mm(PS[0][:, col * NSEQ:(col + 1) * NSEQ], b_.ap[0:1, cc * 128:(cc + 1) * 128],
                          onesf.ap[0:1, 0:NSEQ], False, True, [b_.b, onesf.b], [PB[0]])
            for s in range(NSEQ):
                src = PS[0][:, 0:72 * NSEQ].rearrange("p (c s) -> p c s", s=NSEQ)[:, :, s]
                kb.cp("dve", modT[:, s * 72:(s + 1) * 72], src, [PB[0]], [mod_b])

        def compute_abg(layer):
            for s in range(NSEQ):
                for sub in range(3):
                    o = (s * 3 + sub) * 24
                    m = s * 72 + sub * 24
                    g = ngT[:, (layer * 3 + sub) * 8:(layer * 3 + sub) * 8 + 8]
                    kb.ts("dve", ABG[:, o:o + 8], modT[:, m + 8:m + 16], 1.0, None, ALU.add, None, [mod_b], [abg_b])
                    kb.tt("dve", ABG[:, o:o + 8], ABG[:, o:o + 8], g, ALU.mult, [abg_b, ng_b], [abg_b])
                    kb.ts("dve", ABG[:, o:o + 8], ABG[:, o:o + 8], 32.0, None, ALU.mult, None, [abg_b], [abg_b])
                    kb.cp("dve", ABG[:, o + 8:o + 16], modT[:, m:m + 8], [mod_b], [abg_b])
                    gs = 1.0 if sub == 1 else 0.5
                    kb.ts("dve", ABG[:, o + 16:o + 24], modT[:, m + 16:m + 24], gs, None, ALU.mult, None, [mod_b], [abg_b])

        def h_src(layer, sub, s):
            if layer == 0 and sub == 0:
                return xT[s], xT_b
            return hT[s][:, :], hT_b[s]

        def norm_group(hsrc, hsrc_b, s, sub, g, hg, uT, extra_scale=None, final=False, layer=0):
            sq = A_sq
            rs = A_rs
            tmp = A_tmp
            src = hsrc.rearrange("(c p) t -> p c t", p=128)[:, :, g * 512:(g + 1) * 512]
            kb.dma(hg.ap.rearrange("p (c t) -> p c t", c=8), src, [hsrc_b[g]], [hg.b])
            for c in range(8):
                kb.act(sq.ap[:, c * 512:(c + 1) * 512], hg.ap[:, c * 512:(c + 1) * 512], AF.Square, [hg.b], [sq.b])
            for c in range(8):
                kb.mm(PS[7][:, :], ones_bf[:, :], sq.ap[:, c * 512:(c + 1) * 512], c == 0, c == 7, [sq.b, misc_b], [PB[7]])
            kb.act(rs.ap, PS[7][:, :], AF.Sqrt, [PB[7]], [rs.b], bias=epsb[:, 0:1], scale=1.0)
            kb.recip(rs.ap, rs.ap, [rs.b], [rs.b])
            o = (s * 3 + sub) * 24
            for c in range(8):
                t_ = tmp[c % 2]
                if final:
                    kb.stt("dve", t_.ap, hg.ap[:, c * 512:(c + 1) * 512], fg32[:, c:c + 1], rs.ap, ALU.mult, ALU.mult,
                           [hg.b, rs.b, abg_b], [t_.b])
                    kb.cp("act", uT.ap[:, c * 512:(c + 1) * 512], t_.ap, [t_.b], [uT.b])
                else:
                    kb.stt("dve", t_.ap, hg.ap[:, c * 512:(c + 1) * 512], ABG[:, o + c:o + c + 1], rs.ap, ALU.mult, ALU.mult,
                           [hg.b, rs.b, abg_b], [t_.b])
                    kb.act(uT.ap[:, c * 512:(c + 1) * 512], t_.ap, AF.Identity, [t_.b, abg_b], [uT.b],
                           bias=ABG[:, o + 8 + c:o + 9 + c], scale=1.0)

        epsb = smallf[:, 0:1]
        kb.memset("pool", smallf[:, 0:1], float(D) * 1e-6, [misc_b])
        fg32 = smallf[:, 8:16]
        kb.ts("dve", fg32, ngT[:, DEPTH * 24:DEPTH * 24 + 8], 32.0, None, ALU.mult, None, [ng_b], [abg_b])

        def ffn_phase(layer, which):
            global A_sq, A_rs, A_tmp
            S.fence()
            A.reset()
            sub = 0 if which == 0 else 2
            wi = layer * 2 + which
            wg = A.bf(8 * DFF, "wg")
            wu = A.bf(8 * DFF, "wu")
            wd = A.bf(NFC * D, "wd")
            mark = A.off
            wg.b = load_w_bf(wg.ap, w_gate[wi], 8 * DFF, "wg")
            A.off = mark
            wu.b = load_w_bf(wu.ap, w_up[wi], 8 * DFF, "wu")
            A.off = mark
            wd.b = load_w_bf(wd.ap, w_down[wi], NFC * D, "wd")
            A.off = mark
            S.fence()
            hgs = [A.f32(8 * 512, "hg%d" % i) for i in range(1)]
            uTs = [A.bf(8 * 512, "uT%d" % i) for i in range(1)]
            actT = A.bf(NFC * 512, "actT")
            A_sq = Tl(uTs[0].ap, uTs[0].b)
            A_rs = A.f32(512, "rs")
            A_tmp = [A.f32(512, "tmp%d" % i) for i in range(2)]
            sg = [A.bf(512, "sg%d" % i) for i in range(2)]
            it = 0
            for s in range(NSEQ):
                hsrc, hsrc_b = h_src(layer, sub, s)
                o = (s * 3 + sub) * 24
                for g in range(NG):
                    hg = hgs[0]
                    uT = uTs[0]
                    norm_group(hsrc, hsrc_b, s, sub, g, hg, uT)
                    if cfg.get("dbg_ffn") and s == 0 and g == 0 and layer == 0 and which == 0:
                        du = nc.dram_tensor("dbg_uT", [128, 4096], BF16, kind="ExternalOutput")
                        kb.S.op("sp", (lambda du=du, uT=uT: lambda e: e.dma_start(out=du[:, :], in_=uT.ap))(), [uT.b], [], dma=True)
                    for f in range(NFC):
                        pg = 0 + (f % 2) * 2
                        pu = 1 + (f % 2) * 2
                        for k in range(8):
                            kb.mm(PS[pg][:, :], wg.ap[:, k * DFF + f * 128:k * DFF + (f + 1) * 128], uT.ap[:, k * 512:(k + 1) * 512],
                                  k == 0, k == 7, [wg.b, uT.b], [PB[pg]])
                        for k in range(8):
                            kb.mm(PS[pu][:, :], wu.ap[:, k * DFF + f * 128:k * DFF + (f + 1) * 128], uT.ap[:, k * 512:(k + 1) * 512],
                                  k == 0, k == 7, [wu.b, uT.b], [PB[pu]])
                        sg_ = sg[f % 2]
                        kb.act(sg_.ap, PS[pg][:, :], AF.Silu, [PB[pg]], [sg_.b])
                        kb.tt("dve", actT.ap[:, f * 512:(f + 1) * 512], sg_.ap, PS[pu][:, :], ALU.mult, [sg_.b, PB[pu]], [actT.b])
                    if cfg.get("dbg_ffn") and s == 0 and g == 0 and layer == 0 and which == 0:
                        da = nc.dram_tensor("dbg_actT", [128, NFC * 512], BF16, kind="ExternalOutput")
                        kb.S.op("sp", (lambda da=da, actT=actT: lambda e: e.dma_start(out=da[:, :], in_=actT.ap))(), [actT.b], [], dma=True)
                    for c in range(8):
                        pd = 4 + (c % 2)
                        for f in range(NFC):
                            kb.mm(PS[pd][:, :], wd.ap[:, f * D + c * 128:f * D + (c + 1) * 128], actT.ap[:, f * 512:(f + 1) * 512],
                                  f == 0, f == NFC - 1, [wd.b, actT.b], [PB[pd]])
                        kb.stt("dve", hg.ap[:, c * 512:(c + 1) * 512], PS[pd][:, :], ABG[:, o + 16 + c:o + 17 + c], hg.ap[:, c * 512:(c + 1) * 512],
                               ALU.mult, ALU.add, [PB[pd], abg_b, hg.b], [hg.b])
                        kb.dma(hT[s][c * 128:(c + 1) * 128, g * 512:(g + 1) * 512], hg.ap[:, c * 512:(c + 1) * 512], [hg.b], [hT_b[s][g]])

        def outproj_phase(layer, w_dram, s):
            S.fence()
            A.reset()
            wo = A.bf(8 * D, "wo")
            mark = A.off
            wo.b = load_w_bf(wo.ap, w_dram, 8 * D, "wo")
            A.off = mark
            S.fence()
            mx = [A.bf(8 * 512, "mx%d" % i) for i in range(2)]
            hg = [A.f32(8 * 512, "hgo%d" % i) for i in range(2)]
            ho = [A.f32(512, "hoo%d" % i) for i in range(2)]
            o = (s * 3 + 1) * 24
            mixd, mixb = scr["MIX"]
            for g in range(NG):
                m_ = mx[g % 2]
                h_ = hg[g % 2]
                kb.dma(m_.ap.rearrange("p (c t) -> p c t", c=8),
                       mixd.rearrange("(c p) t -> p c t", p=128)[:, :, g * 512:(g + 1) * 512], [mixb], [m_.b])
                kb.dma(h_.ap.rearrange("p (c t) -> p c t", c=8),
                       hT[s].rearrange("(c p) t -> p c t", p=128)[:, :, g * 512:(g + 1) * 512], [hT_b[s][g]], [h_.b])
                for c in range(8):
                    pd = c % 2
                    for k in range(8):
                        kb.mm(PS[pd][:, :], wo.ap[:, k * D + c * 128:k * D + (c + 1) * 128], m_.ap[:, k * 512:(k + 1) * 512],
                              k == 0, k == 7, [wo.b, m_.b], [PB[pd]])
                    ho_ = ho[c % 2]
                    kb.stt("dve", ho_.ap, PS[pd][:, :], ABG[:, o + 16 + c:o + 17 + c], h_.ap[:, c * 512:(c + 1) * 512],
                           ALU.mult, ALU.add, [PB[pd], abg_b, h_.b], [ho_.b])
                    kb.dma(hT[s][c * 128:(c + 1) * 128, g * 512:(g + 1) * 512], ho_.ap, [ho_.b], [hT_b[s][g]])

        def rope_tables(s, col_scale, cosT, sinT):
            posi = A.f32(SQ, "posi")
            pi_ap = posi.ap.bitcast(I32)
            kb.dma(pi_ap, pos[s:s + 1, :].partition_broadcast(128), [], [posi.b])
            invf = smallf[:, 20:21]
            kb.act(invf, cst[:, col_scale:col_scale + 1], AF.Exp, [cst_b], [misc_b])
            ang = cosT
            kb.cp("dve", ang.ap, pi_ap, [posi.b], [ang.b])
            kb.ts("dve", ang.ap, ang.ap, invf, None, ALU.mult, None, [ang.b, misc_b], [ang.b])
            C1 = 6.28125
            C2 = float(2.0 * np.pi - 6.28125)
            ki = A.f32(SQ, "ki")
            kf = A.f32(SQ, "kf")

            def reduce_to(dst, shift):
                kb.ts("dve", kf.ap, ang.ap, shift, float(1.0 / (2.0 * np.pi)), ALU.add, ALU.mult, [ang.b], [kf.b])
                kb.cp("dve", ki.ap.bitcast(I32), kf.ap, [kf.b], [ki.b])
                kb.cp("dve", kf.ap, ki.ap.bitcast(I32), [ki.b], [kf.b])
                kb.stt("dve", dst.ap, kf.ap, -C1, ang.ap, ALU.mult, ALU.add, [kf.b, ang.b], [dst.b])
                kb.stt("dve", dst.ap, kf.ap, -C2, dst.ap, ALU.mult, ALU.add, [kf.b, dst.b], [dst.b])
                if shift != 0.0:
                    kb.ts("dve", dst.ap, dst.ap, shift, None, ALU.add, None, [dst.b], [dst.b])
                kb.ts("dve", kf.ap, dst.ap, float(np.pi), float(-2.0 * np.pi), ALU.is_gt, ALU.mult, [dst.b], [kf.b])
                kb.tt("dve", dst.ap, dst.ap, kf.ap, ALU.add, [dst.b, kf.b], [dst.b])
                kb.ts("dve", kf.ap, dst.ap, float(-np.pi), float(2.0 * np.pi), ALU.is_lt, ALU.mult, [dst.b], [kf.b])
                kb.tt("dve", dst.ap, dst.ap, kf.ap, ALU.add, [dst.b, kf.b], [dst.b])
                kb.ts("dve", dst.ap, dst.ap, float(np.pi), -float(np.pi), ALU.min, ALU.max, [dst.b], [dst.b])

            reduce_to(sinT, 0.0)
            tmpc = A.f32(SQ, "tmpc")
            reduce_to(tmpc, float(0.5 * np.pi))
            kb.cp("dve", cosT.ap, tmpc.ap, [tmpc.b], [cosT.b])
            kb.act(sinT.ap, sinT.ap, AF.Sin, [sinT.b], [sinT.b])
            kb.act(cosT.ap, cosT.ap, AF.Sin, [cosT.b], [cosT.b])

        def rope_block(y_ps, pb, b0, half, g, cosT, sinT, dst, t1, t2, eng="dve"):
            tk = slice(g * 512, (g + 1) * 512)
            r1 = slice(b0, b0 + half)
            r2 = slice(b0 + 32, b0 + 32 + half)
            kb.tt(eng, t1.ap[r1, :], y_ps[r2, :], sinT.ap[r2, tk], ALU.mult, [pb, sinT.b], [t1.b])
            kb.tt(eng, t2.ap[r2, :], y_ps[r1, :], sinT.ap[r1, tk], ALU.mult, [pb, sinT.b], [t2.b])
            kb.tt(eng, t2.ap[r1, :], y_ps[r1, :], cosT.ap[r1, tk], ALU.mult, [pb, cosT.b], [t2.b])
            kb.tt(eng, t1.ap[r2, :], y_ps[r2, :], cosT.ap[r2, tk], ALU.mult, [pb, cosT.b], [t1.b])
            kb.tt(eng, dst[r1, :], t2.ap[r1, :], t1.ap[r1, :], ALU.subtract, [t1.b, t2.b], [dstb_cur[0]])
            kb.tt(eng, dst[r2, :], t1.ap[r2, :], t2.ap[r2, :], ALU.add, [t1.b, t2.b], [dstb_cur[0]])

        dstb_cur = [None]

        def bound_update(sq_ap, sq_b, rows, col):
            kb.mm(PS[6][:, :], ones_bf[rows, :], sq_ap[rows, :], True, True, [sq_b, misc_b], [PB[6]])
            kb.S.op("dve", lambda e: e.tensor_reduce(smallf[:, 32:33], PS[6][:, :], AX.X, ALU.max), [PB[6]], [misc_b])
            kb.tt("dve", stat[:, col:col + 1], stat[:, col:col + 1], smallf[:, 32:33], ALU.max, [misc_b, stat_b], [stat_b])

        def finish_bounds(nh, scale):
            kb.tt("dve", negB[:, 0:nh], stat[:, 0:nh], stat[:, 16:16 + nh], ALU.mult, [stat_b], [negB_b])
            kb.act(negB[:, 0:nh], negB[:, 0:nh], AF.Sqrt, [negB_b], [negB_b], scale=scale * scale)
            kb.ts("dve", negB[:, 0:nh], negB[:, 0:nh], -1.0, None, ALU.mult, None, [negB_b], [negB_b])

        def hyb_proj_phase(layer, s):
            global A_sq, A_rs, A_tmp
            j = layer // 2
            S.fence()
            A.reset()
            wf = A.bf(8 * NHF, "wf")
            wt = A.bf(8 * NHT, "wt")
            mark = A.off
            wf.b = load_w_bf(wf.ap, hyb_fm[j], 8 * NHF, "wf")
            A.off = mark
            wt.b = load_w_bf(wt.ap, hyb_tm[j], 8 * NHT, "wt")
            A.off = mark
            kb.dma(fb_t[:], fox_b[j, :, :], [], [fb_b])
            kb.ts("dve", fb_t[:], fb_t[:], -1.0, None, ALU.mult, None, [fb_b], [fb_b])
            S.fence()
            cos16 = A.f32(SQ, "cos16")
            sin16 = A.f32(SQ, "sin16")
            mk2 = A.off
            rope_tables(s, 525, cos16, sin16)
            A.off = mk2
            S.fence()
            hg = A.f32(8 * 512, "hg")
            uT = A.bf(8 * 512, "uT")
            A_sq = A.bf(8 * 512, "sq")
            A_rs = A.f32(512, "rs")
            A_tmp = [A.f32(512, "tmp%d" % i) for i in range(2)]
            qst = [A.bf(512, "qst%d" % i) for i in range(2)]
            kst = [A.bf(512, "kst%d" % i) for i in range(2)]
            gst = [A.bf(512, "gst%d" % i) for i in range(3)]
            sqs = [A.bf(512, "sqs%d" % i) for i in range(2)]
            t1 = A.f32(512, "t1")
            t2 = A.f32(512, "t2")
            ge = A.f32(512, "ge")
            gc = A.f32(512, "gc")
            gh = A.f32(512, "gh")
            ghb = A.bf(512, "ghb")
            vst = [A.bf(512, "vst%d" % i) for i in range(2)]
            carry = smallf[:, 34:42]
            kb.memset("pool", carry, 0.0, [misc_b])
            kb.memset("pool", stat[:, 0:32], 0.0, [stat_b])
            for q_ in qst:
                kb.memset("pool", q_.ap[:, :], 0.0, [q_.b])
                kb.memset("pool", q_.ap[96:98, :], 1.0, [q_.b])
            for k_ in kst:
                kb.memset("pool", k_.ap[:, :], 0.0, [k_.b])
                kb.memset("pool", k_.ap[64:66, :], 1.0, [k_.b])
            hsrc, hsrc_b = hT[s][:, :], hT_b[s]
            it = 0
            for g in range(NG):
                tk = slice(g * 512, (g + 1) * 512)
                norm_group(hsrc, hsrc_b, s, 1, g, hg, uT)

                def proj_fm(chunk, pbank):
                    for k in range(8):
                        kb.mm(PS[pbank][:, :], wf.ap[:, k * NHF + chunk * 128:k * NHF + (chunk + 1) * 128],
                              uT.ap[:, k * 512:(k + 1) * 512], k == 0, k == 7, [wf.b, uT.b], [PB[pbank]])

                for h in range(8):
                    pq = (h % 2) * 2
                    pk = (h % 2) * 2 + 1
                    proj_fm(h, pq)
                    proj_fm(8 + h, pk)
                    q_ = qst[h % 2]
                    k_ = kst[h % 2]
                    sq_ = sqs[h % 2]
                    kb.cp("act", q_.ap[0:64, :], PS[pq][0:64, :], [PB[pq]], [q_.b])
                    kb.act(sq_.ap[0:64, :], PS[pq][0:64, :], AF.Square, [PB[pq]], [sq_.b])
                    bound_update(sq_.ap, sq_.b, slice(0, 64), h)
                    kb.cp("act", k_.ap[0:64, :], PS[pk][0:64, :], [PB[pk]], [k_.b])
                    kb.act(sq_.ap[64:128, :], PS[pk][0:64, :], AF.Square, [PB[pk]], [sq_.b])
                    bound_update(sq_.ap, sq_.b, slice(64, 128), 16 + h)
                    R = slice(64, 66)
                    kb.act(ge.ap[R, :], PS[pq][R, :], AF.Exp, [PB[pq], fb_b], [ge.b], bias=fb_t[R, h:h + 1], scale=-1.0)
                    kb.act(ge.ap[R, :], ge.ap[R, :], AF.Ln, [ge.b], [ge.b], bias=1.0, scale=1.0)
                    kb.ts("dve", ge.ap[R, :], ge.ap[R, :], -8.0, None, ALU.mult, None, [ge.b], [ge.b])
                    kb.S.op("dve", (lambda R=R, h=h: lambda e: e.tensor_tensor_scan(gc.ap[R, :], ge.ap[R, :], zeros_f[R, :], carry[R, h:h + 1], ALU.add, ALU.add))(),
                            [ge.b, misc_b], [gc.b])
                    kb.cp("dve", carry[R, h:h + 1], gc.ap[R, 511:512], [gc.b], [misc_b])
                    kb.cp("dve", ghb.ap[R, :], gc.ap[R, :], [gc.b], [ghb.b])
                    kb.cp("dve", gh.ap[R, :], ghb.ap[R, :], [ghb.b], [gh.b])
                    kb.stt("dve", gh.ap[R, :], gh.ap[R, :], cst[R, 527:528], gc.ap[R, :], ALU.mult, ALU.add, [gh.b, gc.b, cst_b], [gh.b])
                    kb.cp("dve", q_.ap[R, :], gh.ap[R, :], [gh.b], [q_.b])
                    kb.S.op("act", (lambda q_=q_, k_=k_: lambda e: e.mul(k_.ap[96:98, :], q_.ap[64:66, :], -1.0))(), [q_.b], [k_.b])
                    kb.dma(scr["QA%d" % h][0][:, tk], q_.ap, [q_.b], [scr["QA%d" % h][1]])
                    kb.dma(scr["KA%d" % h][0][:, tk], k_.ap, [k_.b], [scr["KA%d" % h][1]])
                fm_list = [("DQ%d" % c, 16 + c, 32 + 2 * c) for c in range(4)] + [("DK%d" % c, 20 + c, 48 + 2 * c) for c in range(4)] + \
                          [("IQ%d" % c, 24 + c, None) for c in range(2)] + [("IK", 26, None)]
                for n_i, (nm, chunk, scol) in enumerate(fm_list):
                    pbk = 4 + (n_i % 2)
                    proj_fm(chunk, pbk)
                    d_ = gst[n_i % 3]
                    kb.cp("act", d_.ap[:, :], PS[pbk][:, :], [PB[pbk]], [d_.b])
                    dstb_cur[0] = d_.b
                    for b0 in (0, 64):
                        rope_block(PS[pbk], PB[pbk], b0, 8, g, cos16, sin16, d_.ap, t1, t2)
                    if scol is not None:
                        sq_ = sqs[n_i % 2]
                        kb.act(sq_.ap[:, :], d_.ap[:, :], AF.Square, [d_.b], [sq_.b])
                        hb = (chunk - 16) * 2 if chunk < 20 else (chunk - 20) * 2
                        base = 8 if chunk < 20 else 24
                        bound_update(sq_.ap, sq_.b, slice(0, 64), base + hb)
                        bound_update(sq_.ap, sq_.b, slice(64, 128), base + hb + 1)
                    kb.dma(scr[nm][0][:, tk], d_.ap, [d_.b], [scr[nm][1]])
                for tt_ in range(4):
                    tok = slice(tt_ * 128, (tt_ + 1) * 128)
                    gt = g * 4 + tt_
                    for vi, nm in enumerate(("VF", "DV")):
                        pbk = 4 + vi
                        for k in range(8):
                            kb.mm(PS[pbk][:, :], uT.ap[:, k * 512 + tt_ * 128:k * 512 + (tt_ + 1) * 128],
                                  wt.ap[:, k * NHT + vi * 512:k * NHT + (vi + 1) * 512], k == 0, k == 7, [wt.b, uT.b], [PB[pbk]])
                        v_ = vst[vi]
                        kb.cp("act", v_.ap[:, :], PS[pbk][:, :], [PB[pbk]], [v_.b])
                        kb.dma(scr[nm][0][gt * 128:(gt + 1) * 128, :], v_.ap, [v_.b], [scr[nm][1]])
                    for k in range(8):
                        kb.mm(PS[0][:, 0:4], uT.ap[:, k * 512 + tt_ * 128:k * 512 + (tt_ + 1) * 128],
                              wt.ap[:, k * NHT + 1024:k * NHT + 1028], k == 0, k == 7, [wt.b, uT.b], [PB[0]])
                    kb.ts("dve", iw_all[:, gt * 4:(gt + 1) * 4], PS[0][:, 0:4], 1.0 / 16.0, None, ALU.mult, None, [PB[0]], [iw_b])
            finish_bounds(16, 0.125)
            kb.act(iwabs[:, :], iw_all[:, :], AF.Abs, [iw_b], [iw_b])
            kb.act(iwsgn[:, :], iw_all[:, :], AF.Sign, [iw_b], [iw_b])

        def attn_head(j_blocks, Kt, Kb_, Qt, Qb_, K2, Q2, Vfn, mask_tiles, mask_b, dsa_masks, scale, negcol, dv, out_rows, PT, ost, orec):
            mixd, mixb = scr["MIX"]
            for jb in j_blocks:
                qs = slice(jb * 512, (jb + 1) * 512)
                nkt = 4 * jb + 4
                po = 3 + (jb % 2)
                pd = 5 + (jb % 2)
                pend = None
                for kt in range(nkt + 1):
                    if kt < nkt:
                        ps_ = kt % 3
                        ks = slice(kt * 128, (kt + 1) * 128)
                        diag = kt >= 4 * jb
                        last_plain = (K2 is None) and not (diag and mask_tiles is not None) and dsa_masks is None
                        kb.mm(PS[ps_][:, :], Kt[:, ks], Qt[:, qs], True, last_plain, [Kb_, Qb_], [PB[ps_]])
                        if K2 is not None:
                            kb.mm(PS[ps_][:, :], K2[:, ks], Q2[:, qs], False, not (diag and mask_tiles is not None), [Kb_, Qb_], [PB[ps_]])
                        if diag and mask_tiles is not None:
                            a = kt - 4 * jb
                            kb.mm(PS[ps_][:, :], ident_bf[:, :], mask_tiles[:, a * 512:(a + 1) * 512], False, True, [misc_b, mask_b], [PB[ps_]])
                        if dsa_masks is not None:
                            for a in range(4):
                                mb_ = dsa_masks[a]
                                kb.mm(PS[ps_][:, a * 128:(a + 1) * 128], mb_.ap[:, ks], ident_bf[:, :], False, a == 3, [mb_.b, misc_b], [PB[ps_]])
                        pt_ = PT[kt % 3]
                        kb.act(pt_.ap, PS[ps_][:, :], AF.Exp, [PB[ps_], negB_b], [pt_.b], bias=negB[:, negcol:negcol + 1], scale=scale)
                    if pend is not None:
                        pk_, ppt = pend
                        kb.mm(PS[po][0:dv, :], Vfn(pk_), ppt.ap, pk_ == 0, pk_ == nkt - 1, [ppt.b, Vb_cur[0]], [PB[po]])
                        kb.mm(PS[pd][0:dv, :], ones_bf[:, 0:dv], ppt.ap, pk_ == 0, pk_ == nkt - 1, [ppt.b, misc_b], [PB[pd]])
                    pend = (kt, PT[kt % 3]) if kt < nkt else None
                rc = orec[jb % 2]
                os_ = ost[jb % 2]
                kb.recip(rc.ap[0:dv, :], PS[pd][0:dv, :], [PB[pd]], [rc.b])
                kb.tt("dve", os_.ap[0:dv, :], PS[po][0:dv, :], rc.ap[0:dv, :], ALU.mult, [PB[po], rc.b], [os_.b])
                kb.dma(mixd[out_rows, qs], os_.ap[0:dv, :], [os_.b], [mixb])

        Vb_cur = [None]

        def fox_attn_phase(s):
            S.fence()
            A.reset()
            vall = A.bf(NKT * 512, "vall")
            vd, vb = scr["VF"]
            kb.dma(vall.ap.rearrange("p (t d) -> p t d", d=512), vd.rearrange("(t p) d -> p t d", p=128), [vb], [vall.b])
            Vb_cur[0] = vall.b
            cm = build_mask(0)
            KA = [A.bf(SQ, "KA%d" % i) for i in range(2)]
            QA = [A.bf(SQ, "QA%d" % i) for i in range(2)]
            PT = [A.bf(512, "PT%d" % i) for i in range(3)]
            ost = [A.bf(512, "ost%d" % i) for i in range(2)]
            orec = [A.f32(512, "orec%d" % i) for i in range(2)]
            for h in range(8):
                k_ = KA[h % 2]
                q_ = QA[h % 2]
                kb.dma(k_.ap, scr["KA%d" % h][0][:, :], [scr["KA%d" % h][1]], [k_.b])
                kb.dma(q_.ap, scr["QA%d" % h][0][:, :], [scr["QA%d" % h][1]], [q_.b])
                vv = vall.ap.rearrange("p (t d) -> p t d", d=512)
                attn_head(range(NG), k_.ap, k_.b, q_.ap, q_.b, None, None,
                          (lambda h: lambda kt: vv[:, kt, h * 64:(h + 1) * 64])(h),
                          cm.ap, cm.b, None, 0.125, h, 64, slice(h * 64, (h + 1) * 64), PT, ost, orec)

        def dsa_phase(s):
            S.fence()
            A.reset()
            vall = A.bf(NKT * 512, "dvall")
            vd, vb = scr["DV"]
            kb.dma(vall.ap.rearrange("p (t d) -> p t d", d=512), vd.rearrange("(t p) d -> p t d", p=128), [vb], [vall.b])
            Vb_cur[0] = vall.b
            vv = vall.ap.rearrange("p (t d) -> p t d", d=512)
            DKs = []
            for c in range(4):
                t_ = A.bf(SQ, "DK%d" % c)
                kb.dma(t_.ap, scr["DK%d" % c][0][:, :], [scr["DK%d" % c][1]], [t_.b])
                DKs.append(t_)
            IK = A.bf(SQ, "IK")
            kb.dma(IK.ap, scr["IK"][0][:, :], [scr["IK"][1]], [IK.b])
            DQb = [[A.bf(512, "DQ%d_%d" % (c, i)) for c in range(4)] for i in range(2)]
            IQb = [[A.bf(512, "IQ%d_%d" % (c, i)) for c in range(2)] for i in range(2)]
            Sc = A.f32(SQ, "Sc")
            ccmq = build_mask(2)
            junk = A.bf(SQ, "junk")
            maskb = [A.bf(SQ, "maskb%d" % a) for a in range(4)]
            rr_ = [A.f32(512, "rr%d" % i) for i in range(2)]
            PT = [A.bf(512, "PT%d" % i) for i in range(3)]
            ost = [A.bf(512, "ost%d" % i) for i in range(2)]
            orec = [A.f32(512, "orec%d" % i) for i in range(2)]
            bs = smallf[:, 44:60]
            for jb in range(NG):
                qs = slice(jb * 512, (jb + 1) * 512)
                dq = DQb[jb % 2]
                iq = IQb[jb % 2]
                for c in range(4):
                    kb.dma(dq[c].ap, scr["DQ%d" % c][0][:, qs], [scr["DQ%d" % c][1]], [dq[c].b])
                for c in range(2):
                    kb.dma(iq[c].ap, scr["IQ%d" % c][0][:, qs], [scr["IQ%d" % c][1]], [iq[c].b])
                nk = 512 * (jb + 1)
                for a in range(4):
                    qt = jb * 4 + a
                    for kbk in range(jb + 1):
                        kk = slice(kbk * 512, (kbk + 1) * 512)
                        for ih in range(4):
                            rows = slice((ih % 2) * 64, (ih % 2) * 64 + 64)
                            kb.mm(PS[ih][:, :], iq[ih // 2].ap[rows, a * 128:(a + 1) * 128], IK.ap[rows, kk], True, True,
                                  [iq[ih // 2].b, IK.b], [PB[ih]])
                        for ih in range(4):
                            r_ = rr_[ih % 2]
                            kb.act(r_.ap, PS[ih][:, :], AF.Relu, [PB[ih], iw_b], [r_.b], scale=iwabs[:, qt * 4 + ih:qt * 4 + ih + 1])
                            if ih == 0:
                                kb.ts("dve", Sc.ap[:, kk], r_.ap, iwsgn[:, qt * 4:qt * 4 + 1], None, ALU.mult, None, [r_.b, iw_b], [Sc.b])
                            else:
                                kb.stt("dve", Sc.ap[:, kk], r_.ap, iwsgn[:, qt * 4 + ih:qt * 4 + ih + 1], Sc.ap[:, kk], ALU.mult, ALU.add,
                                       [r_.b, iw_b, Sc.b], [Sc.b])
                    lo = bs[:, 0:1]
                    hi = bs[:, 1:2]
                    mid = bs[:, 2:3]
                    cnt = bs[:, 3:4]
                    cond = bs[:, 4:5]
                    dd = bs[:, 5:6]
                    bsb = Buf("bs")
                    if qt >= 2:
                        kb.S.op("dve", (lambda nk=nk: lambda e: e.tensor_reduce(hi, Sc.ap[:, 0:nk], AX.X, ALU.max))(), [Sc.b], [bsb])
                        kb.S.op("dve", (lambda nk=nk: lambda e: e.tensor_reduce(lo, Sc.ap[:, 0:nk], AX.X, ALU.min))(), [Sc.b], [bsb])
                        kb.tt("dve", dd, hi, lo, ALU.subtract, [bsb], [bsb])
                        kb.stt("dve", hi, dd, 0.001, hi, ALU.mult, ALU.add, [bsb], [bsb])
                        kb.ts("dve", hi, hi, 1e-6, None, ALU.add, None, [bsb], [bsb])
                    dk_ = slice(jb * 512, (jb + 1) * 512)
                    kb.tt("dve", Sc.ap[:, dk_], Sc.ap[:, dk_], ccmq.ap[:, a * 512:(a + 1) * 512], ALU.add, [Sc.b, ccmq.b], [Sc.b])
                    if qt >= 2:
                        for itn in range(20):
                            kb.tt("dve", mid, lo, hi, ALU.add, [bsb], [bsb])
                            kb.ts("dve", mid, mid, 0.5, None, ALU.mult, None, [bsb], [bsb])
                            kb.ts("dve", junk.ap[:, 0:nk], Sc.ap[:, 0:nk], mid, 0.0, ALU.is_ge, ALU.add, [Sc.b, bsb], [junk.b, bsb], accum_out=cnt)
                            kb.ts("dve", cond, cnt, 255.5, None, ALU.is_ge, None, [bsb], [bsb])
                            kb.tt("dve", dd, mid, lo, ALU.subtract, [bsb], [bsb])
                            kb.stt("dve", lo, dd, cond, lo, ALU.mult, ALU.add, [bsb], [bsb])
                            kb.tt("dve", dd, hi, mid, ALU.subtract, [bsb], [bsb])
                            kb.stt("dve", hi, dd, cond, mid, ALU.mult, ALU.add, [bsb], [bsb])
                    else:
                        kb.memset("dve", lo, -1e29, [bsb])
                    kb.ts("dve", maskb[a].ap[:, 0:nk], Sc.ap[:, 0:nk], lo, NEGM, ALU.is_lt, ALU.mult, [Sc.b, bsb], [maskb[a].b])
                for h in range(8):
                    c = h // 2
                    rows = slice((h % 2) * 64, (h % 2) * 64 + 64)
                    attn_block_dsa(jb, DKs[c], dq[c], rows, h, vv, maskb, PT, ost, orec)

        def attn_block_dsa(jb, DKc, dqc, rows, h, vv, maskb, PT, ost, orec):
            mixd, mixb = scr["MIX"]
            nkt = 4 * jb + 4
            po = 3 + (h % 2)
            pd = 5 + (h % 2)
            pend = None
            qs = slice(jb * 512, (jb + 1) * 512)
            for kt in range(nkt + 1):
                if kt < nkt:
                    ps_ = kt % 3
                    ks = slice(kt * 128, (kt + 1) * 128)
                    kb.mm(PS[ps_][:, :], DKc.ap[rows, ks], dqc.ap[rows, :], True, False, [DKc.b, dqc.b], [PB[ps_]])
                    for a in range(4):
                        mb_ = maskb[a]
                        kb.mm(PS[ps_][:, a * 128:(a + 1) * 128], mb_.ap[:, ks], ident_bf[:, :], False, a == 3, [mb_.b, misc_b], [PB[ps_]])
                    pt_ = PT[kt % 3]
                    kb.act(pt_.ap, PS[ps_][:, :], AF.Exp, [PB[ps_], negB_b], [pt_.b], bias=negB[:, 8 + h:9 + h], scale=0.125)
                if pend is not None:
                    pk_, ppt = pend
                    kb.mm(PS[po][0:64, :], vv[:, pk_, h * 64:(h + 1) * 64], ppt.ap, pk_ == 0, pk_ == nkt - 1, [ppt.b, Vb_cur[0]], [PB[po]])
                    kb.mm(PS[pd][0:64, :], ones_bf[:, 0:64], ppt.ap, pk_ == 0, pk_ == nkt - 1, [ppt.b, misc_b], [PB[pd]])
                pend = (kt, PT[kt % 3]) if kt < nkt else None
            rc = orec[h % 2]
            os_ = ost[h % 2]
            kb.recip(rc.ap[0:64, :], PS[pd][0:64, :], [PB[pd]], [rc.b])
            kb.tt("dve", os_.ap[0:64, :], PS[po][0:64, :], rc.ap[0:64, :], ALU.mult, [PB[po], rc.b], [os_.b])
            kb.dma(mixd[512 + h * 64:512 + (h + 1) * 64, qs], os_.ap[0:64, :], [os_.b], [mixb])

        def mla_proj_phase(layer, s):
            global A_sq, A_rs, A_tmp
            j = layer // 2
            S.fence()
            A.reset()
            wdn = A.bf(8 * 768, "wdn")
            wuq = A.bf(3 * 1536, "wuq")
            wukv = A.bf(2 * 2048, "wukv")
            mark = A.off
            wdn.b = load_w_bf(wdn.ap, mla_down[j], 8 * 768, "wdn")
            A.off = mark
            wuq.b = load_w_bf(wuq.ap, mla_uq[j], 3 * 1536, "wuq")
            A.off = mark
            wukv.b = load_w_bf(wukv.ap, mla_ukv[j], 2 * 2048, "wukv")
            A.off = mark
            kb.dma(qn_t[:, 0:3], mla_qn[j, :, :], [], [qn_b])
            kb.dma(qn_t[:, 3:5], mla_kvn[j, :, :], [], [qn_b])
            S.fence()
            cos64 = A.f32(SQ, "cos64")
            sin64 = A.f32(SQ, "sin64")
            mk2 = A.off
            rope_tables(s, 526, cos64, sin64)
            A.off = mk2
            S.fence()
            hg = A.f32(8 * 512, "hg")
            uT = A.bf(8 * 512, "uT")
            A_sq = A.bf(8 * 512, "sq")
            A_rs = A.f32(512, "rs")
            A_tmp = [A.f32(512, "tmp%d" % i) for i in range(2)]
            cl = A.f32(6 * 512, "cl")
            cn = A.bf(5 * 512, "cn")
            sq2 = A.bf(5 * 512, "sq2")
            rs2 = A.f32(1024, "rs2")
            gst = [A.bf(512, "gst%d" % i) for i in range(3)]
            sqs = [A.bf(512, "sqs%d" % i) for i in range(2)]
            t1 = A.f32(512, "t1")
            t2 = A.f32(512, "t2")
            vst = [A.bf(512, "vst%d" % i) for i in range(2)]
            kb.memset("pool", stat[:, 0:32], 0.0, [stat_b])
            epsq = smallf[:, 2:3]
            epsk = smallf[:, 3:4]
            kb.memset("pool", epsq, 384.0 * 1e-6, [misc_b])
            kb.memset("pool", epsk, 256.0 * 1e-6, [misc_b])
            kb.ts("dve", qn_t[:, 0:3], qn_t[:, 0:3], float(np.sqrt(384.0)), None, ALU.mult, None, [qn_b], [qn_b])
            kb.ts("dve", qn_t[:, 3:5], qn_t[:, 3:5], 16.0, None, ALU.mult, None, [qn_b], [qn_b])
            hsrc, hsrc_b = hT[s][:, :], hT_b[s]
            for g in range(NG):
                tk = slice(g * 512, (g + 1) * 512)
                norm_group(hsrc, hsrc_b, s, 1, g, hg, uT)
                for c in range(6):
                    pbk = c % 2
                    for k in range(8):
                        kb.mm(PS[pbk][:, :], wdn.ap[:, k * 768 + c * 128:k * 768 + (c + 1) * 128], uT.ap[:, k * 512:(k + 1) * 512],
                              k == 0, k == 7, [wdn.b, uT.b], [PB[pbk]])
                    if c < 5:
                        kb.cp("act", cl.ap[:, c * 512:(c + 1) * 512], PS[pbk][:, :], [PB[pbk]], [cl.b])
                        kb.act(sq2.ap[:, c * 512:(c + 1) * 512], PS[pbk][:, :], AF.Square, [PB[pbk]], [sq2.b])
                    else:
                        d_ = gst[0]
                        dstb_cur[0] = d_.b
                        rope_block(PS[pbk], PB[pbk], 0, 32, g, cos64, sin64, d_.ap, t1, t2)
                        kb.act(sqs[0].ap[0:64, :], d_.ap[0:64, :], AF.Square, [d_.b], [sqs[0].b])
                        kb.mm(PS[2][:, :], ones_bf[0:64, :], sqs[0].ap[0:64, :], True, True, [sqs[0].b, misc_b], [PB[2]])
                        kb.cp("dve", rs2.ap[:, 512:1024], PS[2][:, :], [PB[2]], [rs2.b])
                        kb.dma(scr["KR"][0][:, tk], d_.ap[0:64, :], [d_.b], [scr["KR"][1]])
                for (c0, c1, epsc, col) in ((0, 3, epsq, 0), (3, 5, epsk, 1)):
                    for c in range(c0, c1):
                        kb.mm(PS[3][:, :], ones_bf[:, :], sq2.ap[:, c * 512:(c + 1) * 512], c == c0, c == c1 - 1, [sq2.b, misc_b], [PB[3]])
                    kb.act(A_rs.ap, PS[3][:, :], AF.Sqrt, [PB[3]], [A_rs.b], bias=epsc, scale=1.0)
                    kb.recip(A_rs.ap, A_rs.ap, [A_rs.b], [A_rs.b])
                    for c in range(c0, c1):
                        kb.stt("dve", cn.ap[:, c * 512:(c + 1) * 512], cl.ap[:, c * 512:(c + 1) * 512], qn_t[:, c:c + 1], A_rs.ap,
                               ALU.mult, ALU.mult, [cl.b, A_rs.b, qn_b], [cn.b])
                for h in range(8):
                    pn = (h % 2) * 2
                    pr = (h % 2) * 2 + 1
                    for k in range(3):
                        kb.mm(PS[pn][:, :], wuq.ap[:, k * 1536 + h * 192:k * 1536 + h * 192 + 128], cn.ap[:, k * 512:(k + 1) * 512],
                              k == 0, k == 2, [wuq.b, cn.b], [PB[pn]])
                    for k in range(3):
                        kb.mm(PS[pr][0:64, :], wuq.ap[:, k * 1536 + h * 192 + 128:k * 1536 + h * 192 + 192], cn.ap[:, k * 512:(k + 1) * 512],
                              k == 0, k == 2, [wuq.b, cn.b], [PB[pr]])
                    dn = gst[1]
                    dr = gst[2]
                    kb.cp("act", dn.ap[:, :], PS[pn][:, :], [PB[pn]], [dn.b])
                    dstb_cur[0] = dr.b
                    rope_block(PS[pr], PB[pr], 0, 32, g, cos64, sin64, dr.ap, t1, t2)
                    kb.act(sqs[0].ap[:, :], dn.ap[:, :], AF.Square, [dn.b], [sqs[0].b])
                    kb.act(sqs[1].ap[0:64, :], dr.ap[0:64, :], AF.Square, [dr.b], [sqs[1].b])
                    kb.mm(PS[6][:, :], ones_bf[:, :], sqs[0].ap[:, :], True, False, [sqs[0].b, misc_b], [PB[6]])
                    kb.mm(PS[6][:, :], ones_bf[0:64, :], sqs[1].ap[0:64, :], False, True, [sqs[1].b, misc_b], [PB[6]])
                    kb.S.op("dve", lambda e: e.tensor_reduce(smallf[:, 32:33], PS[6][:, :], AX.X, ALU.max), [PB[6]], [misc_b])
                    kb.tt("dve", stat[:, h:h + 1], stat[:, h:h + 1], smallf[:, 32:33], ALU.max, [misc_b, stat_b], [stat_b])
                    kb.dma(scr["QN%d" % h][0][:, tk], dn.ap, [dn.b], [scr["QN%d" % h][1]])
                    kb.dma(scr["QR%d" % h][0][:, tk], dr.ap[0:64, :], [dr.b], [scr["QR%d" % h][1]])
                for h in range(8):
                    pn = 4 + (h % 2)
                    for k in range(2):
                        kb.mm(PS[pn][:, :], wukv.ap[:, k * 2048 + h * 128:k * 2048 + (h + 1) * 128], cn.ap[:, (3 + k) * 512:(4 + k) * 512],
                              k == 0, k == 1, [wukv.b, cn.b], [PB[pn]])
                    dn = gst[h % 2]
                    kb.cp("act", dn.ap[:, :], PS[pn][:, :], [PB[pn]], [dn.b])
                    kb.act(sqs[h % 2].ap[:, :], PS[pn][:, :], AF.Square, [PB[pn]], [sqs[h % 2].b])
                    kb.mm(PS[6][:, :], ones_bf[:, :], sqs[h % 2].ap[:, :], True, True, [sqs[h % 2].b, misc_b], [PB[6]])
                    kb.tt("dve", t1.ap, PS[6][:, :], rs2.ap[:, 512:1024], ALU.add, [PB[6], rs2.b], [t1.b])
                    kb.S.op("dve", lambda e: e.tensor_reduce(smallf[:, 32:33], t1.ap, AX.X, ALU.max), [t1.b], [misc_b])
                    kb.tt("dve", stat[:, 16 + h:17 + h], stat[:, 16 + h:17 + h], smallf[:, 32:33], ALU.max, [misc_b, stat_b], [stat_b])
                    kb.dma(scr["KN%d" % h][0][:, tk], dn.ap, [dn.b], [scr["KN%d" % h][1]])
                for tt_ in range(4):
                    gt = g * 4 + tt_
                    for half in range(2):
                        pbk = 2 + half
                        for k in range(2):
                            kb.mm(PS[pbk][:, :], cn.ap[:, (3 + k) * 512 + tt_ * 128:(3 + k) * 512 + (tt_ + 1) * 128],
                                  wukv.ap[:, k * 2048 + 1024 + half * 512:k * 2048 + 1024 + (half + 1) * 512], k == 0, k == 1,
                                  [wukv.b, cn.b], [PB[pbk]])
                        v_ = vst[half]
                        kb.cp("act", v_.ap[:, :], PS[pbk][:, :], [PB[pbk]], [v_.b])
                        kb.dma(scr["MV"][0][gt * 128:(gt + 1) * 128, half * 512:(half + 1) * 512], v_.ap, [v_.b], [scr["MV"][1]])
            finish_bounds(8, float(192.0 ** -0.5))

        def mla_attn_phase(s):
            S.fence()
            A.reset()
            vall = A.bf(NKT * 1024, "mvall")
            vd, vb = scr["MV"]
            kb.dma(vall.ap.rearrange("p (t d) -> p t d", d=1024), vd.rearrange("(t p) d -> p t d", p=128), [vb], [vall.b])
            Vb_cur[0] = vall.b
            vv = vall.ap.rearrange("p (t d) -> p t d", d=1024)
            KR = A.bf(SQ, "KR")
            ccm = build_mask(1)
            kb.dma(KR.ap[0:64, :], scr["KR"][0][:, :], [scr["KR"][1]], [KR.b])
            KN = [A.bf(SQ, "KN%d" % i) for i in range(2)]
            QN = [A.bf(SQ, "QN%d" % i) for i in range(2)]
            QR = [A.bf(SQ, "QR%d" % i) for i in range(2)]
            PT = [A.bf(512, "PT%d" % i) for i in range(3)]
            ost = [A.bf(512, "ost%d" % i) for i in range(2)]
            orec = [A.f32(512, "orec%d" % i) for i in range(2)]
            sc = float(192.0 ** -0.5)
            for h in range(8):
                kn = KN[h % 2]
                qn = QN[h % 2]
                qr = QR[h % 2]
                kb.dma(kn.ap, scr["KN%d" % h][0][:, :], [scr["KN%d" % h][1]], [kn.b])
                kb.dma(qn.ap, scr["QN%d" % h][0][:, :], [scr["QN%d" % h][1]], [qn.b])
                kb.dma(qr.ap[0:64, :], scr["QR%d" % h][0][:, :], [scr["QR%d" % h][1]], [qr.b])
                kq_b = Buf("kq")
                attn_head_mla(h, kn, qn, KR, qr, vv, PT, ost, orec, sc, ccm)

        def attn_head_mla(h, kn, qn, KR, qr, vv, PT, ost, orec, sc, ccm):
            mixd, mixb = scr["MIX"]
            for jb in range(NG):
                qs = slice(jb * 512, (jb + 1) * 512)
                nkt = 4 * jb + 4
                po = 3 + (jb % 2)
                pd = 5 + (jb % 2)
                pend = None
                for kt in range(nkt + 1):
                    if kt < nkt:
                        ps_ = kt % 3
                        ks = slice(kt * 128, (kt + 1) * 128)
                        diag = kt >= 4 * jb
                        kb.mm(PS[ps_][:, :], kn.ap[:, ks], qn.ap[:, qs], True, False, [kn.b, qn.b], [PB[ps_]])
                        kb.mm(PS[ps_][:, :], KR.ap[0:64, ks], qr.ap[0:64, qs], False, not diag, [KR.b, qr.b], [PB[ps_]])
                        if diag:
                            a = kt - 4 * jb
                            kb.mm(PS[ps_][:, :], ident_bf[:, :], ccm.ap[:, a * 512:(a + 1) * 512], False, True, [misc_b, ccm.b], [PB[ps_]])
                        pt_ = PT[kt % 3]
                        kb.act(pt_.ap, PS[ps_][:, :], AF.Exp, [PB[ps_], negB_b], [pt_.b], bias=negB[:, h:h + 1], scale=sc)
                    if pend is not None:
                        pk_, ppt = pend
                        kb.mm(PS[po][:, :], vv[:, pk_, h * 128:(h + 1) * 128], ppt.ap, pk_ == 0, pk_ == nkt - 1, [ppt.b, Vb_cur[0]], [PB[po]])
                        kb.mm(PS[pd][:, :], ones_bf[:, :], ppt.ap, pk_ == 0, pk_ == nkt - 1, [ppt.b, misc_b], [PB[pd]])
                    pend = (kt, PT[kt % 3]) if kt < nkt else None
                rc = orec[jb % 2]
                os_ = ost[jb % 2]
                kb.recip(rc.ap, PS[pd][:, :], [PB[pd]], [rc.b])
                kb.tt("dve", os_.ap, PS[po][:, :], rc.ap, ALU.mult, [PB[po], rc.b], [os_.b])
                kb.dma(mixd[h * 128:(h + 1) * 128, qs], os_.ap, [os_.b], [mixb])

        def final_phase():
            global A_sq, A_rs, A_tmp
            S.fence()
            A.reset()
            hg = A.f32(8 * 512, "hg")
            uo = [A.f32(8 * 512, "uo%d" % i) for i in range(2)]
            A_sq = A.bf(8 * 512, "sq")
            A_rs = A.f32(512, "rs")
            A_tmp = [A.f32(512, "tmp%d" % i) for i in range(2)]
            it = 0
            for s in range(NSEQ):
                for g in range(NG):
                    u_ = uo[it % 2]
                    it += 1
                    norm_group(hT[s][:, :], hT_b[s], s, 0, g, hg, u_, final=True)
                    dst = outT[s].rearrange("(c p) t -> p c t", p=128)[:, :, g * 512:(g + 1) * 512]
                    S.out_dma.append(kb.S.op("sp", (lambda dst=dst, u_=u_: lambda e: e.dma_start(out=dst, in_=u_.ap.rearrange("p (c t) -> p c t", c=8)))(),
                                             [u_.b], [], dma=True))

        steps = []
        for layer in range(DEPTH):
            steps.append(("mod%d" % layer, (lambda layer=layer: (compute_mod(layer), compute_abg(layer)))))
            steps.append(("ffn%d_0" % layer, (lambda layer=layer: ffn_phase(layer, 0))))
            for s in range(NSEQ):
                if layer % 2 == 0:
                    steps.append(("hproj%d_%d" % (layer, s), (lambda layer=layer, s=s: hyb_proj_phase(layer, s))))
                    steps.append(("fox%d_%d" % (layer, s), (lambda layer=layer, s=s: fox_attn_phase(s))))
                    steps.append(("dsa%d_%d" % (layer, s), (lambda layer=layer, s=s: dsa_phase(s))))
                    steps.append(("oproj%d_%d" % (layer, s), (lambda layer=layer, s=s: outproj_phase(layer, hyb_out[layer // 2], s))))
                else:
                    steps.append(("mproj%d_%d" % (layer, s), (lambda layer=layer, s=s: mla_proj_phase(layer, s))))
                    steps.append(("mattn%d_%d" % (layer, s), (lambda layer=layer, s=s: mla_attn_phase(s))))
                    steps.append(("oproj%d_%d" % (layer, s), (lambda layer=layer, s=s: outproj_phase(layer, mla_out[layer // 2], s))))
            steps.append(("ffn%d_1" % layer, (lambda layer=layer: ffn_phase(layer, 1))))
        steps.append(("final", final_phase))
        for name, fn in steps:
            fn()
            if cfg.get("stop") == name:
                break
        if cfg.get("dbg_sb"):
            S.fence()
            named = dict(modT=modT, ABG=ABG, condT=condT, stat=stat, negB=negB, iw_all=iw_all, ngT=ngT, smallf=smallf)
            for nm in cfg["dbg_sb"]:
                t_ = named[nm]
                dd_ = nc.dram_tensor("dbgsb_" + nm, list(t_.shape), F32, kind="ExternalOutput")
                kb.S.op("sp", (lambda dd_=dd_, t_=t_: lambda e: e.dma_start(out=dd_[:, :], in_=t_[:]))(), [], [], dma=True)
        S.emit()
    return nc


PERM64 = list(range(0, 8)) + list(range(16, 40)) + list(range(8, 16)) + list(range(40, 64))


def make_consts():
    c = np.zeros((128, 640), np.float32)
    p = np.arange(128)
    c[:, 0:512] = np.arange(512)[None, :]
    c[:, 512] = p
    for a in range(4):
        c[:, 513 + a] = 128 * a + p
        c[:, 517 + a] = 64 * ((128 * a + p) // 64)
        c[:, 521 + a] = 64 * ((128 * a + p) // 64 + 1)
    r = p % 64
    i16 = np.where(r < 8, r, np.where((r >= 32) & (r < 40), r - 32, 0))
    c[:, 525] = -np.log(500000.0) * 2.0 * i16 / 16.0
    i64 = np.where(r < 32, r, r - 32)
    c[:, 526] = -np.log(10000.0) * 2.0 * i64 / 64.0
    c[:, 527] = np.where((p % 32) == 1, -1.0, 0.0)
    return c


def lay_k(w, ncols_pad=None):
    K, N = w.shape
    kc = K // 128
    return np.ascontiguousarray(w.reshape(kc, 128, N).transpose(1, 0, 2).reshape(128, kc * N))


def gather_cols(w, idx):
    idx = np.asarray(idx)
    out = np.zeros((w.shape[0], len(idx)), w.dtype)
    m = idx >= 0
    out[:, m] = w[:, idx[m]]
    return out


def prep_shared(inp, DEPTH):
    f = {}
    n_even = (DEPTH + 1) // 2
    n_odd = DEPTH // 2
    f["consts"] = make_consts()
    f["ada_w"] = np.stack([lay_k(inp["ada_w"][i]) for i in range(DEPTH)])
    f["ada_b"] = np.ascontiguousarray(inp["ada_b"][:DEPTH].reshape(DEPTH, 1, 9216))
    ng = inp["norm_g"][:DEPTH].reshape(DEPTH * 3, 8, 128).transpose(0, 2, 1)
    f["norm_gT"] = np.ascontiguousarray(ng)
    f["final_gT"] = np.ascontiguousarray(inp["final_g"].reshape(8, 128).T)
    f["w_gate"] = np.stack([lay_k(inp["ffn_w_gate"][i, j]) for i in range(DEPTH) for j in range(2)])
    f["w_up"] = np.stack([lay_k(inp["ffn_w_up"][i, j]) for i in range(DEPTH) for j in range(2)])
    f["w_down"] = np.stack([lay_k(inp["ffn_w_down"][i, j]) for i in range(DEPTH) for j in range(2)])
    off = np.cumsum([0, 512, 512, 512, 8, 512, 512, 512, 256, 4, 64])
    o_fq, o_fk, o_fv, o_ff, o_dq, o_dk, o_dv, o_iq, o_iw, o_ik = off[:10]
    fm = []
    for h in range(8):
        blk = [-1] * 128
        blk[0:64] = list(range(o_fq + h * 64, o_fq + (h + 1) * 64))
        blk[64] = o_ff + h
        blk[65] = o_ff + h
        fm += blk
    for h in range(8):
        blk = [-1] * 128
        blk[0:64] = list(range(o_fk + h * 64, o_fk + (h + 1) * 64))
        fm += blk
    for base in (o_dq, o_dk):
        for h in range(8):
            fm += [base + h * 64 + d for d in PERM64]
    for h in range(4):
        fm += [o_iq + h * 64 + d for d in PERM64]
    fm += [o_ik + d for d in PERM64] * 2
    assert len(fm) == 3456
    tm = list(range(o_fv, o_fv + 512)) + list(range(o_dv, o_dv + 512)) + list(range(o_iw, o_iw + 4)) + [-1] * 4
    f["hyb_fm"] = np.stack([lay_k(gather_cols(inp["hyb_w_in"][j], fm)) for j in range(n_even)])
    f["hyb_tm"] = np.stack([lay_k(gather_cols(inp["hyb_w_in"][j], tm)) for j in range(n_even)])
    f["hyb_out"] = np.stack([lay_k(inp["hyb_w_out"][j]) for j in range(n_even)])
    f["fox_b"] = np.ascontiguousarray(np.broadcast_to(inp["fox_b_f"][:n_even, None, :], (n_even, 128, 8))).astype(np.float32)
    if n_odd:
        dn_idx = list(range(704)) + [-1] * 64
        f["mla_down"] = np.stack([lay_k(gather_cols(inp["mla_w_down"][j], dn_idx)) for j in range(n_odd)])
        f["mla_qn"] = np.ascontiguousarray(inp["mla_q_norm"][:n_odd].reshape(n_odd, 3, 128).transpose(0, 2, 1))
        f["mla_kvn"] = np.ascontiguousarray(inp["mla_kv_norm"][:n_odd].reshape(n_odd, 2, 128).transpose(0, 2, 1))
        f["mla_uq"] = np.stack([lay_k(inp["mla_w_uq"][j]) for j in range(n_odd)])
        kv_idx = [h * 256 + d for h in range(8) for d in range(128)] + [h * 256 + 128 + d for h in range(8) for d in range(128)]
        f["mla_ukv"] = np.stack([lay_k(gather_cols(inp["mla_w_ukv"][j], kv_idx)) for j in range(n_odd)])
        f["mla_out"] = np.stack([lay_k(inp["mla_w_out"][j]) for j in range(n_odd)])
    return f


def run_cfg(inp, cfg, n_cores):
    NSEQ = cfg["NSEQ"]
    DEPTH = cfg["DEPTH"]
    shared = prep_shared(inp, DEPTH)
    nc = build_program(cfg)
    in_maps = []
    for c in range(n_cores):
        sl = slice(c * NSEQ, (c + 1) * NSEQ)
        m = dict(shared)
        m["xT"] = np.ascontiguousarray(inp["x"][sl].transpose(0, 2, 1))
        cc = inp["c"][sl]
        m["cT"] = np.ascontiguousarray(cc.reshape(NSEQ, 8, 128).transpose(2, 1, 0).reshape(128, 8 * NSEQ))
        m["pos"] = np.ascontiguousarray(inp["positions"][sl]).astype(np.int32)
        in_maps.append(m)
    res = run_bass_kernel_spmd(nc, in_maps, core_ids=list(range(n_cores)))
    return res


def kernel(**inputs):
    inp = {k: np.asarray(v) for k, v in inputs.items()}
    cfg = dict(S=4096, NSEQ=2, DEPTH=4)
    res = run_cfg(inp, cfg, 8)
    outs = [np.asarray(r["outT"]).transpose(0, 2, 1) for r in res.results]
    return np.ascontiguousarray(np.concatenate(outs, axis=0)).astype(np.float32)
```

```python
import contextlib
import numpy as np
import ml_dtypes
import concourse.bass as bass
import concourse.mybir as mybir
from concourse.bass_utils import run_bass_kernel_spmd

F32 = mybir.dt.float32
BF16 = mybir.dt.bfloat16
I32 = mybir.dt.int32
ALU = mybir.AluOpType
AF = mybir.ActivationFunctionType
AX = mybir.AxisListType

STREAMS = ("pe", "act", "dve", "pool", "sp")
N_DMA_SEMS = 12
EPOCH = 20000

D = 1024
DFF = 2816
NFC = 22
NEGM = -30000.0


class Buf:
    __slots__ = ("name", "w", "r", "excl")

    def __init__(self, name, excl=False):
        self.name = name
        self.w = None
        self.r = []
        self.excl = excl


class Sched:
    def __init__(self, nc):
        self.nc = nc
        self.ops = []
        self.cnt = {(s, k): 0 for s in STREAMS for k in "cd"}
        self.seen = {s: {} for s in STREAMS}
        self.fence_deps = {s: set() for s in STREAMS}
        self.out_dma = []

    def fence(self):
        deps = set()
        for s in STREAMS:
            n = self.cnt[(s, "c")]
            if n > 0:
                deps.add((s, "c", n - 1))
            nd = self.cnt[(s, "d")]
            for j in range(max(0, nd - N_DMA_SEMS), nd):
                deps.add((s, "d", j))
        for s in STREAMS:
            self.fence_deps[s] = set(deps)
            self.seen[s] = {k: v for k, v in self.seen[s].items() if not isinstance(k, tuple)}

    def op(self, stream, fn, reads=(), writes=(), dma=False):
        kind = "d" if dma else "c"
        idx = self.cnt[(stream, kind)]
        self.cnt[(stream, kind)] += 1
        me = (stream, kind, idx)
        deps = set()
        if self.fence_deps[stream]:
            deps |= self.fence_deps[stream]
            self.fence_deps[stream] = set()
        for b in reads:
            if not b.excl and b.w is not None:
                deps.add(b.w)
        wl = list(writes) + [b for b in reads if b.excl]
        for b in wl:
            if b.w is not None:
                deps.add(b.w)
            deps.update(b.r)
        for b in reads:
            if not b.excl:
                if dma:
                    b.r.append(me)
                else:
                    b.r = [x for x in b.r if not (x[0] == stream and x[1] == "c")] + [me]
        for b in wl:
            b.w = me
            b.r = []
        fd = []
        seen = self.seen[stream]
        best = {}
        for d in deps:
            ps, pk, pi = d
            if d == me:
                continue
            if pk == "d":
                if d in seen:
                    continue
                seen[d] = True
                fd.append(d)
            else:
                if ps == stream and stream == "pe" and not dma:
                    continue
                if ps == stream and pi >= idx and not dma:
                    continue
                if seen.get(ps, -1) >= pi:
                    continue
                if best.get(ps, -1) < pi:
                    best[ps] = pi
        for ps, pi in best.items():
            seen[ps] = pi
            fd.append((ps, "c", pi))
        self.ops.append((stream, kind, idx, fn, fd))
        return me

    def emit(self):
        nc = self.nc
        needs = {s: [False] * self.cnt[(s, "c")] for s in STREAMS}
        for stream, kind, idx, fn, fd in self.ops:
            for (ps, pk, pi) in fd:
                if pk == "c":
                    needs[ps][pi] = True
        val = {}
        for s in STREAMS:
            c = 0
            v = []
            for n in needs[s]:
                if n:
                    c += 1
                v.append(c)
            val[s] = v
        per = {s: [] for s in STREAMS}
        for o in self.ops:
            per[o[0]].append(o)
        self.nwaits = 0
        with contextlib.ExitStack() as st:
            sems = {}
            for s in STREAMS:
                tot = val[s][-1] if val[s] else 0
                sems[s] = [st.enter_context(nc.semaphore("s_%s_%d" % (s, i))) for i in range(tot // EPOCH + 1)]
            dsems = {s: [st.enter_context(nc.semaphore("d_%s_%d" % (s, i))) for i in range(N_DMA_SEMS)]
                     for s in STREAMS if self.cnt[(s, "d")] > 0}
            block = st.enter_context(nc.Block())

            def semv(ps, c):
                ep = (c - 1) // EPOCH
                return sems[ps][ep], c - ep * EPOCH

            def run(s, e):
                for (stream, kind, idx, fn, fd) in per[s]:
                    for (ps, pk, pi) in fd:
                        self.nwaits += 1
                        if pk == "d":
                            e.wait_ge(dsems[ps][pi % N_DMA_SEMS], 16 * (pi // N_DMA_SEMS + 1))
                        else:
                            sm, v = semv(ps, val[ps][pi])
                            e.wait_ge(sm, v)
                    if kind == "d":
                        if idx >= N_DMA_SEMS:
                            e.wait_ge(dsems[s][idx % N_DMA_SEMS], 16 * (idx // N_DMA_SEMS))
                        fn(e).then_inc(dsems[s][idx % N_DMA_SEMS], 16)
                    else:
                        ins = fn(e)
                        if needs[s][idx]:
                            sm, v = semv(s, val[s][idx])
                            ins.then_inc(sm, 1)
                n = self.cnt[(s, "d")]
                for j in range(max(0, n - N_DMA_SEMS), n):
                    e.wait_ge(dsems[s][j % N_DMA_SEMS], 16 * (j // N_DMA_SEMS + 1))

            @block.tensor
            def _(e):
                run("pe", e)

            @block.scalar
            def _(e):
                run("act", e)

            @block.vector
            def _(e):
                run("dve", e)

            @block.gpsimd
            def _(e):
                run("pool", e)

            @block.sync
            def _(e):
                run("sp", e)


class Tl:
    __slots__ = ("ap", "b")

    def __init__(self, ap, b):
        self.ap = ap
        self.b = b


class KB:
    def __init__(self, nc, S, cfg):
        self.nc = nc
        self.S = S
        self.cfg = cfg
        self.st = contextlib.ExitStack()
        self.uid = 0
        self.rr = 0

    def mm(self, out, lhsT, rhs, start, stop, reads, writes):
        self.S.op("pe", lambda e: e.matmul(out, lhsT, rhs, start=start, stop=stop, skip_group_check=True), reads, writes)

    def act(self, out, in_, func, reads, writes, bias=0.0, scale=1.0, accum_out=None):
        if accum_out is None:
            self.S.op("act", lambda e: e.activation(out, in_, func, bias=bias, scale=scale), reads, writes)
        else:
            self.S.op("act", lambda e: e.activation(out, in_, func, bias=bias, scale=scale, accum_out=accum_out), reads, writes)

    def tt(self, eng, out, in0, in1, op, reads, writes):
        self.S.op(eng, lambda e: e.tensor_tensor(out, in0, in1, op), reads, writes)

    def ts(self, eng, out, in0, s1, s2, op0, op1, reads, writes, accum_out=None):
        if accum_out is None:
            if op1 is None:
                self.S.op(eng, lambda e: e.tensor_single_scalar(out, in0, s1, op0), reads, writes)
            else:
                self.S.op(eng, lambda e: e.tensor_scalar(out, in0, s1, s2, op0, op1), reads, writes)
        else:
            self.S.op(eng, lambda e: e.tensor_scalar(out, in0, s1, s2, op0, op1, accum_out=accum_out), reads, writes)

    def stt(self, eng, out, in0, scalar, in1, op0, op1, reads, writes):
        self.S.op(eng, lambda e: e.scalar_tensor_tensor(out, in0, scalar, in1, op0, op1), reads, writes)

    def cp(self, eng, out, in_, reads, writes):
        if eng == "act":
            self.S.op("act", lambda e: e.copy(out, in_), reads, writes)
        else:
            self.S.op(eng, lambda e: e.tensor_copy(out, in_), reads, writes)

    def memset(self, eng, ap, v, writes):
        self.S.op(eng, lambda e: e.memset(ap, v), (), writes)

    def dma(self, out, in_, reads, writes, slow=False):
        if slow:
            self.S.op("sp", lambda e: e.dma_start(out=out, in_=in_, allow_slow_non_contiguous=True), reads, writes, dma=True)
        else:
            self.S.op("sp", lambda e: e.dma_start(out=out, in_=in_), reads, writes, dma=True)

    def recip(self, out, in_, reads, writes):
        self.S.op("dve", lambda e: e.reciprocal(out, in_), reads, writes)

    def sb(self, name, shape, dt):
        t = self.st.enter_context(self.nc.sbuf_tensor(name, list(shape), dt))
        return t

    def psum(self, name, shape, dt):
        return self.st.enter_context(self.nc.psum_tensor(name, list(shape), dt))

    def dram(self, name, shape, dt, kind="Internal"):
        return self.nc.dram_tensor(name, list(shape), dt, kind=kind)


class Arena:
    def __init__(self, t_f32, nwords):
        self.t = t_f32
        self.n = nwords
        self.off = 0

    def reset(self):
        self.off = 0

    def f32(self, n, name="a"):
        o = self.off
        self.off += n
        assert self.off <= self.n, ("arena overflow", name, self.off, self.n)
        return Tl(self.t[:, o:o + n], Buf(name))

    def bf(self, n, name="a"):
        w = (n + 1) // 2
        o = self.off
        self.off += w
        assert self.off <= self.n, ("arena overflow", name, self.off, self.n)
        return Tl(self.t[:, o:o + w].bitcast(BF16), Buf(name))


def build_program(cfg):
    SQ = cfg["S"]
    NSEQ = cfg["NSEQ"]
    DEPTH = cfg["DEPTH"]
    NG = SQ // 512
    NKT = SQ // 128
    nc = bass.Bass("TRN2", target_bir_lowering=False)
    S = Sched(nc)
    kb = KB(nc, S, cfg)
    n_even = (DEPTH + 1) // 2
    n_odd = DEPTH // 2

    def din(name, shape, dt=F32):
        return nc.dram_tensor(name, list(shape), dt, kind="ExternalInput")

    xT = din("xT", [NSEQ, D, SQ])
    cT = din("cT", [128, 8 * NSEQ])
    pos = din("pos", [NSEQ, SQ], I32)
    consts = din("consts", [128, 640])
    ada_w = din("ada_w", [DEPTH, 128, 8 * 9216])
    ada_b = din("ada_b", [DEPTH, 1, 9216])
    norm_gT = din("norm_gT", [DEPTH * 3, 128, 8])
    final_gT = din("final_gT", [128, 8])
    w_gate = din("w_gate", [DEPTH * 2, 128, 8 * DFF])
    w_up = din("w_up", [DEPTH * 2, 128, 8 * DFF])
    w_down = din("w_down", [DEPTH * 2, 128, NFC * D])
    NHF = 3456
    NHT = 1032
    hyb_fm = din("hyb_fm", [n_even, 128, 8 * NHF])
    hyb_tm = din("hyb_tm", [n_even, 128, 8 * NHT])
    hyb_out = din("hyb_out", [n_even, 128, 8 * D])
    fox_b = din("fox_b", [n_even, 128, 8])
    if n_odd:
        mla_down = din("mla_down", [n_odd, 128, 8 * 768])
        mla_qn = din("mla_qn", [n_odd, 128, 3])
        mla_kvn = din("mla_kvn", [n_odd, 128, 2])
        mla_uq = din("mla_uq", [n_odd, 128, 3 * 1536])
        mla_ukv = din("mla_ukv", [n_odd, 128, 2 * 2048])
        mla_out = din("mla_out", [n_odd, 128, 8 * D])
    outT = nc.dram_tensor("outT", [NSEQ, D, SQ], F32, kind="ExternalOutput")

    hT = [nc.dram_tensor("hT%d" % s, [D, SQ], F32, kind=("ExternalOutput" if cfg.get("dbg_h") else "Internal")) for s in range(NSEQ)]
    hT_b = [[Buf("hT%d_%d" % (s, g)) for g in range(NG)] for s in range(NSEQ)]
    xT_b = [Buf("xT%d" % g) for g in range(NG)]
    scr = {}

    def scratch(name, shape, dt=BF16):
        kind = "ExternalOutput" if name in cfg.get("dbg", []) else "Internal"
        scr[name] = (nc.dram_tensor("scr_" + name, list(shape), dt, kind=kind), Buf("scr_" + name))
        return scr[name]

    for h in range(8):
        scratch("QA%d" % h, [128, SQ])
        scratch("KA%d" % h, [128, SQ])
    scratch("VF", [SQ, 512])
    for c in range(4):
        scratch("DQ%d" % c, [128, SQ])
        scratch("DK%d" % c, [128, SQ])
    scratch("DV", [SQ, 512])
    for c in range(2):
        scratch("IQ%d" % c, [128, SQ])
    scratch("IK", [128, SQ])
    scratch("MIX", [D, SQ])
    if n_odd:
        for h in range(8):
            scratch("QN%d" % h, [128, SQ])
            scratch("QR%d" % h, [64, SQ])
            scratch("KN%d" % h, [128, SQ])
        scratch("KR", [64, SQ])
        scratch("MV", [SQ, 1024])
    with kb.st:
        ARENA_WORDS = 50600
        arena_t = kb.sb("arena", [128, ARENA_WORDS], F32)
        A = Arena(arena_t, ARENA_WORDS)
        cst = kb.sb("cst", [128, 640], F32)
        cst_b = Buf("cst")
        ident_bf = kb.sb("ident_bf", [128, 128], BF16)
        ones_bf = kb.sb("ones_bf", [128, 128], BF16)
        zeros_f = kb.sb("zeros_f", [128, 512], F32)
        misc_b = Buf("misc")
        condT = kb.sb("condT", [128, 8 * NSEQ], F32)
        cond_b = Buf("cond")
        modT = kb.sb("modT", [128, NSEQ * 72], F32)
        mod_b = Buf("mod")
        ngT = kb.sb("ngT", [128, DEPTH * 3 * 8 + 8], F32)
        ng_b = Buf("ng")
        ABG = kb.sb("ABG", [128, NSEQ * 3 * 24], F32)
        abg_b = Buf("abg")
        stat = kb.sb("stat", [128, 64], F32)
        stat_b = Buf("stat")
        negB = kb.sb("negB", [128, 16], F32)
        negB_b = Buf("negB")
        iw_all = kb.sb("iw_all", [128, NKT * 4], F32)
        iw_b = Buf("iw")
        iwabs = kb.sb("iwabs", [128, NKT * 4], F32)
        iwsgn = kb.sb("iwsgn", [128, NKT * 4], F32)
        smallf = kb.sb("smallf", [128, 64], F32)
        fb_t = kb.sb("fb_t", [128, 8], F32)
        fb_b = Buf("fb")
        qn_t = kb.sb("qn_t", [128, 8], F32)
        qn_b = Buf("qn")
        PS = [kb.psum("ps%d" % i, [128, 512], F32) for i in range(8)]
        PB = [Buf("ps%d" % i, excl=True) for i in range(8)]

        kb.dma(cst[:], consts[:, :], [], [cst_b])
        IOTA = cst[:, 0:512]
        kb.memset("pool", zeros_f[:], 0.0, [misc_b])
        kb.memset("pool", ones_bf[:], 1.0, [misc_b])
        kb.ts("dve", ident_bf[:], cst[:, 0:128], cst[:, 512:513], None, ALU.is_equal, None, [cst_b], [misc_b])

        def build_mask(kind):
            m = A.bf(4 * 512, "mask%d" % kind)
            for a in range(4):
                if kind == 0:
                    kb.ts("dve", m.ap[:, a * 512:(a + 1) * 512], IOTA, cst[:, 513 + a:514 + a], NEGM, ALU.is_lt, ALU.mult, [cst_b], [m.b])
                elif kind == 1:
                    kb.ts("dve", m.ap[:, a * 512:(a + 1) * 512], IOTA, cst[:, 517 + a:518 + a], NEGM, ALU.is_lt, ALU.mult, [cst_b], [m.b])
                else:
                    kb.ts("dve", m.ap[:, a * 512:(a + 1) * 512], IOTA, cst[:, 521 + a:522 + a], -1e30, ALU.is_ge, ALU.mult, [cst_b], [m.b])
            return m
        kb.dma(condT[:], cT[:, :], [], [cond_b])
        kb.act(condT[:], condT[:], AF.Silu, [cond_b], [cond_b])
        for i in range(DEPTH * 3):
            kb.dma(ngT[:, i * 8:(i + 1) * 8], norm_gT[i, :, :], [], [ng_b])
        kb.dma(ngT[:, DEPTH * 24:DEPTH * 24 + 8], final_gT[:, :], [], [ng_b])

        def load_w_bf(dst_bf_ap, src_dram_ap, ncols, name):
            CH = 2048
            nst = 3
            stg = [A.f32(CH, "stg%d" % i) for i in range(nst)]
            dstb = Buf(name)
            i = 0
            engs = ("pool", "act", "dve")
            for o in range(0, ncols, CH):
                n = min(CH, ncols - o)
                s_ = stg[i % nst]
                kb.dma(s_.ap[:, 0:n], src_dram_ap[:, o:o + n], [], [s_.b])
                kb.cp(("dve", "act", "dve", "act", "pool")[i % 5], dst_bf_ap[:, o:o + n], s_.ap[:, 0:n], [s_.b], [dstb])
                i += 1
            S.fence()
            return dstb

        def compute_mod(layer):
            A.reset()
            PIECE = 512
            wst = [A.f32(8 * PIECE, "adaw%d" % i) for i in range(2)]
            bst = [A.f32(PIECE, "adab%d" % i) for i in range(2)]
            onesf = A.f32(8, "onesf")
            kb.memset("pool", onesf.ap[0:1, 0:NSEQ], 1.0, [onesf.b])
            npiece = 9216 // PIECE
            for pi in range(npiece):
                w_ = wst[pi % 2]
                b_ = bst[pi % 2]
                src = ada_w[layer].rearrange("p (k n) -> p k n", k=8)[:, :, pi * PIECE:(pi + 1) * PIECE]
                kb.dma(w_.ap.rearrange("p (k n) -> p k n", k=8), src, [], [w_.b])
                kb.dma(b_.ap[0:1, :], ada_b[layer, :, pi * PIECE:(pi + 1) * PIECE], [], [b_.b])
                for cc in range(PIECE // 128):
                    col = pi * (PIECE // 128) + cc
                    for k in range(8):
                        kb.mm(PS[0][:, col * NSEQ:(col + 1) * NSEQ], w_.ap[:, k * PIECE + cc * 128:k * PIECE + (cc + 1) * 128],
                              condT[:, k * NSEQ:(k + 1) * NSEQ], k == 0, False, [w_.b, cond_b], [PB[0]])
                    kb.mm(PS[0][:, col * NSEQ:(col + 1) * NSEQ], b_.ap[0:1, cc * 128:(cc + 1) * 128],
                          onesf.ap[0:1, 0:NSEQ], False, True, [b_.b, onesf.b], [PB[0]])
            for s in range(NSEQ):
                src = PS[0][:, 0:72 * NSEQ].rearrange("p (c s) -> p c s", s=NSEQ)[:, :, s]
                kb.cp("dve", modT[:, s * 72:(s + 1) * 72], src, [PB[0]], [mod_b])

        def compute_abg(layer):
            for s in range(NSEQ):
                for sub in range(3):
                    o = (s * 3 + sub) * 24
                    m = s * 72 + sub * 24
                    g = ngT[:, (layer * 3 + sub) * 8:(layer * 3 + sub) * 8 + 8]
                    kb.ts("dve", ABG[:, o:o + 8], modT[:, m + 8:m + 16], 1.0, None, ALU.add, None, [mod_b], [abg_b])
                    kb.tt("dve", ABG[:, o:o + 8], ABG[:, o:o + 8], g, ALU.mult, [abg_b, ng_b], [abg_b])
                    kb.ts("dve", ABG[:, o:o + 8], ABG[:, o:o + 8], 32.0, None, ALU.mult, None, [abg_b], [abg_b])
                    kb.cp("dve", ABG[:, o + 8:o + 16], modT[:, m:m + 8], [mod_b], [abg_b])
                    gs = 1.0 if sub == 1 else 0.5
                    kb.ts("dve", ABG[:, o + 16:o + 24], modT[:, m + 16:m + 24], gs, None, ALU.mult, None, [mod_b], [abg_b])

        def h_src(layer, sub, s):
            if layer == 0 and sub == 0:
                return xT[s], xT_b
            return hT[s][:, :], hT_b[s]

        def norm_group(hsrc, hsrc_b, s, sub, g, hg, uT, extra_scale=None, final=False, layer=0):
            sq = A_sq
            rs = A_rs
            tmp = A_tmp
            src = hsrc.rearrange("(c p) t -> p c t", p=128)[:, :, g * 512:(g + 1) * 512]
            kb.dma(hg.ap.rearrange("p (c t) -> p c t", c=8), src, [hsrc_b[g]], [hg.b])
            for c in range(8):
                kb.act(sq.ap[:, c * 512:(c + 1) * 512], hg.ap[:, c * 512:(c + 1) * 512], AF.Square, [hg.b], [sq.b])
            for c in range(8):
                kb.mm(PS[7][:, :], ones_bf[:, :], sq.ap[:, c * 512:(c + 1) * 512], c == 0, c == 7, [sq.b, misc_b], [PB[7]])
            kb.act(rs.ap, PS[7][:, :], AF.Sqrt, [PB[7]], [rs.b], bias=epsb[:, 0:1], scale=1.0)
            kb.recip(rs.ap, rs.ap, [rs.b], [rs.b])
            o = (s * 3 + sub) * 24
            for c in range(8):
                t_ = tmp[c % 2]
                if final:
                    kb.stt("dve", t_.ap, hg.ap[:, c * 512:(c + 1) * 512], fg32[:, c:c + 1], rs.ap, ALU.mult, ALU.mult,
                           [hg.b, rs.b, abg_b], [t_.b])
                    kb.cp("act", uT.ap[:, c * 512:(c + 1) * 512], t_.ap, [t_.b], [uT.b])
                else:
                    kb.stt("dve", t_.ap, hg.ap[:, c * 512:(c + 1) * 512], ABG[:, o + c:o + c + 1], rs.ap, ALU.mult, ALU.mult,
                           [hg.b, rs.b, abg_b], [t_.b])
                    kb.act(uT.ap[:, c * 512:(c + 1) * 512], t_.ap, AF.Identity, [t_.b, abg_b], [uT.b],
                           bias=ABG[:, o + 8 + c:o + 9 + c], scale=1.0)

        epsb = smallf[:, 0:1]
        kb.memset("pool", smallf[:, 0:1], float(D) * 1e-6, [misc_b])
        fg32 = smallf[:, 8:16]
        kb.ts("dve", fg32, ngT[:, DEPTH * 24:DEPTH * 24 + 8], 32.0, None, ALU.mult, None, [ng_b], [abg_b])

        def ffn_phase(layer, which):
            global A_sq, A_rs, A_tmp
            S.fence()
            A.reset()
            sub = 0 if which == 0 else 2
            wi = layer * 2 + which
            wg = A.bf(8 * DFF, "wg")
            wu = A.bf(8 * DFF, "wu")
            wd = A.bf(NFC * D, "wd")
            mark = A.off
            wg.b = load_w_bf(wg.ap, w_gate[wi], 8 * DFF, "wg")
            A.off = mark
            wu.b = load_w_bf(wu.ap, w_up[wi], 8 * DFF, "wu")
            A.off = mark
            wd.b = load_w_bf(wd.ap, w_down[wi], NFC * D, "wd")
            A.off = mark
            S.fence()
            hgs = [A.f32(8 * 512, "hg%d" % i) for i in range(1)]
            uTs = [A.bf(8 * 512, "uT%d" % i) for i in range(1)]
            actT = A.bf(NFC * 512, "actT")
            A_sq = Tl(uTs[0].ap, uTs[0].b)
            A_rs = A.f32(512, "rs")
            A_tmp = [A.f32(512, "tmp%d" % i) for i in range(2)]
            sg = [A.bf(512, "sg%d" % i) for i in range(2)]
            it = 0
            for s in range(NSEQ):
                hsrc, hsrc_b = h_src(layer, sub, s)
                o = (s * 3 + sub) * 24
                for g in range(NG):
                    hg = hgs[0]
                    uT = uTs[0]
                    norm_group(hsrc, hsrc_b, s, sub, g, hg, uT)
                    if cfg.get("dbg_ffn") and s == 0 and g == 0 and layer == 0 and which == 0:
                        du = nc.dram_tensor("dbg_uT", [128, 4096], BF16, kind="ExternalOutput")
                        kb.S.op("sp", (lambda du=du, uT=uT: lambda e: e.dma_start(out=du[:, :], in_=uT.ap))(), [uT.b], [], dma=True)
                    for f in range(NFC):
                        pg = 0 + (f % 2) * 2
                        pu = 1 + (f % 2) * 2
                        for k in range(8):
                            kb.mm(PS[pg][:, :], wg.ap[:, k * DFF + f * 128:k * DFF + (f + 1) * 128], uT.ap[:, k * 512:(k + 1) * 512],
                                  k == 0, k == 7, [wg.b, uT.b], [PB[pg]])
                        for k in range(8):
                            kb.mm(PS[pu][:, :], wu.ap[:, k * DFF + f * 128:k * DFF + (f + 1) * 128], uT.ap[:, k * 512:(k + 1) * 512],
                                  k == 0, k == 7, [wu.b, uT.b], [PB[pu]])
                        sg_ = sg[f % 2]
                        kb.act(sg_.ap, PS[pg][:, :], AF.Silu, [PB[pg]], [sg_.b])
                        kb.tt("dve", actT.ap[:, f * 512:(f + 1) * 512], sg_.ap, PS[pu][:, :], ALU.mult, [sg_.b, PB[pu]], [actT.b])
                    if cfg.get("dbg_ffn") and s == 0 and g == 0 and layer == 0 and which == 0:
                        da = nc.dram_tensor("dbg_actT", [128, NFC * 512], BF16, kind="ExternalOutput")
                        kb.S.op("sp", (lambda da=da, actT=actT: lambda e: e.dma_start(out=da[:, :], in_=actT.ap))(), [actT.b], [], dma=True)
                    for c in range(8):
                        pd = 4 + (c % 2)
                        for f in range(NFC):
                            kb.mm(PS[pd][:, :], wd.ap[:, f * D + c * 128:f * D + (c + 1) * 128], actT.ap[:, f * 512:(f + 1) * 512],
                                  f == 0, f == NFC - 1, [wd.b, actT.b], [PB[pd]])
                        kb.stt("dve", hg.ap[:, c * 512:(c + 1) * 512], PS[pd][:, :], ABG[:, o + 16 + c:o + 17 + c], hg.ap[:, c * 512:(c + 1) * 512],
                               ALU.mult, ALU.add, [PB[pd], abg_b, hg.b], [hg.b])
                        kb.dma(hT[s][c * 128:(c + 1) * 128, g * 512:(g + 1) * 512], hg.ap[:, c * 512:(c + 1) * 512], [hg.b], [hT_b[s][g]])

        def outproj_phase(layer, w_dram, s):
            S.fence()
            A.reset()
            wo = A.bf(8 * D, "wo")
            mark = A.off
            wo.b = load_w_bf(wo.ap, w_dram, 8 * D, "wo")
            A.off = mark
            S.fence()
            mx = [A.bf(8 * 512, "mx%d" % i) for i in range(2)]
            hg = [A.f32(8 * 512, "hgo%d" % i) for i in range(2)]
            ho = [A.f32(512, "hoo%d" % i) for i in range(2)]
            o = (s * 3 + 1) * 24
            mixd, mixb = scr["MIX"]
            for g in range(NG):
                m_ = mx[g % 2]
                h_ = hg[g % 2]
                kb.dma(m_.ap.rearrange("p (c t) -> p c t", c=8),
                       mixd.rearrange("(c p) t -> p c t", p=128)[:, :, g * 512:(g + 1) * 512], [mixb], [m_.b])
                kb.dma(h_.ap.rearrange("p (c t) -> p c t", c=8),
                       hT[s].rearrange("(c p) t -> p c t", p=128)[:, :, g * 512:(g + 1) * 512], [hT_b[s][g]], [h_.b])
                for c in range(8):
                    pd = c % 2
                    for k in range(8):
                        kb.mm(PS[pd][:, :], wo.ap[:, k * D + c * 128:k * D + (c + 1) * 128], m_.ap[:, k * 512:(k + 1) * 512],
                              k == 0, k == 7, [wo.b, m_.b], [PB[pd]])
                    ho_ = ho[c % 2]
                    kb.stt("dve", ho_.ap, PS[pd][:, :], ABG[:, o + 16 + c:o + 17 + c], h_.ap[:, c * 512:(c + 1) * 512],
                           ALU.mult, ALU.add, [PB[pd], abg_b, h_.b], [ho_.b])
                    kb.dma(hT[s][c * 128:(c + 1) * 128, g * 512:(g + 1) * 512], ho_.ap, [ho_.b], [hT_b[s][g]])

        def rope_tables(s, col_scale, cosT, sinT):
            posi = A.f32(SQ, "posi")
            pi_ap = posi.ap.bitcast(I32)
            kb.dma(pi_ap, pos[s:s + 1, :].partition_broadcast(128), [], [posi.b])
            invf = smallf[:, 20:21]
            kb.act(invf, cst[:, col_scale:col_scale + 1], AF.Exp, [cst_b], [misc_b])
            ang = cosT
            kb.cp("dve", ang.ap, pi_ap, [posi.b], [ang.b])
            kb.ts("dve", ang.ap, ang.ap, invf, None, ALU.mult, None, [ang.b, misc_b], [ang.b])
            C1 = 6.28125
            C2 = float(2.0 * np.pi - 6.28125)
            ki = A.f32(SQ, "ki")
            kf = A.f32(SQ, "kf")

            def reduce_to(dst, shift):
                kb.ts("dve", kf.ap, ang.ap, shift, float(1.0 / (2.0 * np.pi)), ALU.add, ALU.mult, [ang.b], [kf.b])
                kb.cp("dve", ki.ap.bitcast(I32), kf.ap, [kf.b], [ki.b])
                kb.cp("dve", kf.ap, ki.ap.bitcast(I32), [ki.b], [kf.b])
                kb.stt("dve", dst.ap, kf.ap, -C1, ang.ap, ALU.mult, ALU.add, [kf.b, ang.b], [dst.b])
                kb.stt("dve", dst.ap, kf.ap, -C2, dst.ap, ALU.mult, ALU.add, [kf.b, dst.b], [dst.b])
                if shift != 0.0:
                    kb.ts("dve", dst.ap, dst.ap, shift, None, ALU.add, None, [dst.b], [dst.b])
                kb.ts("dve", kf.ap, dst.ap, float(np.pi), float(-2.0 * np.pi), ALU.is_gt, ALU.mult, [dst.b], [kf.b])
                kb.tt("dve", dst.ap, dst.ap, kf.ap, ALU.add, [dst.b, kf.b], [dst.b])
                kb.ts("dve", kf.ap, dst.ap, float(-np.pi), float(2.0 * np.pi), ALU.is_lt, ALU.mult, [dst.b], [kf.b])
                kb.tt("dve", dst.ap, dst.ap, kf.ap, ALU.add, [dst.b, kf.b], [dst.b])
                kb.ts("dve", dst.ap, dst.ap, float(np.pi), -float(np.pi), ALU.min, ALU.max, [dst.b], [dst.b])

            reduce_to(sinT, 0.0)
            tmpc = A.f32(SQ, "tmpc")
            reduce_to(tmpc, float(0.5 * np.pi))
            kb.cp("dve", cosT.ap, tmpc.ap, [tmpc.b], [cosT.b])
            kb.act(sinT.ap, sinT.ap, AF.Sin, [sinT.b], [sinT.b])
            kb.act(cosT.ap, cosT.ap, AF.Sin, [cosT.b], [cosT.b])

        def rope_block(y_ps, pb, b0, half, g, cosT, sinT, dst, t1, t2, eng="dve"):
            tk = slice(g * 512, (g + 1) * 512)
            r1 = slice(b0, b0 + half)
            r2 = slice(b0 + 32, b0 + 32 + half)
            kb.tt(eng, t1.ap[r1, :], y_ps[r2, :], sinT.ap[r2, tk], ALU.mult, [pb, sinT.b], [t1.b])
            kb.tt(eng, t2.ap[r2, :], y_ps[r1, :], sinT.ap[r1, tk], ALU.mult, [pb, sinT.b], [t2.b])
            kb.tt(eng, t2.ap[r1, :], y_ps[r1, :], cosT.ap[r1, tk], ALU.mult, [pb, cosT.b], [t2.b])
            kb.tt(eng, t1.ap[r2, :], y_ps[r2, :], cosT.ap[r2, tk], ALU.mult, [pb, cosT.b], [t1.b])
            kb.tt(eng, dst[r1, :], t2.ap[r1, :], t1.ap[r1, :], ALU.subtract, [t1.b, t2.b], [dstb_cur[0]])
            kb.tt(eng, dst[r2, :], t1.ap[r2, :], t2.ap[r2, :], ALU.add, [t1.b, t2.b], [dstb_cur[0]])

        dstb_cur = [None]

        def bound_update(sq_ap, sq_b, rows, col):
            kb.mm(PS[6][:, :], ones_bf[rows, :], sq_ap[rows, :], True, True, [sq_b, misc_b], [PB[6]])
            kb.S.op("dve", lambda e: e.tensor_reduce(smallf[:, 32:33], PS[6][:, :], AX.X, ALU.max), [PB[6]], [misc_b])
            kb.tt("dve", stat[:, col:col + 1], stat[:, col:col + 1], smallf[:, 32:33], ALU.max, [misc_b, stat_b], [stat_b])

        def finish_bounds(nh, scale):
            kb.tt("dve", negB[:, 0:nh], stat[:, 0:nh], stat[:, 16:16 + nh], ALU.mult, [stat_b], [negB_b])
            kb.act(negB[:, 0:nh], negB[:, 0:nh], AF.Sqrt, [negB_b], [negB_b], scale=scale * scale)
            kb.ts("dve", negB[:, 0:nh], negB[:, 0:nh], -1.0, None, ALU.mult, None, [negB_b], [negB_b])

        def hyb_proj_phase(layer, s):
            global A_sq, A_rs, A_tmp
            j = layer // 2
            S.fence()
            A.reset()
            wf = A.bf(8 * NHF, "wf")
            wt = A.bf(8 * NHT, "wt")
            mark = A.off
            wf.b = load_w_bf(wf.ap, hyb_fm[j], 8 * NHF, "wf")
            A.off = mark
            wt.b = load_w_bf(wt.ap, hyb_tm[j], 8 * NHT, "wt")
            A.off = mark
            kb.dma(fb_t[:], fox_b[j, :, :], [], [fb_b])
            kb.ts("dve", fb_t[:], fb_t[:], -1.0, None, ALU.mult, None, [fb_b], [fb_b])
            S.fence()
            cos16 = A.f32(SQ, "cos16")
            sin16 = A.f32(SQ, "sin16")
            mk2 = A.off
            rope_tables(s, 525, cos16, sin16)
            A.off = mk2
            S.fence()
            hg = A.f32(8 * 512, "hg")
            uT = A.bf(8 * 512, "uT")
            A_sq = A.bf(8 * 512, "sq")
            A_rs = A.f32(512, "rs")
            A_tmp = [A.f32(512, "tmp%d" % i) for i in range(2)]
            qst = [A.bf(512, "qst%d" % i) for i in range(2)]
            kst = [A.bf(512, "kst%d" % i) for i in range(2)]
            gst = [A.bf(512, "gst%d" % i) for i in range(3)]
            sqs = [A.bf(512, "sqs%d" % i) for i in range(2)]
            t1 = A.f32(512, "t1")
            t2 = A.f32(512, "t2")
            ge = A.f32(512, "ge")
            gc = A.f32(512, "gc")
            gh = A.f32(512, "gh")
            ghb = A.bf(512, "ghb")
            vst = [A.bf(512, "vst%d" % i) for i in range(2)]
            carry = smallf[:, 34:42]
            kb.memset("pool", carry, 0.0, [misc_b])
            kb.memset("pool", stat[:, 0:32], 0.0, [stat_b])
            for q_ in qst:
                kb.memset("pool", q_.ap[:, :], 0.0, [q_.b])
                kb.memset("pool", q_.ap[96:98, :], 1.0, [q_.b])
            for k_ in kst:
                kb.memset("pool", k_.ap[:, :], 0.0, [k_.b])
                kb.memset("pool", k_.ap[64:66, :], 1.0, [k_.b])
            hsrc, hsrc_b = hT[s][:, :], hT_b[s]
            it = 0
            for g in range(NG):
                tk = slice(g * 512, (g + 1) * 512)
                norm_group(hsrc, hsrc_b, s, 1, g, hg, uT)

                def proj_fm(chunk, pbank):
                    for k in range(8):
                        kb.mm(PS[pbank][:, :], wf.ap[:, k * NHF + chunk * 128:k * NHF + (chunk + 1) * 128],
                              uT.ap[:, k * 512:(k + 1) * 512], k == 0, k == 7, [wf.b, uT.b], [PB[pbank]])

                for h in range(8):
                    pq = (h % 2) * 2
                    pk = (h % 2) * 2 + 1
                    proj_fm(h, pq)
                    proj_fm(8 + h, pk)
                    q_ = qst[h % 2]
                    k_ = kst[h % 2]
                    sq_ = sqs[h % 2]
                    kb.cp("act", q_.ap[0:64, :], PS[pq][0:64, :], [PB[pq]], [q_.b])
                    kb.act(sq_.ap[0:64, :], PS[pq][0:64, :], AF.Square, [PB[pq]], [sq_.b])
                    bound_update(sq_.ap, sq_.b, slice(0, 64), h)
                    kb.cp("act", k_.ap[0:64, :], PS[pk][0:64, :], [PB[pk]], [k_.b])
                    kb.act(sq_.ap[64:128, :], PS[pk][0:64, :], AF.Square, [PB[pk]], [sq_.b])
                    bound_update(sq_.ap, sq_.b, slice(64, 128), 16 + h)
                    R = slice(64, 66)
                    kb.act(ge.ap[R, :], PS[pq][R, :], AF.Exp, [PB[pq], fb_b], [ge.b], bias=fb_t[R, h:h + 1], scale=-1.0)
                    kb.act(ge.ap[R, :], ge.ap[R, :], AF.Ln, [ge.b], [ge.b], bias=1.0, scale=1.0)
                    kb.ts("dve", ge.ap[R, :], ge.ap[R, :], -8.0, None, ALU.mult, None, [ge.b], [ge.b])
                    kb.S.op("dve", (lambda R=R, h=h: lambda e: e.tensor_tensor_scan(gc.ap[R, :], ge.ap[R, :], zeros_f[R, :], carry[R, h:h + 1], ALU.add, ALU.add))(),
                            [ge.b, misc_b], [gc.b])
                    kb.cp("dve", carry[R, h:h + 1], gc.ap[R, 511:512], [gc.b], [misc_b])
                    kb.cp("dve", ghb.ap[R, :], gc.ap[R, :], [gc.b], [ghb.b])
                    kb.cp("dve", gh.ap[R, :], ghb.ap[R, :], [ghb.b], [gh.b])
                    kb.stt("dve", gh.ap[R, :], gh.ap[R, :], cst[R, 527:528], gc.ap[R, :], ALU.mult, ALU.add, [gh.b, gc.b, cst_b], [gh.b])
                    kb.cp("dve", q_.ap[R, :], gh.ap[R, :], [gh.b], [q_.b])
                    kb.S.op("act", (lambda q_=q_, k_=k_: lambda e: e.mul(k_.ap[96:98, :], q_.ap[64:66, :], -1.0))(), [q_.b], [k_.b])
                    kb.dma(scr["QA%d" % h][0][:, tk], q_.ap, [q_.b], [scr["QA%d" % h][1]])
                    kb.dma(scr["KA%d" % h][0][:, tk], k_.ap, [k_.b], [scr["KA%d" % h][1]])
                fm_list = [("DQ%d" % c, 16 + c, 32 + 2 * c) for c in range(4)] + [("DK%d" % c, 20 + c, 48 + 2 * c) for c in range(4)] + \
                          [("IQ%d" % c, 24 + c, None) for c in range(2)] + [("IK", 26, None)]
                for n_i, (nm, chunk, scol) in enumerate(fm_list):
                    pbk = 4 + (n_i % 2)
                    proj_fm(chunk, pbk)
                    d_ = gst[n_i % 3]
                    kb.cp("act", d_.ap[:, :], PS[pbk][:, :], [PB[pbk]], [d_.b])
                    dstb_cur[0] = d_.b
                    for b0 in (0, 64):
                        rope_block(PS[pbk], PB[pbk], b0, 8, g, cos16, sin16, d_.ap, t1, t2)
                    if scol is not None:
                        sq_ = sqs[n_i % 2]
                        kb.act(sq_.ap[:, :], d_.ap[:, :], AF.Square, [d_.b], [sq_.b])
                        hb = (chunk - 16) * 2 if chunk < 20 else (chunk - 20) * 2
                        base = 8 if chunk < 20 else 24
                        bound_update(sq_.ap, sq_.b, slice(0, 64), base + hb)
                        bound_update(sq_.ap, sq_.b, slice(64, 128), base + hb + 1)
                    kb.dma(scr[nm][0][:, tk], d_.ap, [d_.b], [scr[nm][1]])
                for tt_ in range(4):
                    tok = slice(tt_ * 128, (tt_ + 1) * 128)
                    gt = g * 4 + tt_
                    for vi, nm in enumerate(("VF", "DV")):
                        pbk = 4 + vi
                        for k in range(8):
                            kb.mm(PS[pbk][:, :], uT.ap[:, k * 512 + tt_ * 128:k * 512 + (tt_ + 1) * 128],
                                  wt.ap[:, k * NHT + vi * 512:k * NHT + (vi + 1) * 512], k == 0, k == 7, [wt.b, uT.b], [PB[pbk]])
                        v_ = vst[vi]
                        kb.cp("act", v_.ap[:, :], PS[pbk][:, :], [PB[pbk]], [v_.b])
                        kb.dma(scr[nm][0][gt * 128:(gt + 1) * 128, :], v_.ap, [v_.b], [scr[nm][1]])
                    for k in range(8):
                        kb.mm(PS[0][:, 0:4], uT.ap[:, k * 512 + tt_ * 128:k * 512 + (tt_ + 1) * 128],
                              wt.ap[:, k * NHT + 1024:k * NHT + 1028], k == 0, k == 7, [wt.b, uT.b], [PB[0]])
                    kb.ts("dve", iw_all[:, gt * 4:(gt + 1) * 4], PS[0][:, 0:4], 1.0 / 16.0, None, ALU.mult, None, [PB[0]], [iw_b])
            finish_bounds(16, 0.125)
            kb.act(iwabs[:, :], iw_all[:, :], AF.Abs, [iw_b], [iw_b])
            kb.act(iwsgn[:, :], iw_all[:, :], AF.Sign, [iw_b], [iw_b])

        def attn_head(j_blocks, Kt, Kb_, Qt, Qb_, K2, Q2, Vfn, mask_tiles, mask_b, dsa_masks, scale, negcol, dv, out_rows, PT, ost, orec):
            mixd, mixb = scr["MIX"]
            for jb in j_blocks:
                qs = slice(jb * 512, (jb + 1) * 512)
                nkt = 4 * jb + 4
                po = 3 + (jb % 2)
                pd = 5 + (jb % 2)
                pend = None
                for kt in range(nkt + 1):
                    if kt < nkt:
                        ps_ = kt % 3
                        ks = slice(kt * 128, (kt + 1) * 128)
                        diag = kt >= 4 * jb
                        last_plain = (K2 is None) and not (diag and mask_tiles is not None) and dsa_masks is None
                        kb.mm(PS[ps_][:, :], Kt[:, ks], Qt[:, qs], True, last_plain, [Kb_, Qb_], [PB[ps_]])
                        if K2 is not None:
                            kb.mm(PS[ps_][:, :], K2[:, ks], Q2[:, qs], False, not (diag and mask_tiles is not None), [Kb_, Qb_], [PB[ps_]])
                        if diag and mask_tiles is not None:
                            a = kt - 4 * jb
                            kb.mm(PS[ps_][:, :], ident_bf[:, :], mask_tiles[:, a * 512:(a + 1) * 512], False, True, [misc_b, mask_b], [PB[ps_]])
                        if dsa_masks is not None:
                            for a in range(4):
                                mb_ = dsa_masks[a]
                                kb.mm(PS[ps_][:, a * 128:(a + 1) * 128], mb_.ap[:, ks], ident_bf[:, :], False, a == 3, [mb_.b, misc_b], [PB[ps_]])
                        pt_ = PT[kt % 3]
                        kb.act(pt_.ap, PS[ps_][:, :], AF.Exp, [PB[ps_], negB_b], [pt_.b], bias=negB[:, negcol:negcol + 1], scale=scale)
                    if pend is not None:
                        pk_, ppt = pend
                        kb.mm(PS[po][0:dv, :], Vfn(pk_), ppt.ap, pk_ == 0, pk_ == nkt - 1, [ppt.b, Vb_cur[0]], [PB[po]])
                        kb.mm(PS[pd][0:dv, :], ones_bf[:, 0:dv], ppt.ap, pk_ == 0, pk_ == nkt - 1, [ppt.b, misc_b], [PB[pd]])
                    pend = (kt, PT[kt % 3]) if kt < nkt else None
                rc = orec[jb % 2]
                os_ = ost[jb % 2]
                kb.recip(rc.ap[0:dv, :], PS[pd][0:dv, :], [PB[pd]], [rc.b])
                kb.tt("dve", os_.ap[0:dv, :], PS[po][0:dv, :], rc.ap[0:dv, :], ALU.mult, [PB[po], rc.b], [os_.b])
                kb.dma(mixd[out_rows, qs], os_.ap[0:dv, :], [os_.b], [mixb])

        Vb_cur = [None]

        def fox_attn_phase(s):
            S.fence()
            A.reset()
            vall = A.bf(NKT * 512, "vall")
            vd, vb = scr["VF"]
            kb.dma(vall.ap.rearrange("p (t d) -> p t d", d=512), vd.rearrange("(t p) d -> p t d", p=128), [vb], [vall.b])
            Vb_cur[0] = vall.b
            cm = build_mask(0)
            KA = [A.bf(SQ, "KA%d" % i) for i in range(2)]
            QA = [A.bf(SQ, "QA%d" % i) for i in range(2)]
            PT = [A.bf(512, "PT%d" % i) for i in range(3)]
            ost = [A.bf(512, "ost%d" % i) for i in range(2)]
            orec = [A.f32(512, "orec%d" % i) for i in range(2)]
            for h in range(8):
                k_ = KA[h % 2]
                q_ = QA[h % 2]
                kb.dma(k_.ap, scr["KA%d" % h][0][:, :], [scr["KA%d" % h][1]], [k_.b])
                kb.dma(q_.ap, scr["QA%d" % h][0][:, :], [scr["QA%d" % h][1]], [q_.b])
                vv = vall.ap.rearrange("p (t d) -> p t d", d=512)
                attn_head(range(NG), k_.ap, k_.b, q_.ap, q_.b, None, None,
                          (lambda h: lambda kt: vv[:, kt, h * 64:(h + 1) * 64])(h),
                          cm.ap, cm.b, None, 0.125, h, 64, slice(h * 64, (h + 1) * 64), PT, ost, orec)

        def dsa_phase(s):
            S.fence()
            A.reset()
            vall = A.bf(NKT * 512, "dvall")
            vd, vb = scr["DV"]
            kb.dma(vall.ap.rearrange("p (t d) -> p t d", d=512), vd.rearrange("(t p) d -> p t d", p=128), [vb], [vall.b])
            Vb_cur[0] = vall.b
            vv = vall.ap.rearrange("p (t d) -> p t d", d=512)
            DKs = []
            for c in range(4):
                t_ = A.bf(SQ, "DK%d" % c)
                kb.dma(t_.ap, scr["DK%d" % c][0][:, :], [scr["DK%d" % c][1]], [t_.b])
                DKs.append(t_)
            IK = A.bf(SQ, "IK")
            kb.dma(IK.ap, scr["IK"][0][:, :], [scr["IK"][1]], [IK.b])
            DQb = [[A.bf(512, "DQ%d_%d" % (c, i)) for c in range(4)] for i in range(2)]
            IQb = [[A.bf(512, "IQ%d_%d" % (c, i)) for c in range(2)] for i in range(2)]
            Sc = A.f32(SQ, "Sc")
            ccmq = build_mask(2)
            junk = A.bf(SQ, "junk")
            maskb = [[A.bf(SQ, "maskb%d_%d" % (i, a)) for a in range(4)] for i in range(2)]
            rr_ = [A.f32(512, "rr%d" % i) for i in range(2)]
            PT = [A.bf(512, "PT%d" % i) for i in range(3)]
            ost = [A.bf(512, "ost%d" % i) for i in range(2)]
            orec = [A.f32(512, "orec%d" % i) for i in range(2)]
            osb = [A.f32(512, "osb%d" % i) for i in range(2)]
            bs = smallf[:, 44:60]
            IBANK = (4, 6, 7)
            ictr = [0]

            def load_block(jb):
                qs = slice(jb * 512, (jb + 1) * 512)
                for c in range(4):
                    kb.dma(DQb[jb % 2][c].ap, scr["DQ%d" % c][0][:, qs], [scr["DQ%d" % c][1]], [DQb[jb % 2][c].b])
                for c in range(2):
                    kb.dma(IQb[jb % 2][c].ap, scr["IQ%d" % c][0][:, qs], [scr["IQ%d" % c][1]], [IQb[jb % 2][c].b])

            def idx_tile(jb, a):
                iq = IQb[jb % 2]
                nk = 512 * (jb + 1)
                qt = jb * 4 + a
                mb_out = maskb[jb % 2][a]
                for kbk in range(jb + 1):
                    kk = slice(kbk * 512, (kbk + 1) * 512)
                    for ih in range(4):
                        rows = slice((ih % 2) * 64, (ih % 2) * 64 + 64)
                        bank = IBANK[ictr[0] % 3]
                        ictr[0] += 1
                        kb.mm(PS[bank][:, :], iq[ih // 2].ap[rows, a * 128:(a + 1) * 128], IK.ap[rows, kk], True, True,
                              [iq[ih // 2].b, IK.b], [PB[bank]])
                        r_ = rr_[ih % 2]
                        kb.act(r_.ap, PS[bank][:, :], AF.Relu, [PB[bank], iw_b], [r_.b], scale=iwabs[:, qt * 4 + ih:qt * 4 + ih + 1])
                        if ih == 0:
                            kb.ts("dve", Sc.ap[:, kk], r_.ap, iwsgn[:, qt * 4:qt * 4 + 1], None, ALU.mult, None, [r_.b, iw_b], [Sc.b])
                        else:
                            kb.stt("dve", Sc.ap[:, kk], r_.ap, iwsgn[:, qt * 4 + ih:qt * 4 + ih + 1], Sc.ap[:, kk], ALU.mult, ALU.add,
                                   [r_.b, iw_b, Sc.b], [Sc.b])
                lo = bs[:, 0:1]
                hi = bs[:, 1:2]
                mid = bs[:, 2:3]
                cnt = bs[:, 3:4]
                cond = bs[:, 4:5]
                dd = bs[:, 5:6]
                bsb = Buf("bs")
                if qt >= 2:
                    kb.S.op("dve", (lambda nk=nk: lambda e: e.tensor_reduce(hi, Sc.ap[:, 0:nk], AX.X, ALU.max))(), [Sc.b], [bsb])
                    kb.S.op("dve", (lambda nk=nk: lambda e: e.tensor_reduce(lo, Sc.ap[:, 0:nk], AX.X, ALU.min))(), [Sc.b], [bsb])
                    kb.tt("dve", dd, hi, lo, ALU.subtract, [bsb], [bsb])
                    kb.stt("dve", hi, dd, 0.001, hi, ALU.mult, ALU.add, [bsb], [bsb])
                    kb.ts("dve", hi, hi, 1e-6, None, ALU.add, None, [bsb], [bsb])
                dk_ = slice(jb * 512, (jb + 1) * 512)
                kb.tt("dve", Sc.ap[:, dk_], Sc.ap[:, dk_], ccmq.ap[:, a * 512:(a + 1) * 512], ALU.add, [Sc.b, ccmq.b], [Sc.b])
                if qt >= 2:
                    for itn in range(13):
                        kb.tt("dve", mid, lo, hi, ALU.add, [bsb], [bsb])
                        kb.ts("dve", mid, mid, 0.5, None, ALU.mult, None, [bsb], [bsb])
                        kb.ts("dve", junk.ap[:, 0:nk], Sc.ap[:, 0:nk], mid, 0.0, ALU.is_ge, ALU.add, [Sc.b, bsb], [junk.b, bsb], accum_out=cnt)
                        kb.ts("dve", cond, cnt, 255.5, None, ALU.is_ge, None, [bsb], [bsb])
                        kb.tt("dve", dd, mid, lo, ALU.subtract, [bsb], [bsb])
                        kb.stt("dve", lo, dd, cond, lo, ALU.mult, ALU.add, [bsb], [bsb])
                        kb.tt("dve", dd, hi, mid, ALU.subtract, [bsb], [bsb])
                        kb.stt("dve", hi, dd, cond, mid, ALU.mult, ALU.add, [bsb], [bsb])
                else:
                    kb.memset("dve", lo, -1e29, [bsb])
                kb.ts("dve", mb_out.ap[:, 0:nk], Sc.ap[:, 0:nk], lo, NEGM, ALU.is_lt, ALU.mult, [Sc.b, bsb], [mb_out.b])

            def attn_head_dsa(jb, h):
                c = h // 2
                rows = slice((h % 2) * 64, (h % 2) * 64 + 64)
                DKc = DKs[c]
                dqc = DQb[jb % 2][c]
                mbs = maskb[jb % 2]
                mixd, mixb = scr["MIX"]
                nkt = 4 * jb + 4
                po = 3
                pd = 5
                pend = None
                qs = slice(jb * 512, (jb + 1) * 512)
                for kt in range(nkt + 1):
                    if kt < nkt:
                        ps_ = kt % 3
                        ks = slice(kt * 128, (kt + 1) * 128)
                        kb.mm(PS[ps_][:, :], DKc.ap[rows, ks], dqc.ap[rows, :], True, False, [DKc.b, dqc.b], [PB[ps_]])
                        for a in range(4):
                            mb_ = mbs[a]
                            kb.mm(PS[ps_][:, a * 128:(a + 1) * 128], mb_.ap[:, ks], ident_bf[:, :], False, a == 3, [mb_.b, misc_b], [PB[ps_]])
                        pt_ = PT[kt % 3]
                        kb.act(pt_.ap, PS[ps_][:, :], AF.Exp, [PB[ps_], negB_b], [pt_.b], bias=negB[:, 8 + h:9 + h], scale=0.125)
                    if pend is not None:
                        pk_, ppt = pend
                        kb.mm(PS[po][0:64, :], vv[:, pk_, h * 64:(h + 1) * 64], ppt.ap, pk_ == 0, pk_ == nkt - 1, [ppt.b, Vb_cur[0]], [PB[po]])
                        kb.mm(PS[pd][0:64, :], ones_bf[:, 0:64], ppt.ap, pk_ == 0, pk_ == nkt - 1, [ppt.b, misc_b], [PB[pd]])
                    pend = (kt, PT[kt % 3]) if kt < nkt else None
                rc = orec[h % 2]
                os_ = ost[h % 2]
                ob_ = osb[h % 2]
                kb.act(rc.ap[0:64, :], PS[pd][0:64, :], AF.Ln, [PB[pd]], [rc.b])
                kb.act(rc.ap[0:64, :], rc.ap[0:64, :], AF.Exp, [rc.b], [rc.b], scale=-1.0)
                kb.cp("act", ob_.ap[0:64, :], PS[po][0:64, :], [PB[po]], [ob_.b])
                kb.tt("pool", os_.ap[0:64, :], ob_.ap[0:64, :], rc.ap[0:64, :], ALU.mult, [ob_.b, rc.b], [os_.b])
                kb.dma(mixd[512 + h * 64:512 + (h + 1) * 64, qs], os_.ap[0:64, :], [os_.b], [mixb])

            load_block(0)
            for a in range(4):
                idx_tile(0, a)
            for jb in range(NG):
                if jb + 1 < NG:
                    load_block(jb + 1)
                for a in range(4):
                    if jb + 1 < NG:
                        idx_tile(jb + 1, a)
                    attn_head_dsa(jb, 2 * a)
                    attn_head_dsa(jb, 2 * a + 1)

        def mla_proj_phase(layer, s):
            global A_sq, A_rs, A_tmp
            j = layer // 2
            S.fence()
            A.reset()
            wdn = A.bf(8 * 768, "wdn")
            wuq = A.bf(3 * 1536, "wuq")
            wukv = A.bf(2 * 2048, "wukv")
            mark = A.off
            wdn.b = load_w_bf(wdn.ap, mla_down[j], 8 * 768, "wdn")
            A.off = mark
            wuq.b = load_w_bf(wuq.ap, mla_uq[j], 3 * 1536, "wuq")
            A.off = mark
            wukv.b = load_w_bf(wukv.ap, mla_ukv[j], 2 * 2048, "wukv")
            A.off = mark
            kb.dma(qn_t[:, 0:3], mla_qn[j, :, :], [], [qn_b])
            kb.dma(qn_t[:, 3:5], mla_kvn[j, :, :], [], [qn_b])
            S.fence()
            cos64 = A.f32(SQ, "cos64")
            sin64 = A.f32(SQ, "sin64")
            mk2 = A.off
            rope_tables(s, 526, cos64, sin64)
            A.off = mk2
            S.fence()
            hg = A.f32(8 * 512, "hg")
            uT = A.bf(8 * 512, "uT")
            A_sq = A.bf(8 * 512, "sq")
            A_rs = A.f32(512, "rs")
            A_tmp = [A.f32(512, "tmp%d" % i) for i in range(2)]
            cl = A.f32(6 * 512, "cl")
            cn = A.bf(5 * 512, "cn")
            sq2 = A.bf(5 * 512, "sq2")
            rs2 = A.f32(1024, "rs2")
            gst = [A.bf(512, "gst%d" % i) for i in range(3)]
            sqs = [A.bf(512, "sqs%d" % i) for i in range(2)]
            t1 = A.f32(512, "t1")
            t2 = A.f32(512, "t2")
            vst = [A.bf(512, "vst%d" % i) for i in range(2)]
            kb.memset("pool", stat[:, 0:32], 0.0, [stat_b])
            epsq = smallf[:, 2:3]
            epsk = smallf[:, 3:4]
            kb.memset("pool", epsq, 384.0 * 1e-6, [misc_b])
            kb.memset("pool", epsk, 256.0 * 1e-6, [misc_b])
            kb.ts("dve", qn_t[:, 0:3], qn_t[:, 0:3], float(np.sqrt(384.0)), None, ALU.mult, None, [qn_b], [qn_b])
            kb.ts("dve", qn_t[:, 3:5], qn_t[:, 3:5], 16.0, None, ALU.mult, None, [qn_b], [qn_b])
            hsrc, hsrc_b = hT[s][:, :], hT_b[s]
            for g in range(NG):
                tk = slice(g * 512, (g + 1) * 512)
                norm_group(hsrc, hsrc_b, s, 1, g, hg, uT)
                for c in range(6):
                    pbk = c % 2
                    for k in range(8):
                        kb.mm(PS[pbk][:, :], wdn.ap[:, k * 768 + c * 128:k * 768 + (c + 1) * 128], uT.ap[:, k * 512:(k + 1) * 512],
                              k == 0, k == 7, [wdn.b, uT.b], [PB[pbk]])
                    if c < 5:
                        kb.cp("act", cl.ap[:, c * 512:(c + 1) * 512], PS[pbk][:, :], [PB[pbk]], [cl.b])
                        kb.act(sq2.ap[:, c * 512:(c + 1) * 512], PS[pbk][:, :], AF.Square, [PB[pbk]], [sq2.b])
                    else:
                        d_ = gst[0]
                        dstb_cur[0] = d_.b
                        rope_block(PS[pbk], PB[pbk], 0, 32, g, cos64, sin64, d_.ap, t1, t2)
                        kb.act(sqs[0].ap[0:64, :], d_.ap[0:64, :], AF.Square, [d_.b], [sqs[0].b])
                        kb.mm(PS[2][:, :], ones_bf[0:64, :], sqs[0].ap[0:64, :], True, True, [sqs[0].b, misc_b], [PB[2]])
                        kb.cp("dve", rs2.ap[:, 512:1024], PS[2][:, :], [PB[2]], [rs2.b])
                        kb.dma(scr["KR"][0][:, tk], d_.ap[0:64, :], [d_.b], [scr["KR"][1]])
                for (c0, c1, epsc, col) in ((0, 3, epsq, 0), (3, 5, epsk, 1)):
                    for c in range(c0, c1):
                        kb.mm(PS[3][:, :], ones_bf[:, :], sq2.ap[:, c * 512:(c + 1) * 512], c == c0, c == c1 - 1, [sq2.b, misc_b], [PB[3]])
                    kb.act(A_rs.ap, PS[3][:, :], AF.Sqrt, [PB[3]], [A_rs.b], bias=epsc, scale=1.0)
                    kb.recip(A_rs.ap, A_rs.ap, [A_rs.b], [A_rs.b])
                    for c in range(c0, c1):
                        kb.stt("dve", cn.ap[:, c * 512:(c + 1) * 512], cl.ap[:, c * 512:(c + 1) * 512], qn_t[:, c:c + 1], A_rs.ap,
                               ALU.mult, ALU.mult, [cl.b, A_rs.b, qn_b], [cn.b])
                for h in range(8):
                    pn = (h % 2) * 2
                    pr = (h % 2) * 2 + 1
                    for k in range(3):
                        kb.mm(PS[pn][:, :], wuq.ap[:, k * 1536 + h * 192:k * 1536 + h * 192 + 128], cn.ap[:, k * 512:(k + 1) * 512],
                              k == 0, k == 2, [wuq.b, cn.b], [PB[pn]])
                    for k in range(3):
                        kb.mm(PS[pr][0:64, :], wuq.ap[:, k * 1536 + h * 192 + 128:k * 1536 + h * 192 + 192], cn.ap[:, k * 512:(k + 1) * 512],
                              k == 0, k == 2, [wuq.b, cn.b], [PB[pr]])
                    dn = gst[1]
                    dr = gst[2]
                    kb.cp("act", dn.ap[:, :], PS[pn][:, :], [PB[pn]], [dn.b])
                    dstb_cur[0] = dr.b
                    rope_block(PS[pr], PB[pr], 0, 32, g, cos64, sin64, dr.ap, t1, t2)
                    kb.act(sqs[0].ap[:, :], dn.ap[:, :], AF.Square, [dn.b], [sqs[0].b])
                    kb.act(sqs[1].ap[0:64, :], dr.ap[0:64, :], AF.Square, [dr.b], [sqs[1].b])
                    kb.mm(PS[6][:, :], ones_bf[:, :], sqs[0].ap[:, :], True, False, [sqs[0].b, misc_b], [PB[6]])
                    kb.mm(PS[6][:, :], ones_bf[0:64, :], sqs[1].ap[0:64, :], False, True, [sqs[1].b, misc_b], [PB[6]])
                    kb.S.op("dve", lambda e: e.tensor_reduce(smallf[:, 32:33], PS[6][:, :], AX.X, ALU.max), [PB[6]], [misc_b])
                    kb.tt("dve", stat[:, h:h + 1], stat[:, h:h + 1], smallf[:, 32:33], ALU.max, [misc_b, stat_b], [stat_b])
                    kb.dma(scr["QN%d" % h][0][:, tk], dn.ap, [dn.b], [scr["QN%d" % h][1]])
                    kb.dma(scr["QR%d" % h][0][:, tk], dr.ap[0:64, :], [dr.b], [scr["QR%d" % h][1]])
                for h in range(8):
                    pn = 4 + (h % 2)
                    for k in range(2):
                        kb.mm(PS[pn][:, :], wukv.ap[:, k * 2048 + h * 128:k * 2048 + (h + 1) * 128], cn.ap[:, (3 + k) * 512:(4 + k) * 512],
                              k == 0, k == 1, [wukv.b, cn.b], [PB[pn]])
                    dn = gst[h % 2]
                    kb.cp("act", dn.ap[:, :], PS[pn][:, :], [PB[pn]], [dn.b])
                    kb.act(sqs[h % 2].ap[:, :], PS[pn][:, :], AF.Square, [PB[pn]], [sqs[h % 2].b])
                    kb.mm(PS[6][:, :], ones_bf[:, :], sqs[h % 2].ap[:, :], True, True, [sqs[h % 2].b, misc_b], [PB[6]])
                    kb.tt("dve", t1.ap, PS[6][:, :], rs2.ap[:, 512:1024], ALU.add, [PB[6], rs2.b], [t1.b])
                    kb.S.op("dve", lambda e: e.tensor_reduce(smallf[:, 32:33], t1.ap, AX.X, ALU.max), [t1.b], [misc_b])
                    kb.tt("dve", stat[:, 16 + h:17 + h], stat[:, 16 + h:17 + h], smallf[:, 32:33], ALU.max, [misc_b, stat_b], [stat_b])
                    kb.dma(scr["KN%d" % h][0][:, tk], dn.ap, [dn.b], [scr["KN%d" % h][1]])
                for tt_ in range(4):
                    gt = g * 4 + tt_
                    for half in range(2):
                        pbk = 2 + half
                        for k in range(2):
                            kb.mm(PS[pbk][:, :], cn.ap[:, (3 + k) * 512 + tt_ * 128:(3 + k) * 512 + (tt_ + 1) * 128],
                                  wukv.ap[:, k * 2048 + 1024 + half * 512:k * 2048 + 1024 + (half + 1) * 512], k == 0, k == 1,
                                  [wukv.b, cn.b], [PB[pbk]])
                        v_ = vst[half]
                        kb.cp("act", v_.ap[:, :], PS[pbk][:, :], [PB[pbk]], [v_.b])
                        kb.dma(scr["MV"][0][gt * 128:(gt + 1) * 128, half * 512:(half + 1) * 512], v_.ap, [v_.b], [scr["MV"][1]])
            finish_bounds(8, float(192.0 ** -0.5))

        def mla_attn_phase(s):
            S.fence()
            A.reset()
            vall = A.bf(NKT * 1024, "mvall")
            vd, vb = scr["MV"]
            kb.dma(vall.ap.rearrange("p (t d) -> p t d", d=1024), vd.rearrange("(t p) d -> p t d", p=128), [vb], [vall.b])
            Vb_cur[0] = vall.b
            vv = vall.ap.rearrange("p (t d) -> p t d", d=1024)
            KR = A.bf(SQ, "KR")
            ccm = build_mask(1)
            kb.dma(KR.ap[0:64, :], scr["KR"][0][:, :], [scr["KR"][1]], [KR.b])
            KN = [A.bf(SQ, "KN%d" % i) for i in range(2)]
            QN = [A.bf(SQ, "QN%d" % i) for i in range(2)]
            QR = [A.bf(SQ, "QR%d" % i) for i in range(2)]
            PT = [A.bf(512, "PT%d" % i) for i in range(3)]
            ost = [A.bf(512, "ost%d" % i) for i in range(2)]
            orec = [A.f32(512, "orec%d" % i) for i in range(2)]
            sc = float(192.0 ** -0.5)
            for h in range(8):
                kn = KN[h % 2]
                qn = QN[h % 2]
                qr = QR[h % 2]
                kb.dma(kn.ap, scr["KN%d" % h][0][:, :], [scr["KN%d" % h][1]], [kn.b])
                kb.dma(qn.ap, scr["QN%d" % h][0][:, :], [scr["QN%d" % h][1]], [qn.b])
                kb.dma(qr.ap[0:64, :], scr["QR%d" % h][0][:, :], [scr["QR%d" % h][1]], [qr.b])
                kq_b = Buf("kq")
                attn_head_mla(h, kn, qn, KR, qr, vv, PT, ost, orec, sc, ccm)

        def attn_head_mla(h, kn, qn, KR, qr, vv, PT, ost, orec, sc, ccm):
            mixd, mixb = scr["MIX"]
            for jb in range(NG):
                qs = slice(jb * 512, (jb + 1) * 512)
                nkt = 4 * jb + 4
                po = 3 + (jb % 2)
                pd = 5 + (jb % 2)
                pend = None
                for kt in range(nkt + 1):
                    if kt < nkt:
                        ps_ = kt % 3
                        ks = slice(kt * 128, (kt + 1) * 128)
                        diag = kt >= 4 * jb
                        kb.mm(PS[ps_][:, :], kn.ap[:, ks], qn.ap[:, qs], True, False, [kn.b, qn.b], [PB[ps_]])
                        kb.mm(PS[ps_][:, :], KR.ap[0:64, ks], qr.ap[0:64, qs], False, not diag, [KR.b, qr.b], [PB[ps_]])
                        if diag:
                            a = kt - 4 * jb
                            kb.mm(PS[ps_][:, :], ident_bf[:, :], ccm.ap[:, a * 512:(a + 1) * 512], False, True, [misc_b, ccm.b], [PB[ps_]])
                        pt_ = PT[kt % 3]
                        kb.act(pt_.ap, PS[ps_][:, :], AF.Exp, [PB[ps_], negB_b], [pt_.b], bias=negB[:, h:h + 1], scale=sc)
                    if pend is not None:
                        pk_, ppt = pend
                        kb.mm(PS[po][:, :], vv[:, pk_, h * 128:(h + 1) * 128], ppt.ap, pk_ == 0, pk_ == nkt - 1, [ppt.b, Vb_cur[0]], [PB[po]])
                        kb.mm(PS[pd][:, :], ones_bf[:, :], ppt.ap, pk_ == 0, pk_ == nkt - 1, [ppt.b, misc_b], [PB[pd]])
                    pend = (kt, PT[kt % 3]) if kt < nkt else None
                rc = orec[jb % 2]
                os_ = ost[jb % 2]
                kb.recip(rc.ap, PS[pd][:, :], [PB[pd]], [rc.b])
                kb.tt("dve", os_.ap, PS[po][:, :], rc.ap, ALU.mult, [PB[po], rc.b], [os_.b])
                kb.dma(mixd[h * 128:(h + 1) * 128, qs], os_.ap, [os_.b], [mixb])

        def final_phase():
            global A_sq, A_rs, A_tmp
            S.fence()
            A.reset()
            hg = A.f32(8 * 512, "hg")
            uo = [A.f32(8 * 512, "uo%d" % i) for i in range(2)]
            A_sq = A.bf(8 * 512, "sq")
            A_rs = A.f32(512, "rs")
            A_tmp = [A.f32(512, "tmp%d" % i) for i in range(2)]
            it = 0
            for s in range(NSEQ):
                for g in range(NG):
                    u_ = uo[it % 2]
                    it += 1
                    norm_group(hT[s][:, :], hT_b[s], s, 0, g, hg, u_, final=True)
                    dst = outT[s].rearrange("(c p) t -> p c t", p=128)[:, :, g * 512:(g + 1) * 512]
                    S.out_dma.append(kb.S.op("sp", (lambda dst=dst, u_=u_: lambda e: e.dma_start(out=dst, in_=u_.ap.rearrange("p (c t) -> p c t", c=8)))(),
                                             [u_.b], [], dma=True))

        steps = []
        for layer in range(DEPTH):
            steps.append(("mod%d" % layer, (lambda layer=layer: (compute_mod(layer), compute_abg(layer)))))
            steps.append(("ffn%d_0" % layer, (lambda layer=layer: ffn_phase(layer, 0))))
            for s in range(NSEQ):
                if layer % 2 == 0:
                    steps.append(("hproj%d_%d" % (layer, s), (lambda layer=layer, s=s: hyb_proj_phase(layer, s))))
                    steps.append(("fox%d_%d" % (layer, s), (lambda layer=layer, s=s: fox_attn_phase(s))))
                    steps.append(("dsa%d_%d" % (layer, s), (lambda layer=layer, s=s: dsa_phase(s))))
                    steps.append(("oproj%d_%d" % (layer, s), (lambda layer=layer, s=s: outproj_phase(layer, hyb_out[layer // 2], s))))
                else:
                    steps.append(("mproj%d_%d" % (layer, s), (lambda layer=layer, s=s: mla_proj_phase(layer, s))))
                    steps.append(("mattn%d_%d" % (layer, s), (lambda layer=layer, s=s: mla_attn_phase(s))))
                    steps.append(("oproj%d_%d" % (layer, s), (lambda layer=layer, s=s: outproj_phase(layer, mla_out[layer // 2], s))))
            steps.append(("ffn%d_1" % layer, (lambda layer=layer: ffn_phase(layer, 1))))
        steps.append(("final", final_phase))
        for name, fn in steps:
            fn()
            if cfg.get("stop") == name:
                break
        if cfg.get("dbg_sb"):
            S.fence()
            named = dict(modT=modT, ABG=ABG, condT=condT, stat=stat, negB=negB, iw_all=iw_all, ngT=ngT, smallf=smallf)
            for nm in cfg["dbg_sb"]:
                t_ = named[nm]
                dd_ = nc.dram_tensor("dbgsb_" + nm, list(t_.shape), F32, kind="ExternalOutput")
                kb.S.op("sp", (lambda dd_=dd_, t_=t_: lambda e: e.dma_start(out=dd_[:, :], in_=t_[:]))(), [], [], dma=True)
        S.emit()
    return nc


PERM64 = list(range(0, 8)) + list(range(16, 40)) + list(range(8, 16)) + list(range(40, 64))


def make_consts():
    c = np.zeros((128, 640), np.float32)
    p = np.arange(128)
    c[:, 0:512] = np.arange(512)[None, :]
    c[:, 512] = p
    for a in range(4):
        c[:, 513 + a] = 128 * a + p
        c[:, 517 + a] = 64 * ((128 * a + p) // 64)
        c[:, 521 + a] = 64 * ((128 * a + p) // 64 + 1)
    r = p % 64
    i16 = np.where(r < 8, r, np.where((r >= 32) & (r < 40), r - 32, 0))
    c[:, 525] = -np.log(500000.0) * 2.0 * i16 / 16.0
    i64 = np.where(r < 32, r, r - 32)
    c[:, 526] = -np.log(10000.0) * 2.0 * i64 / 64.0
    c[:, 527] = np.where((p % 32) == 1, -1.0, 0.0)
    return c


def lay_k(w, ncols_pad=None):
    K, N = w.shape
    kc = K // 128
    return np.ascontiguousarray(w.reshape(kc, 128, N).transpose(1, 0, 2).reshape(128, kc * N))


def gather_cols(w, idx):
    idx = np.asarray(idx)
    out = np.zeros((w.shape[0], len(idx)), w.dtype)
    m = idx >= 0
    out[:, m] = w[:, idx[m]]
    return out


def prep_shared(inp, DEPTH):
    f = {}
    n_even = (DEPTH + 1) // 2
    n_odd = DEPTH // 2
    f["consts"] = make_consts()
    f["ada_w"] = np.stack([lay_k(inp["ada_w"][i]) for i in range(DEPTH)])
    f["ada_b"] = np.ascontiguousarray(inp["ada_b"][:DEPTH].reshape(DEPTH, 1, 9216))
    ng = inp["norm_g"][:DEPTH].reshape(DEPTH * 3, 8, 128).transpose(0, 2, 1)
    f["norm_gT"] = np.ascontiguousarray(ng)
    f["final_gT"] = np.ascontiguousarray(inp["final_g"].reshape(8, 128).T)
    f["w_gate"] = np.stack([lay_k(inp["ffn_w_gate"][i, j]) for i in range(DEPTH) for j in range(2)])
    f["w_up"] = np.stack([lay_k(inp["ffn_w_up"][i, j]) for i in range(DEPTH) for j in range(2)])
    f["w_down"] = np.stack([lay_k(inp["ffn_w_down"][i, j]) for i in range(DEPTH) for j in range(2)])
    off = np.cumsum([0, 512, 512, 512, 8, 512, 512, 512, 256, 4, 64])
    o_fq, o_fk, o_fv, o_ff, o_dq, o_dk, o_dv, o_iq, o_iw, o_ik = off[:10]
    fm = []
    for h in range(8):
        blk = [-1] * 128
        blk[0:64] = list(range(o_fq + h * 64, o_fq + (h + 1) * 64))
        blk[64] = o_ff + h
        blk[65] = o_ff + h
        fm += blk
    for h in range(8):
        blk = [-1] * 128
        blk[0:64] = list(range(o_fk + h * 64, o_fk + (h + 1) * 64))
        fm += blk
    for base in (o_dq, o_dk):
        for h in range(8):
            fm += [base + h * 64 + d for d in PERM64]
    for h in range(4):
        fm += [o_iq + h * 64 + d for d in PERM64]
    fm += [o_ik + d for d in PERM64] * 2
    assert len(fm) == 3456
    tm = list(range(o_fv, o_fv + 512)) + list(range(o_dv, o_dv + 512)) + list(range(o_iw, o_iw + 4)) + [-1] * 4
    f["hyb_fm"] = np.stack([lay_k(gather_cols(inp["hyb_w_in"][j], fm)) for j in range(n_even)])
    f["hyb_tm"] = np.stack([lay_k(gather_cols(inp["hyb_w_in"][j], tm)) for j in range(n_even)])
    f["hyb_out"] = np.stack([lay_k(inp["hyb_w_out"][j]) for j in range(n_even)])
    f["fox_b"] = np.ascontiguousarray(np.broadcast_to(inp["fox_b_f"][:n_even, None, :], (n_even, 128, 8))).astype(np.float32)
    if n_odd:
        dn_idx = list(range(704)) + [-1] * 64
        f["mla_down"] = np.stack([lay_k(gather_cols(inp["mla_w_down"][j], dn_idx)) for j in range(n_odd)])
        f["mla_qn"] = np.ascontiguousarray(inp["mla_q_norm"][:n_odd].reshape(n_odd, 3, 128).transpose(0, 2, 1))
        f["mla_kvn"] = np.ascontiguousarray(inp["mla_kv_norm"][:n_odd].reshape(n_odd, 2, 128).transpose(0, 2, 1))
        f["mla_uq"] = np.stack([lay_k(inp["mla_w_uq"][j]) for j in range(n_odd)])
        kv_idx = [h * 256 + d for h in range(8) for d in range(128)] + [h * 256 + 128 + d for h in range(8) for d in range(128)]
        f["mla_ukv"] = np.stack([lay_k(gather_cols(inp["mla_w_ukv"][j], kv_idx)) for j in range(n_odd)])
        f["mla_out"] = np.stack([lay_k(inp["mla_w_out"][j]) for j in range(n_odd)])
    return f


def run_cfg(inp, cfg, n_cores):
    NSEQ = cfg["NSEQ"]
    DEPTH = cfg["DEPTH"]
    shared = prep_shared(inp, DEPTH)
    nc = build_program(cfg)
    in_maps = []
    for c in range(n_cores):
        sl = slice(c * NSEQ, (c + 1) * NSEQ)
        m = dict(shared)
        m["xT"] = np.ascontiguousarray(inp["x"][sl].transpose(0, 2, 1))
        cc = inp["c"][sl]
        m["cT"] = np.ascontiguousarray(cc.reshape(NSEQ, 8, 128).transpose(2, 1, 0).reshape(128, 8 * NSEQ))
        m["pos"] = np.ascontiguousarray(inp["positions"][sl]).astype(np.int32)
        in_maps.append(m)
    res = run_bass_kernel_spmd(nc, in_maps, core_ids=list(range(n_cores)))
    return res


def kernel(**inputs):
    inp = {k: np.asarray(v) for k, v in inputs.items()}
    cfg = dict(S=4096, NSEQ=2, DEPTH=4)
    res = run_cfg(inp, cfg, 8)
    outs = [np.asarray(r["outT"]).transpose(0, 2, 1) for r in res.results]
    return np.ascontiguousarray(np.concatenate(outs, axis=0)).astype(np.float32)
```

```python
import contextlib
import numpy as np
import ml_dtypes
import concourse.bass as bass
import concourse.mybir as mybir
from concourse.bass_utils import run_bass_kernel_spmd

F32 = mybir.dt.float32
BF16 = mybir.dt.bfloat16
I32 = mybir.dt.int32
ALU = mybir.AluOpType
AF = mybir.ActivationFunctionType
AX = mybir.AxisListType

STREAMS = ("pe", "act", "dve", "pool", "sp")
N_DMA_SEMS = 12
EPOCH = 20000

D = 1024
DFF = 2816
NFC = 22
NEGM = -30000.0


class Buf:
    __slots__ = ("name", "w", "r", "excl")

    def __init__(self, name, excl=False):
        self.name = name
        self.w = None
        self.r = []
        self.excl = excl


class Sched:
    def __init__(self, nc):
        self.nc = nc
        self.ops = []
        self.cnt = {(s, k): 0 for s in STREAMS for k in "cd"}
        self.seen = {s: {} for s in STREAMS}
        self.fence_deps = {s: set() for s in STREAMS}
        self.out_dma = []

    def fence(self):
        deps = set()
        for s in STREAMS:
            n = self.cnt[(s, "c")]
            if n > 0:
                deps.add((s, "c", n - 1))
            nd = self.cnt[(s, "d")]
            for j in range(max(0, nd - N_DMA_SEMS), nd):
                deps.add((s, "d", j))
        for s in STREAMS:
            self.fence_deps[s] = set(deps)
            self.seen[s] = {k: v for k, v in self.seen[s].items() if not isinstance(k, tuple)}

    def op(self, stream, fn, reads=(), writes=(), dma=False):
        kind = "d" if dma else "c"
        idx = self.cnt[(stream, kind)]
        self.cnt[(stream, kind)] += 1
        me = (stream, kind, idx)
        deps = set()
        if self.fence_deps[stream]:
            deps |= self.fence_deps[stream]
            self.fence_deps[stream] = set()
        for b in reads:
            if not b.excl and b.w is not None:
                deps.add(b.w)
        wl = list(writes) + [b for b in reads if b.excl]
        for b in wl:
            if b.w is not None:
                deps.add(b.w)
            deps.update(b.r)
        for b in reads:
            if not b.excl:
                if dma:
                    b.r.append(me)
                else:
                    b.r = [x for x in b.r if not (x[0] == stream and x[1] == "c")] + [me]
        for b in wl:
            b.w = me
            b.r = []
        fd = []
        seen = self.seen[stream]
        best = {}
        for d in deps:
            ps, pk, pi = d
            if d == me:
                continue
            if pk == "d":
                if d in seen:
                    continue
                seen[d] = True
                fd.append(d)
            else:
                if ps == stream and stream == "pe" and not dma:
                    continue
                if ps == stream and pi >= idx and not dma:
                    continue
                if seen.get(ps, -1) >= pi:
                    continue
                if best.get(ps, -1) < pi:
                    best[ps] = pi
        for ps, pi in best.items():
            seen[ps] = pi
            fd.append((ps, "c", pi))
        self.ops.append((stream, kind, idx, fn, fd))
        return me

    def emit(self):
        nc = self.nc
        needs = {s: [False] * self.cnt[(s, "c")] for s in STREAMS}
        for stream, kind, idx, fn, fd in self.ops:
            for (ps, pk, pi) in fd:
                if pk == "c":
                    needs[ps][pi] = True
        val = {}
        for s in STREAMS:
            c = 0
            v = []
            for n in needs[s]:
                if n:
                    c += 1
                v.append(c)
            val[s] = v
        per = {s: [] for s in STREAMS}
        for o in self.ops:
            per[o[0]].append(o)
        self.nwaits = 0
        with contextlib.ExitStack() as st:
            sems = {}
            for s in STREAMS:
                tot = val[s][-1] if val[s] else 0
                sems[s] = [st.enter_context(nc.semaphore("s_%s_%d" % (s, i))) for i in range(tot // EPOCH + 1)]
            dsems = {s: [st.enter_context(nc.semaphore("d_%s_%d" % (s, i))) for i in range(N_DMA_SEMS)]
                     for s in STREAMS if self.cnt[(s, "d")] > 0}
            block = st.enter_context(nc.Block())

            def semv(ps, c):
                ep = (c - 1) // EPOCH
                return sems[ps][ep], c - ep * EPOCH

            def run(s, e):
                for (stream, kind, idx, fn, fd) in per[s]:
                    for (ps, pk, pi) in fd:
                        self.nwaits += 1
                        if pk == "d":
                            e.wait_ge(dsems[ps][pi % N_DMA_SEMS], 16 * (pi // N_DMA_SEMS + 1))
                        else:
                            sm, v = semv(ps, val[ps][pi])
                            e.wait_ge(sm, v)
                    if kind == "d":
                        if idx >= N_DMA_SEMS:
                            e.wait_ge(dsems[s][idx % N_DMA_SEMS], 16 * (idx // N_DMA_SEMS))
                        fn(e).then_inc(dsems[s][idx % N_DMA_SEMS], 16)
                    else:
                        ins = fn(e)
                        if needs[s][idx]:
                            sm, v = semv(s, val[s][idx])
                            ins.then_inc(sm, 1)
                n = self.cnt[(s, "d")]
                for j in range(max(0, n - N_DMA_SEMS), n):
                    e.wait_ge(dsems[s][j % N_DMA_SEMS], 16 * (j // N_DMA_SEMS + 1))

            @block.tensor
            def _(e):
                run("pe", e)

            @block.scalar
            def _(e):
                run("act", e)

            @block.vector
            def _(e):
                run("dve", e)

            @block.gpsimd
            def _(e):
                run("pool", e)

            @block.sync
            def _(e):
                run("sp", e)


class Tl:
    __slots__ = ("ap", "b")

    def __init__(self, ap, b):
        self.ap = ap
        self.b = b


class KB:
    def __init__(self, nc, S, cfg):
        self.nc = nc
        self.S = S
        self.cfg = cfg
        self.st = contextlib.ExitStack()
        self.uid = 0
        self.rr = 0

    def mm(self, out, lhsT, rhs, start, stop, reads, writes):
        self.S.op("pe", lambda e: e.matmul(out, lhsT, rhs, start=start, stop=stop, skip_group_check=True), reads, writes)

    def act(self, out, in_, func, reads, writes, bias=0.0, scale=1.0, accum_out=None):
        if accum_out is None:
            self.S.op("act", lambda e: e.activation(out, in_, func, bias=bias, scale=scale), reads, writes)
        else:
            self.S.op("act", lambda e: e.activation(out, in_, func, bias=bias, scale=scale, accum_out=accum_out), reads, writes)

    def tt(self, eng, out, in0, in1, op, reads, writes):
        self.S.op(eng, lambda e: e.tensor_tensor(out, in0, in1, op), reads, writes)

    def ts(self, eng, out, in0, s1, s2, op0, op1, reads, writes, accum_out=None):
        if accum_out is None:
            if op1 is None:
                self.S.op(eng, lambda e: e.tensor_single_scalar(out, in0, s1, op0), reads, writes)
            else:
                self.S.op(eng, lambda e: e.tensor_scalar(out, in0, s1, s2, op0, op1), reads, writes)
        else:
            self.S.op(eng, lambda e: e.tensor_scalar(out, in0, s1, s2, op0, op1, accum_out=accum_out), reads, writes)

    def stt(self, eng, out, in0, scalar, in1, op0, op1, reads, writes):
        self.S.op(eng, lambda e: e.scalar_tensor_tensor(out, in0, scalar, in1, op0, op1), reads, writes)

    def cp(self, eng, out, in_, reads, writes):
        if eng == "act":
            self.S.op("act", lambda e: e.copy(out, in_), reads, writes)
        else:
            self.S.op(eng, lambda e: e.tensor_copy(out, in_), reads, writes)

    def memset(self, eng, ap, v, writes):
        self.S.op(eng, lambda e: e.memset(ap, v), (), writes)

    def dma(self, out, in_, reads, writes, slow=False):
        if slow:
            self.S.op("sp", lambda e: e.dma_start(out=out, in_=in_, allow_slow_non_contiguous=True), reads, writes, dma=True)
        else:
            self.S.op("sp", lambda e: e.dma_start(out=out, in_=in_), reads, writes, dma=True)

    def recip(self, out, in_, reads, writes):
        self.S.op("dve", lambda e: e.reciprocal(out, in_), reads, writes)

    def sb(self, name, shape, dt):
        t = self.st.enter_context(self.nc.sbuf_tensor(name, list(shape), dt))
        return t

    def psum(self, name, shape, dt):
        return self.st.enter_context(self.nc.psum_tensor(name, list(shape), dt))

    def dram(self, name, shape, dt, kind="Internal"):
        return self.nc.dram_tensor(name, list(shape), dt, kind=kind)


class Arena:
    def __init__(self, t_f32, nwords):
        self.t = t_f32
        self.n = nwords
        self.off = 0

    def reset(self):
        self.off = 0

    def f32(self, n, name="a"):
        o = self.off
        self.off += n
        assert self.off <= self.n, ("arena overflow", name, self.off, self.n)
        return Tl(self.t[:, o:o + n], Buf(name))

    def bf(self, n, name="a"):
        w = (n + 1) // 2
        o = self.off
        self.off += w
        assert self.off <= self.n, ("arena overflow", name, self.off, self.n)
        return Tl(self.t[:, o:o + w].bitcast(BF16), Buf(name))


def build_program(cfg):
    SQ = cfg["S"]
    NSEQ = cfg["NSEQ"]
    DEPTH = cfg["DEPTH"]
    NG = SQ // 512
    NKT = SQ // 128
    nc = bass.Bass("TRN2", target_bir_lowering=False)
    S = Sched(nc)
    kb = KB(nc, S, cfg)
    n_even = (DEPTH + 1) // 2
    n_odd = DEPTH // 2

    def din(name, shape, dt=F32):
        return nc.dram_tensor(name, list(shape), dt, kind="ExternalInput")

    xT = din("xT", [NSEQ, D, SQ])
    cT = din("cT", [128, 8 * NSEQ])
    pos = din("pos", [NSEQ, SQ], I32)
    consts = din("consts", [128, 640])
    ada_w = din("ada_w", [DEPTH, 128, 8 * 9216])
    ada_b = din("ada_b", [DEPTH, 1, 9216])
    norm_gT = din("norm_gT", [DEPTH * 3, 128, 8])
    final_gT = din("final_gT", [128, 8])
    w_gate = din("w_gate", [DEPTH * 2, 128, 8 * DFF])
    w_up = din("w_up", [DEPTH * 2, 128, 8 * DFF])
    w_down = din("w_down", [DEPTH * 2, 128, NFC * D])
    NHF = 3456
    NHT = 1032
    hyb_fm = din("hyb_fm", [n_even, 128, 8 * NHF])
    hyb_tm = din("hyb_tm", [n_even, 128, 8 * NHT])
    hyb_out = din("hyb_out", [n_even, 128, 8 * D])
    fox_b = din("fox_b", [n_even, 128, 8])
    if n_odd:
        mla_down = din("mla_down", [n_odd, 128, 8 * 768])
        mla_qn = din("mla_qn", [n_odd, 128, 3])
        mla_kvn = din("mla_kvn", [n_odd, 128, 2])
        mla_uq = din("mla_uq", [n_odd, 128, 3 * 1536])
        mla_ukv = din("mla_ukv", [n_odd, 128, 2 * 2048])
        mla_out = din("mla_out", [n_odd, 128, 8 * D])
    outT = nc.dram_tensor("outT", [NSEQ, D, SQ], F32, kind="ExternalOutput")

    hT = [nc.dram_tensor("hT%d" % s, [D, SQ], F32, kind=("ExternalOutput" if cfg.get("dbg_h") else "Internal")) for s in range(NSEQ)]
    hT_b = [[[Buf("hT%d_%d_%d" % (s, g, c)) for c in range(8)] for g in range(NG)] for s in range(NSEQ)]
    xT_b = [[Buf("xT%d_%d" % (g, c)) for c in range(8)] for g in range(NG)]
    scr = {}

    def scratch(name, shape, dt=BF16):
        kind = "ExternalOutput" if name in cfg.get("dbg", []) else "Internal"
        scr[name] = (nc.dram_tensor("scr_" + name, list(shape), dt, kind=kind), Buf("scr_" + name))
        return scr[name]

    for h in range(8):
        scratch("QA%d" % h, [128, SQ])
        scratch("KA%d" % h, [128, SQ])
    scratch("VF", [SQ, 1024])
    for c in range(4):
        scratch("DQ%d" % c, [128, SQ])
        scratch("DK%d" % c, [128, SQ])
    scratch("DV", [SQ, 512])
    for c in range(2):
        scratch("IQ%d" % c, [128, SQ])
    scratch("IK", [128, SQ])
    scratch("MIX", [D, SQ])
    if n_odd:
        for h in range(8):
            scratch("QN%d" % h, [128, SQ])
            scratch("QR%d" % h, [64, SQ])
            scratch("KN%d" % h, [128, SQ])
        scratch("KR", [64, SQ])
        scratch("MV", [SQ, 1024])
    with kb.st:
        ARENA_WORDS = 50600
        arena_t = kb.sb("arena", [128, ARENA_WORDS], F32)
        A = Arena(arena_t, ARENA_WORDS)
        cst = kb.sb("cst", [128, 640], F32)
        cst_b = Buf("cst")
        ident_bf = kb.sb("ident_bf", [128, 128], BF16)
        ones_bf = kb.sb("ones_bf", [128, 128], BF16)
        zeros_f = kb.sb("zeros_f", [128, 512], F32)
        misc_b = Buf("misc")
        condT = kb.sb("condT", [128, 8 * NSEQ], F32)
        cond_b = Buf("cond")
        modT = kb.sb("modT", [128, NSEQ * 72], F32)
        mod_b = Buf("mod")
        ngT = kb.sb("ngT", [128, DEPTH * 3 * 8 + 8], F32)
        ng_b = Buf("ng")
        ABG = kb.sb("ABG", [128, NSEQ * 3 * 24], F32)
        abg_b = Buf("abg")
        stat = kb.sb("stat", [128, 64], F32)
        stat_b = Buf("stat")
        negB = kb.sb("negB", [128, 16], F32)
        negB_b = Buf("negB")
        iw_all = kb.sb("iw_all", [128, NKT * 4], F32)
        iw_b = Buf("iw")
        iwabs = kb.sb("iwabs", [128, NKT * 4], F32)
        iwsgn = kb.sb("iwsgn", [128, NKT * 4], F32)
        smallf = kb.sb("smallf", [128, 64], F32)
        fb_t = kb.sb("fb_t", [128, 8], F32)
        fb_b = Buf("fb")
        qn_t = kb.sb("qn_t", [128, 8], F32)
        qn_b = Buf("qn")
        PS = [kb.psum("ps%d" % i, [128, 512], F32) for i in range(8)]
        PB = [Buf("ps%d" % i, excl=True) for i in range(8)]

        kb.dma(cst[:], consts[:, :], [], [cst_b])
        IOTA = cst[:, 0:512]
        kb.memset("pool", zeros_f[:], 0.0, [misc_b])
        kb.memset("pool", ones_bf[:], 1.0, [misc_b])
        kb.ts("dve", ident_bf[:], cst[:, 0:128], cst[:, 512:513], None, ALU.is_equal, None, [cst_b], [misc_b])

        def build_mask(kind):
            m = A.bf(4 * 512, "mask%d" % kind)
            for a in range(4):
                if kind == 0:
                    kb.ts("dve", m.ap[:, a * 512:(a + 1) * 512], IOTA, cst[:, 513 + a:514 + a], NEGM, ALU.is_lt, ALU.mult, [cst_b], [m.b])
                elif kind == 1:
                    kb.ts("dve", m.ap[:, a * 512:(a + 1) * 512], IOTA, cst[:, 517 + a:518 + a], NEGM, ALU.is_lt, ALU.mult, [cst_b], [m.b])
                else:
                    kb.ts("dve", m.ap[:, a * 512:(a + 1) * 512], IOTA, cst[:, 521 + a:522 + a], -1e30, ALU.is_ge, ALU.mult, [cst_b], [m.b])
            return m
        kb.dma(condT[:], cT[:, :], [], [cond_b])
        kb.act(condT[:], condT[:], AF.Silu, [cond_b], [cond_b])
        for i in range(DEPTH * 3):
            kb.dma(ngT[:, i * 8:(i + 1) * 8], norm_gT[i, :, :], [], [ng_b])
        kb.dma(ngT[:, DEPTH * 24:DEPTH * 24 + 8], final_gT[:, :], [], [ng_b])

        def load_w_bf(dst_bf_ap, src_dram_ap, ncols, name):
            CH = 2048
            nst = 3
            stg = [A.f32(CH, "stg%d" % i) for i in range(nst)]
            dstb = Buf(name)
            i = 0
            engs = ("pool", "act", "dve")
            for o in range(0, ncols, CH):
                n = min(CH, ncols - o)
                s_ = stg[i % nst]
                kb.dma(s_.ap[:, 0:n], src_dram_ap[:, o:o + n], [], [s_.b])
                kb.cp(("dve", "act", "dve", "act", "pool")[i % 5], dst_bf_ap[:, o:o + n], s_.ap[:, 0:n], [s_.b], [dstb])
                i += 1
            S.fence()
            return dstb

        def compute_mod(layer):
            A.reset()
            PIECE = 512
            wst = [A.f32(8 * PIECE, "adaw%d" % i) for i in range(2)]
            bst = [A.f32(PIECE, "adab%d" % i) for i in range(2)]
            onesf = A.f32(8, "onesf")
            kb.memset("pool", onesf.ap[0:1, 0:NSEQ], 1.0, [onesf.b])
            npiece = 9216 // PIECE
            for pi in range(npiece):
                w_ = wst[pi % 2]
                b_ = bst[pi % 2]
                src = ada_w[layer].rearrange("p (k n) -> p k n", k=8)[:, :, pi * PIECE:(pi + 1) * PIECE]
                kb.dma(w_.ap.rearrange("p (k n) -> p k n", k=8), src, [], [w_.b])
                kb.dma(b_.ap[0:1, :], ada_b[layer, :, pi * PIECE:(pi + 1) * PIECE], [], [b_.b])
                for cc in range(PIECE // 128):
                    col = pi * (PIECE // 128) + cc
                    for k in range(8):
                        kb.mm(PS[0][:, col * NSEQ:(col + 1) * NSEQ], w_.ap[:, k * PIECE + cc * 128:k * PIECE + (cc + 1) * 128],
                              condT[:, k * NSEQ:(k + 1) * NSEQ], k == 0, False, [w_.b, cond_b], [PB[0]])
                    kb.mm(PS[0][:, col * NSEQ:(col + 1) * NSEQ], b_.ap[0:1, cc * 128:(cc + 1) * 128],
                          onesf.ap[0:1, 0:NSEQ], False, True, [b_.b, onesf.b], [PB[0]])
            for s in range(NSEQ):
                src = PS[0][:, 0:72 * NSEQ].rearrange("p (c s) -> p c s", s=NSEQ)[:, :, s]
                kb.cp("dve", modT[:, s * 72:(s + 1) * 72], src, [PB[0]], [mod_b])

        def compute_abg(layer):
            for s in range(NSEQ):
                for sub in range(3):
                    o = (s * 3 + sub) * 24
                    m = s * 72 + sub * 24
                    g = ngT[:, (layer * 3 + sub) * 8:(layer * 3 + sub) * 8 + 8]
                    kb.ts("dve", ABG[:, o:o + 8], modT[:, m + 8:m + 16], 1.0, None, ALU.add, None, [mod_b], [abg_b])
                    kb.tt("dve", ABG[:, o:o + 8], ABG[:, o:o + 8], g, ALU.mult, [abg_b, ng_b], [abg_b])
                    kb.ts("dve", ABG[:, o:o + 8], ABG[:, o:o + 8], 32.0, None, ALU.mult, None, [abg_b], [abg_b])
                    kb.cp("dve", ABG[:, o + 8:o + 16], modT[:, m:m + 8], [mod_b], [abg_b])
                    gs = 1.0 if sub == 1 else 0.5
                    kb.ts("dve", ABG[:, o + 16:o + 24], modT[:, m + 16:m + 24], gs, None, ALU.mult, None, [mod_b], [abg_b])

        def h_src(layer, sub, s):
            if layer == 0 and sub == 0:
                return xT[s], xT_b
            return hT[s][:, :], hT_b[s]

        def norm_group(hsrc, hsrc_b, s, sub, g, hg, uT, extra_scale=None, final=False, layer=0):
            sq = A_sq
            rs = A_rs
            tmp = A_tmp
            src = hsrc.rearrange("(c p) t -> p c t", p=128)[:, :, g * 512:(g + 1) * 512]
            kb.dma(hg.ap.rearrange("p (c t) -> p c t", c=8), src, list(hsrc_b[g]), [hg.b])
            for c in range(8):
                kb.act(sq.ap[:, c * 512:(c + 1) * 512], hg.ap[:, c * 512:(c + 1) * 512], AF.Square, [hg.b], [sq.b])
            for c in range(8):
                kb.mm(PS[7][:, :], ones_bf[:, :], sq.ap[:, c * 512:(c + 1) * 512], c == 0, c == 7, [sq.b, misc_b], [PB[7]])
            kb.act(rs.ap, PS[7][:, :], AF.Sqrt, [PB[7]], [rs.b], bias=epsb[:, 0:1], scale=1.0)
            kb.recip(rs.ap, rs.ap, [rs.b], [rs.b])
            o = (s * 3 + sub) * 24
            for c in range(8):
                t_ = tmp[c % 2]
                if final:
                    kb.stt("dve", t_.ap, hg.ap[:, c * 512:(c + 1) * 512], fg32[:, c:c + 1], rs.ap, ALU.mult, ALU.mult,
                           [hg.b, rs.b, abg_b], [t_.b])
                    kb.cp("act", uT.ap[:, c * 512:(c + 1) * 512], t_.ap, [t_.b], [uT.b])
                else:
                    kb.stt("dve", t_.ap, hg.ap[:, c * 512:(c + 1) * 512], ABG[:, o + c:o + c + 1], rs.ap, ALU.mult, ALU.mult,
                           [hg.b, rs.b, abg_b], [t_.b])
                    kb.act(uT.ap[:, c * 512:(c + 1) * 512], t_.ap, AF.Identity, [t_.b, abg_b], [uT.b],
                           bias=ABG[:, o + 8 + c:o + 9 + c], scale=1.0)

        epsb = smallf[:, 0:1]
        kb.memset("pool", smallf[:, 0:1], float(D) * 1e-6, [misc_b])
        fg32 = smallf[:, 8:16]
        kb.ts("dve", fg32, ngT[:, DEPTH * 24:DEPTH * 24 + 8], 32.0, None, ALU.mult, None, [ng_b], [abg_b])

        def ffn_phase(layer, which):
            global A_sq, A_rs, A_tmp
            S.fence()
            A.reset()
            sub = 0 if which == 0 else 2
            wi = layer * 2 + which
            wg = A.bf(8 * DFF, "wg")
            wu = A.bf(8 * DFF, "wu")
            wd = A.bf(NFC * D, "wd")
            mark = A.off
            wg.b = load_w_bf(wg.ap, w_gate[wi], 8 * DFF, "wg")
            A.off = mark
            wu.b = load_w_bf(wu.ap, w_up[wi], 8 * DFF, "wu")
            A.off = mark
            wd.b = load_w_bf(wd.ap, w_down[wi], NFC * D, "wd")
            A.off = mark
            S.fence()
            hg = A.f32(8 * 512, "hg")
            uT = A.bf(8 * 512, "uT")
            actT = A.bf(NFC * 512, "actT")
            sq = Tl(uT.ap, uT.b)
            rs = A.f32(512, "rs")
            tmp = [A.f32(512, "tmp%d" % i) for i in range(2)]
            sg = [A.bf(512, "sg%d" % i) for i in range(2)]
            hr = [A.f32(512, "hr%d" % i) for i in range(2)]
            items = [(s, g) for s in range(NSEQ) for g in range(NG)]

            def n_load(i):
                s, g = items[i]
                hsrc, hsrc_b = h_src(layer, sub, s)
                src = hsrc.rearrange("(c p) t -> p c t", p=128)[:, :, g * 512:(g + 1) * 512]
                kb.dma(hg.ap.rearrange("p (c t) -> p c t", c=8), src, list(hsrc_b[g]), [hg.b])

            def n_sq(i):
                for c in range(8):
                    kb.act(sq.ap[:, c * 512:(c + 1) * 512], hg.ap[:, c * 512:(c + 1) * 512], AF.Square, [hg.b], [sq.b])

            def n_stats(i):
                for c in range(8):
                    kb.mm(PS[7][:, :], ones_bf[:, :], sq.ap[:, c * 512:(c + 1) * 512], c == 0, c == 7, [sq.b, misc_b], [PB[7]])
                kb.act(rs.ap, PS[7][:, :], AF.Sqrt, [PB[7]], [rs.b], bias=epsb[:, 0:1], scale=1.0)
                kb.recip(rs.ap, rs.ap, [rs.b], [rs.b])

            def n_u(i, c):
                s, g = items[i]
                o = (s * 3 + sub) * 24
                t_ = tmp[c % 2]
                kb.stt("dve", t_.ap, hg.ap[:, c * 512:(c + 1) * 512], ABG[:, o + c:o + c + 1], rs.ap, ALU.mult, ALU.mult,
                       [hg.b, rs.b, abg_b], [t_.b])
                kb.act(uT.ap[:, c * 512:(c + 1) * 512], t_.ap, AF.Identity, [t_.b, abg_b], [uT.b],
                       bias=ABG[:, o + 8 + c:o + 9 + c], scale=1.0)

            def r_load(i, c):
                s, g = items[i]
                hsrc, hsrc_b = h_src(layer, sub, s)
                kb.dma(hr[c % 2].ap, hsrc[c * 128:(c + 1) * 128, g * 512:(g + 1) * 512], [hsrc_b[g][c]], [hr[c % 2].b])

            def down(i, c):
                s, g = items[i]
                o = (s * 3 + sub) * 24
                pd = 4 + (c % 2)
                for f in range(NFC):
                    kb.mm(PS[pd][:, :], wd.ap[:, f * D + c * 128:f * D + (c + 1) * 128], actT.ap[:, f * 512:(f + 1) * 512],
                          f == 0, f == NFC - 1, [wd.b, actT.b], [PB[pd]])
                h_ = hr[c % 2]
                kb.stt("dve", h_.ap, PS[pd][:, :], ABG[:, o + 16 + c:o + 17 + c], h_.ap, ALU.mult, ALU.add, [PB[pd], abg_b, h_.b], [h_.b])
                kb.dma(hT[s][c * 128:(c + 1) * 128, g * 512:(g + 1) * 512], h_.ap, [h_.b], [hT_b[s][g][c]])
                if c + 2 < 8:
                    r_load(i, c + 2)

            n_load(0)
            n_sq(0)
            n_stats(0)
            for c in range(8):
                n_u(0, c)
            for i in range(len(items)):
                s, g = items[i]
                nxt = i + 1 < len(items)
                if cfg.get("dbg_ffn") and i == 0 and layer == 0 and which == 0:
                    du = nc.dram_tensor("dbg_uT", [128, 4096], BF16, kind="ExternalOutput")
                    kb.S.op("sp", (lambda du=du, uT=uT: lambda e: e.dma_start(out=du[:, :], in_=uT.ap))(), [uT.b], [], dma=True)
                r_load(i, 0)
                r_load(i, 1)
                if nxt:
                    n_load(i + 1)
                for f in range(NFC):
                    pg = 0 + (f % 2) * 2
                    pu = 1 + (f % 2) * 2
                    for k in range(8):
                        kb.mm(PS[pg][:, :], wg.ap[:, k * DFF + f * 128:k * DFF + (f + 1) * 128], uT.ap[:, k * 512:(k + 1) * 512],
                              k == 0, k == 7, [wg.b, uT.b], [PB[pg]])
                    for k in range(8):
                        kb.mm(PS[pu][:, :], wu.ap[:, k * DFF + f * 128:k * DFF + (f + 1) * 128], uT.ap[:, k * 512:(k + 1) * 512],
                              k == 0, k == 7, [wu.b, uT.b], [PB[pu]])
                    sg_ = sg[f % 2]
                    kb.act(sg_.ap, PS[pg][:, :], AF.Silu, [PB[pg]], [sg_.b])
                    kb.tt("dve", actT.ap[:, f * 512:(f + 1) * 512], sg_.ap, PS[pu][:, :], ALU.mult, [sg_.b, PB[pu]], [actT.b])
                if cfg.get("dbg_ffn") and i == 0 and layer == 0 and which == 0:
                    da = nc.dram_tensor("dbg_actT", [128, NFC * 512], BF16, kind="ExternalOutput")
                    kb.S.op("sp", (lambda da=da, actT=actT: lambda e: e.dma_start(out=da[:, :], in_=actT.ap))(), [actT.b], [], dma=True)
                if nxt:
                    n_sq(i + 1)
                down(i, 0)
                down(i, 1)
                if nxt:
                    n_stats(i + 1)
                for c in range(2, 8):
                    down(i, c)
                    if nxt:
                        n_u(i + 1, c - 2)
                if nxt:
                    n_u(i + 1, 6)
                    n_u(i + 1, 7)

        def outproj_phase(layer, w_dram, s):
            S.fence()
            A.reset()
            wo = A.bf(8 * D, "wo")
            mark = A.off
            wo.b = load_w_bf(wo.ap, w_dram, 8 * D, "wo")
            A.off = mark
            S.fence()
            mx = [A.bf(8 * 512, "mx%d" % i) for i in range(2)]
            hg = [A.f32(8 * 512, "hgo%d" % i) for i in range(2)]
            ho = [A.f32(512, "hoo%d" % i) for i in range(2)]
            o = (s * 3 + 1) * 24
            mixd, mixb = scr["MIX"]
            for g in range(NG):
                m_ = mx[g % 2]
                h_ = hg[g % 2]
                kb.dma(m_.ap.rearrange("p (c t) -> p c t", c=8),
                       mixd.rearrange("(c p) t -> p c t", p=128)[:, :, g * 512:(g + 1) * 512], [mixb], [m_.b])
                kb.dma(h_.ap.rearrange("p (c t) -> p c t", c=8),
                       hT[s].rearrange("(c p) t -> p c t", p=128)[:, :, g * 512:(g + 1) * 512], list(hT_b[s][g]), [h_.b])
                for c in range(8):
                    pd = c % 2
                    for k in range(8):
                        kb.mm(PS[pd][:, :], wo.ap[:, k * D + c * 128:k * D + (c + 1) * 128], m_.ap[:, k * 512:(k + 1) * 512],
                              k == 0, k == 7, [wo.b, m_.b], [PB[pd]])
                    ho_ = ho[c % 2]
                    kb.stt("dve", ho_.ap, PS[pd][:, :], ABG[:, o + 16 + c:o + 17 + c], h_.ap[:, c * 512:(c + 1) * 512],
                           ALU.mult, ALU.add, [PB[pd], abg_b, h_.b], [ho_.b])
                    kb.dma(hT[s][c * 128:(c + 1) * 128, g * 512:(g + 1) * 512], ho_.ap, [ho_.b], [hT_b[s][g][c]])

        def rope_tables(s, col_scale, cosT, sinT):
            posi = A.f32(SQ, "posi")
            pi_ap = posi.ap.bitcast(I32)
            kb.dma(pi_ap, pos[s:s + 1, :].partition_broadcast(128), [], [posi.b])
            invf = smallf[:, 20:21]
            kb.act(invf, cst[:, col_scale:col_scale + 1], AF.Exp, [cst_b], [misc_b])
            ang = cosT
            kb.cp("dve", ang.ap, pi_ap, [posi.b], [ang.b])
            kb.ts("dve", ang.ap, ang.ap, invf, None, ALU.mult, None, [ang.b, misc_b], [ang.b])
            C1 = 6.28125
            C2 = float(2.0 * np.pi - 6.28125)
            ki = A.f32(SQ, "ki")
            kf = A.f32(SQ, "kf")

            def reduce_to(dst, shift):
                kb.ts("dve", kf.ap, ang.ap, shift, float(1.0 / (2.0 * np.pi)), ALU.add, ALU.mult, [ang.b], [kf.b])
                kb.cp("dve", ki.ap.bitcast(I32), kf.ap, [kf.b], [ki.b])
                kb.cp("dve", kf.ap, ki.ap.bitcast(I32), [ki.b], [kf.b])
                kb.stt("dve", dst.ap, kf.ap, -C1, ang.ap, ALU.mult, ALU.add, [kf.b, ang.b], [dst.b])
                kb.stt("dve", dst.ap, kf.ap, -C2, dst.ap, ALU.mult, ALU.add, [kf.b, dst.b], [dst.b])
                if shift != 0.0:
                    kb.ts("dve", dst.ap, dst.ap, shift, None, ALU.add, None, [dst.b], [dst.b])
                kb.ts("dve", kf.ap, dst.ap, float(np.pi), float(-2.0 * np.pi), ALU.is_gt, ALU.mult, [dst.b], [kf.b])
                kb.tt("dve", dst.ap, dst.ap, kf.ap, ALU.add, [dst.b, kf.b], [dst.b])
                kb.ts("dve", kf.ap, dst.ap, float(-np.pi), float(2.0 * np.pi), ALU.is_lt, ALU.mult, [dst.b], [kf.b])
                kb.tt("dve", dst.ap, dst.ap, kf.ap, ALU.add, [dst.b, kf.b], [dst.b])
                kb.ts("dve", dst.ap, dst.ap, float(np.pi), -float(np.pi), ALU.min, ALU.max, [dst.b], [dst.b])

            reduce_to(sinT, 0.0)
            tmpc = A.f32(SQ, "tmpc")
            reduce_to(tmpc, float(0.5 * np.pi))
            kb.cp("dve", cosT.ap, tmpc.ap, [tmpc.b], [cosT.b])
            kb.act(sinT.ap, sinT.ap, AF.Sin, [sinT.b], [sinT.b])
            kb.act(cosT.ap, cosT.ap, AF.Sin, [cosT.b], [cosT.b])

        def rope_block(y_ps, pb, b0, half, g, cosT, sinT, dst, t1, t2, eng="dve"):
            tk = slice(g * 512, (g + 1) * 512)
            r1 = slice(b0, b0 + half)
            r2 = slice(b0 + 32, b0 + 32 + half)
            kb.tt(eng, t1.ap[r1, :], y_ps[r2, :], sinT.ap[r2, tk], ALU.mult, [pb, sinT.b], [t1.b])
            kb.tt(eng, t2.ap[r2, :], y_ps[r1, :], sinT.ap[r1, tk], ALU.mult, [pb, sinT.b], [t2.b])
            kb.tt(eng, t2.ap[r1, :], y_ps[r1, :], cosT.ap[r1, tk], ALU.mult, [pb, cosT.b], [t2.b])
            kb.tt(eng, t1.ap[r2, :], y_ps[r2, :], cosT.ap[r2, tk], ALU.mult, [pb, cosT.b], [t1.b])
            kb.tt(eng, dst[r1, :], t2.ap[r1, :], t1.ap[r1, :], ALU.subtract, [t1.b, t2.b], [dstb_cur[0]])
            kb.tt(eng, dst[r2, :], t1.ap[r2, :], t2.ap[r2, :], ALU.add, [t1.b, t2.b], [dstb_cur[0]])

        dstb_cur = [None]

        def bound_update(sq_ap, sq_b, rows, col):
            kb.mm(PS[6][:, :], ones_bf[rows, :], sq_ap[rows, :], True, True, [sq_b, misc_b], [PB[6]])
            kb.S.op("dve", lambda e: e.tensor_reduce(smallf[:, 32:33], PS[6][:, :], AX.X, ALU.max), [PB[6]], [misc_b])
            kb.tt("dve", stat[:, col:col + 1], stat[:, col:col + 1], smallf[:, 32:33], ALU.max, [misc_b, stat_b], [stat_b])

        def finish_bounds(nh, scale):
            kb.tt("dve", negB[:, 0:nh], stat[:, 0:nh], stat[:, 16:16 + nh], ALU.mult, [stat_b], [negB_b])
            kb.act(negB[:, 0:nh], negB[:, 0:nh], AF.Sqrt, [negB_b], [negB_b], scale=scale * scale)
            kb.ts("dve", negB[:, 0:nh], negB[:, 0:nh], -1.0, None, ALU.mult, None, [negB_b], [negB_b])

        def hyb_proj_phase(layer, s):
            global A_sq, A_rs, A_tmp
            j = layer // 2
            S.fence()
            A.reset()
            wf = A.bf(8 * NHF, "wf")
            wt = A.bf(8 * NHT, "wt")
            mark = A.off
            wf.b = load_w_bf(wf.ap, hyb_fm[j], 8 * NHF, "wf")
            A.off = mark
            wt.b = load_w_bf(wt.ap, hyb_tm[j], 8 * NHT, "wt")
            A.off = mark
            kb.dma(fb_t[:], fox_b[j, :, :], [], [fb_b])
            kb.ts("dve", fb_t[:], fb_t[:], -1.0, None, ALU.mult, None, [fb_b], [fb_b])
            S.fence()
            cos16 = A.f32(SQ, "cos16")
            sin16 = A.f32(SQ, "sin16")
            mk2 = A.off
            rope_tables(s, 525, cos16, sin16)
            A.off = mk2
            S.fence()
            hg = A.f32(8 * 512, "hg")
            uT = A.bf(8 * 512, "uT")
            A_sq = A.bf(8 * 512, "sq")
            A_rs = A.f32(512, "rs")
            A_tmp = [A.f32(512, "tmp%d" % i) for i in range(2)]
            qst = [A.bf(512, "qst%d" % i) for i in range(2)]
            kst = [A.bf(512, "kst%d" % i) for i in range(2)]
            gst = [A.bf(512, "gst%d" % i) for i in range(3)]
            sqs = [A.bf(512, "sqs%d" % i) for i in range(2)]
            t1 = A.f32(512, "t1")
            t2 = A.f32(512, "t2")
            ge = A.f32(512, "ge")
            gc = A.f32(512, "gc")
            gh = A.f32(512, "gh")
            ghb = A.bf(512, "ghb")
            vst = [A.bf(1024, "vstf"), A.bf(512, "vstd")]
            kb.memset("pool", vst[0].ap[:, :], 1.0, [vst[0].b])
            carry = smallf[:, 34:42]
            kb.memset("pool", carry, 0.0, [misc_b])
            kb.memset("pool", stat[:, 0:32], 0.0, [stat_b])
            for q_ in qst:
                kb.memset("pool", q_.ap[:, :], 0.0, [q_.b])
                kb.memset("pool", q_.ap[96:98, :], 1.0, [q_.b])
            for k_ in kst:
                kb.memset("pool", k_.ap[:, :], 0.0, [k_.b])
                kb.memset("pool", k_.ap[64:66, :], 1.0, [k_.b])
            hsrc, hsrc_b = hT[s][:, :], hT_b[s]
            it = 0
            for g in range(NG):
                tk = slice(g * 512, (g + 1) * 512)
                norm_group(hsrc, hsrc_b, s, 1, g, hg, uT)

                def proj_fm(chunk, pbank):
                    for k in range(8):
                        kb.mm(PS[pbank][:, :], wf.ap[:, k * NHF + chunk * 128:k * NHF + (chunk + 1) * 128],
                              uT.ap[:, k * 512:(k + 1) * 512], k == 0, k == 7, [wf.b, uT.b], [PB[pbank]])

                for h in range(8):
                    pq = (h % 2) * 2
                    pk = (h % 2) * 2 + 1
                    proj_fm(h, pq)
                    proj_fm(8 + h, pk)
                    q_ = qst[h % 2]
                    k_ = kst[h % 2]
                    sq_ = sqs[h % 2]
                    kb.cp("act", q_.ap[0:64, :], PS[pq][0:64, :], [PB[pq]], [q_.b])
                    kb.act(sq_.ap[0:64, :], PS[pq][0:64, :], AF.Square, [PB[pq]], [sq_.b])
                    bound_update(sq_.ap, sq_.b, slice(0, 64), h)
                    kb.cp("act", k_.ap[0:64, :], PS[pk][0:64, :], [PB[pk]], [k_.b])
                    kb.act(sq_.ap[64:128, :], PS[pk][0:64, :], AF.Square, [PB[pk]], [sq_.b])
                    bound_update(sq_.ap, sq_.b, slice(64, 128), 16 + h)
                    R = slice(64, 66)
                    kb.act(ge.ap[R, :], PS[pq][R, :], AF.Exp, [PB[pq], fb_b], [ge.b], bias=fb_t[R, h:h + 1], scale=-1.0)
                    kb.act(ge.ap[R, :], ge.ap[R, :], AF.Ln, [ge.b], [ge.b], bias=1.0, scale=1.0)
                    kb.ts("dve", ge.ap[R, :], ge.ap[R, :], -8.0, None, ALU.mult, None, [ge.b], [ge.b])
                    kb.S.op("dve", (lambda R=R, h=h: lambda e: e.tensor_tensor_scan(gc.ap[R, :], ge.ap[R, :], zeros_f[R, :], carry[R, h:h + 1], ALU.add, ALU.add))(),
                            [ge.b, misc_b], [gc.b])
                    kb.cp("dve", carry[R, h:h + 1], gc.ap[R, 511:512], [gc.b], [misc_b])
                    kb.cp("dve", ghb.ap[R, :], gc.ap[R, :], [gc.b], [ghb.b])
                    kb.cp("dve", gh.ap[R, :], ghb.ap[R, :], [ghb.b], [gh.b])
                    kb.stt("dve", gh.ap[R, :], gh.ap[R, :], cst[R, 527:528], gc.ap[R, :], ALU.mult, ALU.add, [gh.b, gc.b, cst_b], [gh.b])
                    kb.cp("dve", q_.ap[R, :], gh.ap[R, :], [gh.b], [q_.b])
                    kb.S.op("act", (lambda q_=q_, k_=k_: lambda e: e.mul(k_.ap[96:98, :], q_.ap[64:66, :], -1.0))(), [q_.b], [k_.b])
                    kb.dma(scr["QA%d" % h][0][:, tk], q_.ap, [q_.b], [scr["QA%d" % h][1]])
                    kb.dma(scr["KA%d" % h][0][:, tk], k_.ap, [k_.b], [scr["KA%d" % h][1]])
                fm_list = [("DQ%d" % c, 16 + c, 32 + 2 * c) for c in range(4)] + [("DK%d" % c, 20 + c, 48 + 2 * c) for c in range(4)] + \
                          [("IQ%d" % c, 24 + c, None) for c in range(2)] + [("IK", 26, None)]
                for n_i, (nm, chunk, scol) in enumerate(fm_list):
                    pbk = 4 + (n_i % 2)
                    proj_fm(chunk, pbk)
                    d_ = gst[n_i % 3]
                    kb.cp("act", d_.ap[:, :], PS[pbk][:, :], [PB[pbk]], [d_.b])
                    dstb_cur[0] = d_.b
                    for b0 in (0, 64):
                        rope_block(PS[pbk], PB[pbk], b0, 8, g, cos16, sin16, d_.ap, t1, t2)
                    if scol is not None:
                        sq_ = sqs[n_i % 2]
                        kb.act(sq_.ap[:, :], d_.ap[:, :], AF.Square, [d_.b], [sq_.b])
                        hb = (chunk - 16) * 2 if chunk < 20 else (chunk - 20) * 2
                        base = 8 if chunk < 20 else 24
                        bound_update(sq_.ap, sq_.b, slice(0, 64), base + hb)
                        bound_update(sq_.ap, sq_.b, slice(64, 128), base + hb + 1)
                    kb.dma(scr[nm][0][:, tk], d_.ap, [d_.b], [scr[nm][1]])
                for tt_ in range(4):
                    tok = slice(tt_ * 128, (tt_ + 1) * 128)
                    gt = g * 4 + tt_
                    for vi, nm in enumerate(("VF", "DV")):
                        pbk = 4 + vi
                        for k in range(8):
                            kb.mm(PS[pbk][:, :], uT.ap[:, k * 512 + tt_ * 128:k * 512 + (tt_ + 1) * 128],
                                  wt.ap[:, k * NHT + vi * 512:k * NHT + (vi + 1) * 512], k == 0, k == 7, [wt.b, uT.b], [PB[pbk]])
                        v_ = vst[vi]
                        if vi == 0:
                            kb.cp("act", v_.ap.rearrange("p (h e) -> p h e", e=128)[:, :, 0:64],
                                  PS[pbk][:, :].rearrange("p (h d) -> p h d", d=64), [PB[pbk]], [v_.b])
                        else:
                            kb.cp("act", v_.ap[:, :], PS[pbk][:, :], [PB[pbk]], [v_.b])
                        kb.dma(scr[nm][0][gt * 128:(gt + 1) * 128, :], v_.ap, [v_.b], [scr[nm][1]])
                    for k in range(8):
                        kb.mm(PS[0][:, 0:4], uT.ap[:, k * 512 + tt_ * 128:k * 512 + (tt_ + 1) * 128],
                              wt.ap[:, k * NHT + 1024:k * NHT + 1028], k == 0, k == 7, [wt.b, uT.b], [PB[0]])
                    kb.ts("dve", iw_all[:, gt * 4:(gt + 1) * 4], PS[0][:, 0:4], 1.0 / 16.0, None, ALU.mult, None, [PB[0]], [iw_b])
            finish_bounds(16, 0.125)
            kb.act(iwabs[:, :], iw_all[:, :], AF.Abs, [iw_b], [iw_b])
            kb.act(iwsgn[:, :], iw_all[:, :], AF.Sign, [iw_b], [iw_b])

        def attn_head(j_blocks, Kt, Kb_, Qt, Qb_, K2, Q2, Vfn, mask_tiles, mask_b, dsa_masks, scale, negcol, dv, out_rows, PT, ost, orec):
            mixd, mixb = scr["MIX"]
            for jb in j_blocks:
                qs = slice(jb * 512, (jb + 1) * 512)
                nkt = 4 * jb + 4
                po = 3 + (jb % 2)
                pd = 5 + (jb % 2)
                pend = None
                for kt in range(nkt + 1):
                    if kt < nkt:
                        ps_ = kt % 3
                        ks = slice(kt * 128, (kt + 1) * 128)
                        diag = kt >= 4 * jb
                        last_plain = (K2 is None) and not (diag and mask_tiles is not None) and dsa_masks is None
                        kb.mm(PS[ps_][:, :], Kt[:, ks], Qt[:, qs], True, last_plain, [Kb_, Qb_], [PB[ps_]])
                        if K2 is not None:
                            kb.mm(PS[ps_][:, :], K2[:, ks], Q2[:, qs], False, not (diag and mask_tiles is not None), [Kb_, Qb_], [PB[ps_]])
                        if diag and mask_tiles is not None:
                            a = kt - 4 * jb
                            kb.mm(PS[ps_][:, :], ident_bf[:, :], mask_tiles[:, a * 512:(a + 1) * 512], False, True, [misc_b, mask_b], [PB[ps_]])
                        if dsa_masks is not None:
                            for a in range(4):
                                mb_ = dsa_masks[a]
                                kb.mm(PS[ps_][:, a * 128:(a + 1) * 128], mb_.ap[:, ks], ident_bf[:, :], False, a == 3, [mb_.b, misc_b], [PB[ps_]])
                        pt_ = PT[kt % 3]
                        kb.act(pt_.ap, PS[ps_][:, :], AF.Exp, [PB[ps_], negB_b], [pt_.b], bias=negB[:, negcol:negcol + 1], scale=scale)
                    if pend is not None:
                        pk_, ppt = pend
                        kb.mm(PS[po][:, :], Vfn(pk_), ppt.ap, pk_ == 0, pk_ == nkt - 1, [ppt.b, Vb_cur[0]], [PB[po]])
                    pend = (kt, PT[kt % 3]) if kt < nkt else None
                rc = orec[jb % 2]
                os_ = ost[jb % 2]
                kb.recip(rc.ap[0:dv, :], PS[po][64:128, :], [PB[po]], [rc.b])
                kb.tt("dve", os_.ap[0:dv, :], PS[po][0:dv, :], rc.ap[0:dv, :], ALU.mult, [PB[po], rc.b], [os_.b])
                kb.dma(mixd[out_rows, qs], os_.ap[0:dv, :], [os_.b], [mixb])

        Vb_cur = [None]

        def fox_attn_phase(s):
            S.fence()
            A.reset()
            vall = A.bf(NKT * 1024, "vall")
            vd, vb = scr["VF"]
            kb.dma(vall.ap.rearrange("p (t d) -> p t d", d=1024), vd.rearrange("(t p) d -> p t d", p=128), [vb], [vall.b])
            Vb_cur[0] = vall.b
            cm = build_mask(0)
            KA = [A.bf(SQ, "KA%d" % i) for i in range(2)]
            QA = [A.bf(SQ, "QA%d" % i) for i in range(2)]
            PT = [A.bf(512, "PT%d" % i) for i in range(3)]
            ost = [A.bf(512, "ost%d" % i) for i in range(2)]
            orec = [A.f32(512, "orec%d" % i) for i in range(2)]
            for h in range(8):
                k_ = KA[h % 2]
                q_ = QA[h % 2]
                kb.dma(k_.ap, scr["KA%d" % h][0][:, :], [scr["KA%d" % h][1]], [k_.b])
                kb.dma(q_.ap, scr["QA%d" % h][0][:, :], [scr["QA%d" % h][1]], [q_.b])
                vv = vall.ap.rearrange("p (t d) -> p t d", d=1024)
                attn_head(range(NG), k_.ap, k_.b, q_.ap, q_.b, None, None,
                          (lambda h: lambda kt: vv[:, kt, h * 128:(h + 1) * 128])(h),
                          cm.ap, cm.b, None, 0.125, h, 64, slice(h * 64, (h + 1) * 64), PT, ost, orec)

        def dsa_phase(s):
            S.fence()
            A.reset()
            vall = A.bf(NKT * 512, "dvall")
            vd, vb = scr["DV"]
            kb.dma(vall.ap.rearrange("p (t d) -> p t d", d=512), vd.rearrange("(t p) d -> p t d", p=128), [vb], [vall.b])
            Vb_cur[0] = vall.b
            vv = vall.ap.rearrange("p (t d) -> p t d", d=512)
            DKs = []
            for c in range(4):
                t_ = A.bf(SQ, "DK%d" % c)
                kb.dma(t_.ap, scr["DK%d" % c][0][:, :], [scr["DK%d" % c][1]], [t_.b])
                DKs.append(t_)
            IK = A.bf(SQ, "IK")
            kb.dma(IK.ap, scr["IK"][0][:, :], [scr["IK"][1]], [IK.b])
            DQb = [[A.bf(512, "DQ%d_%d" % (c, i)) for c in range(4)] for i in range(2)]
            IQb = [[A.bf(512, "IQ%d_%d" % (c, i)) for c in range(2)] for i in range(2)]
            Sc = A.f32(SQ, "Sc")
            ccmq = build_mask(2)
            junk = A.bf(SQ, "junk")
            maskb = [[A.bf(SQ, "maskb%d_%d" % (i, a)) for a in range(4)] for i in range(2)]
            rr_ = [A.f32(512, "rr%d" % i) for i in range(2)]
            PT = [A.bf(512, "PT%d" % i) for i in range(3)]
            ost = [A.bf(512, "ost%d" % i) for i in range(2)]
            orec = [A.f32(512, "orec%d" % i) for i in range(2)]
            osb = [A.f32(512, "osb%d" % i) for i in range(2)]
            bs = smallf[:, 44:60]
            IBANK = (4, 6, 7)
            ictr = [0]

            def load_block(jb):
                qs = slice(jb * 512, (jb + 1) * 512)
                for c in range(4):
                    kb.dma(DQb[jb % 2][c].ap, scr["DQ%d" % c][0][:, qs], [scr["DQ%d" % c][1]], [DQb[jb % 2][c].b])
                for c in range(2):
                    kb.dma(IQb[jb % 2][c].ap, scr["IQ%d" % c][0][:, qs], [scr["IQ%d" % c][1]], [IQb[jb % 2][c].b])

            def idx_tile(jb, a):
                iq = IQb[jb % 2]
                nk = 512 * (jb + 1)
                qt = jb * 4 + a
                mb_out = maskb[jb % 2][a]
                for kbk in range(jb + 1):
                    kk = slice(kbk * 512, (kbk + 1) * 512)
                    for ih in range(4):
                        rows = slice((ih % 2) * 64, (ih % 2) * 64 + 64)
                        bank = IBANK[ictr[0] % 3]
                        ictr[0] += 1
                        kb.mm(PS[bank][:, :], iq[ih // 2].ap[rows, a * 128:(a + 1) * 128], IK.ap[rows, kk], True, True,
                              [iq[ih // 2].b, IK.b], [PB[bank]])
                        r_ = rr_[ih % 2]
                        kb.act(r_.ap, PS[bank][:, :], AF.Relu, [PB[bank], iw_b], [r_.b], scale=iwabs[:, qt * 4 + ih:qt * 4 + ih + 1])
                        if ih == 0:
                            kb.ts("dve", Sc.ap[:, kk], r_.ap, iwsgn[:, qt * 4:qt * 4 + 1], None, ALU.mult, None, [r_.b, iw_b], [Sc.b])
                        else:
                            kb.stt("dve", Sc.ap[:, kk], r_.ap, iwsgn[:, qt * 4 + ih:qt * 4 + ih + 1], Sc.ap[:, kk], ALU.mult, ALU.add,
                                   [r_.b, iw_b, Sc.b], [Sc.b])
                lo = bs[:, 0:1]
                hi = bs[:, 1:2]
                mid = bs[:, 2:3]
                cnt = bs[:, 3:4]
                cond = bs[:, 4:5]
                dd = bs[:, 5:6]
                bsb = Buf("bs")
                if qt >= 2:
                    kb.S.op("dve", (lambda nk=nk: lambda e: e.tensor_reduce(hi, Sc.ap[:, 0:nk], AX.X, ALU.max))(), [Sc.b], [bsb])
                    kb.S.op("dve", (lambda nk=nk: lambda e: e.tensor_reduce(lo, Sc.ap[:, 0:nk], AX.X, ALU.min))(), [Sc.b], [bsb])
                    kb.tt("dve", dd, hi, lo, ALU.subtract, [bsb], [bsb])
                    kb.ts("dve", dd, dd, 1.001, 1e-6, ALU.mult, ALU.add, [bsb], [bsb])
                dk_ = slice(jb * 512, (jb + 1) * 512)
                kb.tt("dve", Sc.ap[:, dk_], Sc.ap[:, dk_], ccmq.ap[:, a * 512:(a + 1) * 512], ALU.add, [Sc.b, ccmq.b], [Sc.b])
                if qt >= 2:
                    for itn in range(13):
                        kb.ts("dve", dd, dd, 0.5, None, ALU.mult, None, [bsb], [bsb])
                        kb.tt("dve", mid, lo, dd, ALU.add, [bsb], [bsb])
                        kb.ts("dve", junk.ap[:, 0:nk], Sc.ap[:, 0:nk], mid, 0.0, ALU.is_ge, ALU.add, [Sc.b, bsb], [junk.b, bsb], accum_out=cnt)
                        kb.stt("dve", cond, cnt, 255.5, dd, ALU.is_ge, ALU.mult, [bsb], [bsb])
                        kb.tt("dve", lo, lo, cond, ALU.add, [bsb], [bsb])
                else:
                    kb.memset("dve", lo, -1e29, [bsb])
                kb.ts("dve", mb_out.ap[:, 0:nk], Sc.ap[:, 0:nk], lo, NEGM, ALU.is_lt, ALU.mult, [Sc.b, bsb], [mb_out.b])

            def attn_head_dsa(jb, h):
                c = h // 2
                rows = slice((h % 2) * 64, (h % 2) * 64 + 64)
                DKc = DKs[c]
                dqc = DQb[jb % 2][c]
                mbs = maskb[jb % 2]
                mixd, mixb = scr["MIX"]
                nkt = 4 * jb + 4
                po = 3
                pd = 5
                pend = None
                qs = slice(jb * 512, (jb + 1) * 512)
                for kt in range(nkt + 1):
                    if kt < nkt:
                        ps_ = kt % 3
                        ks = slice(kt * 128, (kt + 1) * 128)
                        kb.mm(PS[ps_][:, :], DKc.ap[rows, ks], dqc.ap[rows, :], True, False, [DKc.b, dqc.b], [PB[ps_]])
                        for a in range(4):
                            mb_ = mbs[a]
                            kb.mm(PS[ps_][:, a * 128:(a + 1) * 128], mb_.ap[:, ks], ident_bf[:, :], False, a == 3, [mb_.b, misc_b], [PB[ps_]])
                        pt_ = PT[kt % 3]
                        kb.act(pt_.ap, PS[ps_][:, :], AF.Exp, [PB[ps_], negB_b], [pt_.b], bias=negB[:, 8 + h:9 + h], scale=0.125)
                    if pend is not None:
                        pk_, ppt = pend
                        kb.mm(PS[po][0:64, :], vv[:, pk_, h * 64:(h + 1) * 64], ppt.ap, pk_ == 0, pk_ == nkt - 1, [ppt.b, Vb_cur[0]], [PB[po]])
                        kb.mm(PS[pd][0:64, :], ones_bf[:, 0:64], ppt.ap, pk_ == 0, pk_ == nkt - 1, [ppt.b, misc_b], [PB[pd]])
                    pend = (kt, PT[kt % 3]) if kt < nkt else None
                rc = orec[h % 2]
                os_ = ost[h % 2]
                ob_ = osb[h % 2]
                kb.act(rc.ap[0:64, :], PS[pd][0:64, :], AF.Ln, [PB[pd]], [rc.b])
                kb.act(rc.ap[0:64, :], rc.ap[0:64, :], AF.Exp, [rc.b], [rc.b], scale=-1.0)
                kb.cp("act", ob_.ap[0:64, :], PS[po][0:64, :], [PB[po]], [ob_.b])
                kb.tt("pool", os_.ap[0:64, :], ob_.ap[0:64, :], rc.ap[0:64, :], ALU.mult, [ob_.b, rc.b], [os_.b])
                kb.dma(mixd[512 + h * 64:512 + (h + 1) * 64, qs], os_.ap[0:64, :], [os_.b], [mixb])

            load_block(0)
            for a in range(4):
                idx_tile(0, a)
            for jb in range(NG):
                if jb + 1 < NG:
                    load_block(jb + 1)
                for a in range(4):
                    if jb + 1 < NG:
                        idx_tile(jb + 1, a)
                    attn_head_dsa(jb, 2 * a)
                    attn_head_dsa(jb, 2 * a + 1)

        def mla_proj_phase(layer, s):
            global A_sq, A_rs, A_tmp
            j = layer // 2
            S.fence()
            A.reset()
            wdn = A.bf(8 * 768, "wdn")
            wuq = A.bf(3 * 1536, "wuq")
            wukv = A.bf(2 * 2048, "wukv")
            mark = A.off
            wdn.b = load_w_bf(wdn.ap, mla_down[j], 8 * 768, "wdn")
            A.off = mark
            wuq.b = load_w_bf(wuq.ap, mla_uq[j], 3 * 1536, "wuq")
            A.off = mark
            wukv.b = load_w_bf(wukv.ap, mla_ukv[j], 2 * 2048, "wukv")
            A.off = mark
            kb.dma(qn_t[:, 0:3], mla_qn[j, :, :], [], [qn_b])
            kb.dma(qn_t[:, 3:5], mla_kvn[j, :, :], [], [qn_b])
            S.fence()
            cos64 = A.f32(SQ, "cos64")
            sin64 = A.f32(SQ, "sin64")
            mk2 = A.off
            rope_tables(s, 526, cos64, sin64)
            A.off = mk2
            S.fence()
            hg = A.f32(8 * 512, "hg")
            uT = A.bf(8 * 512, "uT")
            A_sq = A.bf(8 * 512, "sq")
            A_rs = A.f32(512, "rs")
            A_tmp = [A.f32(512, "tmp%d" % i) for i in range(2)]
            cl = A.f32(6 * 512, "cl")
            cn = A.bf(5 * 512, "cn")
            sq2 = A.bf(5 * 512, "sq2")
            rs2 = A.f32(1024, "rs2")
            gst = [A.bf(512, "gst%d" % i) for i in range(3)]
            sqs = [A.bf(512, "sqs%d" % i) for i in range(2)]
            t1 = A.f32(512, "t1")
            t2 = A.f32(512, "t2")
            vst = [A.bf(512, "vst%d" % i) for i in range(2)]
            kb.memset("pool", stat[:, 0:32], 0.0, [stat_b])
            epsq = smallf[:, 2:3]
            epsk = smallf[:, 3:4]
            kb.memset("pool", epsq, 384.0 * 1e-6, [misc_b])
            kb.memset("pool", epsk, 256.0 * 1e-6, [misc_b])
            kb.ts("dve", qn_t[:, 0:3], qn_t[:, 0:3], float(np.sqrt(384.0)), None, ALU.mult, None, [qn_b], [qn_b])
            kb.ts("dve", qn_t[:, 3:5], qn_t[:, 3:5], 16.0, None, ALU.mult, None, [qn_b], [qn_b])
            hsrc, hsrc_b = hT[s][:, :], hT_b[s]
            for g in range(NG):
                tk = slice(g * 512, (g + 1) * 512)
                norm_group(hsrc, hsrc_b, s, 1, g, hg, uT)
                for c in range(6):
                    pbk = c % 2
                    for k in range(8):
                        kb.mm(PS[pbk][:, :], wdn.ap[:, k * 768 + c * 128:k * 768 + (c + 1) * 128], uT.ap[:, k * 512:(k + 1) * 512],
                              k == 0, k == 7, [wdn.b, uT.b], [PB[pbk]])
                    if c < 5:
                        kb.cp("act", cl.ap[:, c * 512:(c + 1) * 512], PS[pbk][:, :], [PB[pbk]], [cl.b])
                        kb.act(sq2.ap[:, c * 512:(c + 1) * 512], PS[pbk][:, :], AF.Square, [PB[pbk]], [sq2.b])
                    else:
                        d_ = gst[0]
                        dstb_cur[0] = d_.b
                        rope_block(PS[pbk], PB[pbk], 0, 32, g, cos64, sin64, d_.ap, t1, t2)
                        kb.act(sqs[0].ap[0:64, :], d_.ap[0:64, :], AF.Square, [d_.b], [sqs[0].b])
                        kb.mm(PS[2][:, :], ones_bf[0:64, :], sqs[0].ap[0:64, :], True, True, [sqs[0].b, misc_b], [PB[2]])
                        kb.cp("dve", rs2.ap[:, 512:1024], PS[2][:, :], [PB[2]], [rs2.b])
                        kb.dma(scr["KR"][0][:, tk], d_.ap[0:64, :], [d_.b], [scr["KR"][1]])
                for (c0, c1, epsc, col) in ((0, 3, epsq, 0), (3, 5, epsk, 1)):
                    for c in range(c0, c1):
                        kb.mm(PS[3][:, :], ones_bf[:, :], sq2.ap[:, c * 512:(c + 1) * 512], c == c0, c == c1 - 1, [sq2.b, misc_b], [PB[3]])
                    kb.act(A_rs.ap, PS[3][:, :], AF.Sqrt, [PB[3]], [A_rs.b], bias=epsc, scale=1.0)
                    kb.recip(A_rs.ap, A_rs.ap, [A_rs.b], [A_rs.b])
                    for c in range(c0, c1):
                        kb.stt("dve", cn.ap[:, c * 512:(c + 1) * 512], cl.ap[:, c * 512:(c + 1) * 512], qn_t[:, c:c + 1], A_rs.ap,
                               ALU.mult, ALU.mult, [cl.b, A_rs.b, qn_b], [cn.b])
                for h in range(8):
                    pn = (h % 2) * 2
                    pr = (h % 2) * 2 + 1
                    for k in range(3):
                        kb.mm(PS[pn][:, :], wuq.ap[:, k * 1536 + h * 192:k * 1536 + h * 192 + 128], cn.ap[:, k * 512:(k + 1) * 512],
                              k == 0, k == 2, [wuq.b, cn.b], [PB[pn]])
                    for k in range(3):
                        kb.mm(PS[pr][0:64, :], wuq.ap[:, k * 1536 + h * 192 + 128:k * 1536 + h * 192 + 192], cn.ap[:, k * 512:(k + 1) * 512],
                              k == 0, k == 2, [wuq.b, cn.b], [PB[pr]])
                    dn = gst[1]
                    dr = gst[2]
                    kb.cp("act", dn.ap[:, :], PS[pn][:, :], [PB[pn]], [dn.b])
                    dstb_cur[0] = dr.b
                    rope_block(PS[pr], PB[pr], 0, 32, g, cos64, sin64, dr.ap, t1, t2)
                    kb.act(sqs[0].ap[:, :], dn.ap[:, :], AF.Square, [dn.b], [sqs[0].b])
                    kb.act(sqs[1].ap[0:64, :], dr.ap[0:64, :], AF.Square, [dr.b], [sqs[1].b])
                    kb.mm(PS[6][:, :], ones_bf[:, :], sqs[0].ap[:, :], True, False, [sqs[0].b, misc_b], [PB[6]])
                    kb.mm(PS[6][:, :], ones_bf[0:64, :], sqs[1].ap[0:64, :], False, True, [sqs[1].b, misc_b], [PB[6]])
                    kb.S.op("dve", lambda e: e.tensor_reduce(smallf[:, 32:33], PS[6][:, :], AX.X, ALU.max), [PB[6]], [misc_b])
                    kb.tt("dve", stat[:, h:h + 1], stat[:, h:h + 1], smallf[:, 32:33], ALU.max, [misc_b, stat_b], [stat_b])
                    kb.dma(scr["QN%d" % h][0][:, tk], dn.ap, [dn.b], [scr["QN%d" % h][1]])
                    kb.dma(scr["QR%d" % h][0][:, tk], dr.ap[0:64, :], [dr.b], [scr["QR%d" % h][1]])
                for h in range(8):
                    pn = 4 + (h % 2)
                    for k in range(2):
                        kb.mm(PS[pn][:, :], wukv.ap[:, k * 2048 + h * 128:k * 2048 + (h + 1) * 128], cn.ap[:, (3 + k) * 512:(4 + k) * 512],
                              k == 0, k == 1, [wukv.b, cn.b], [PB[pn]])
                    dn = gst[h % 2]
                    kb.cp("act", dn.ap[:, :], PS[pn][:, :], [PB[pn]], [dn.b])
                    kb.act(sqs[h % 2].ap[:, :], PS[pn][:, :], AF.Square, [PB[pn]], [sqs[h % 2].b])
                    kb.mm(PS[6][:, :], ones_bf[:, :], sqs[h % 2].ap[:, :], True, True, [sqs[h % 2].b, misc_b], [PB[6]])
                    kb.tt("dve", t1.ap, PS[6][:, :], rs2.ap[:, 512:1024], ALU.add, [PB[6], rs2.b], [t1.b])
                    kb.S.op("dve", lambda e: e.tensor_reduce(smallf[:, 32:33], t1.ap, AX.X, ALU.max), [t1.b], [misc_b])
                    kb.tt("dve", stat[:, 16 + h:17 + h], stat[:, 16 + h:17 + h], smallf[:, 32:33], ALU.max, [misc_b, stat_b], [stat_b])
                    kb.dma(scr["KN%d" % h][0][:, tk], dn.ap, [dn.b], [scr["KN%d" % h][1]])
                for tt_ in range(4):
                    gt = g * 4 + tt_
                    for half in range(2):
                        pbk = 2 + half
                        for k in range(2):
                            kb.mm(PS[pbk][:, :], cn.ap[:, (3 + k) * 512 + tt_ * 128:(3 + k) * 512 + (tt_ + 1) * 128],
                                  wukv.ap[:, k * 2048 + 1024 + half * 512:k * 2048 + 1024 + (half + 1) * 512], k == 0, k == 1,
                                  [wukv.b, cn.b], [PB[pbk]])
                        v_ = vst[half]
                        kb.cp("act", v_.ap[:, :], PS[pbk][:, :], [PB[pbk]], [v_.b])
                        kb.dma(scr["MV"][0][gt * 128:(gt + 1) * 128, half * 512:(half + 1) * 512], v_.ap, [v_.b], [scr["MV"][1]])
            finish_bounds(8, float(192.0 ** -0.5))

        def mla_attn_phase(s):
            S.fence()
            A.reset()
            vall = A.bf(NKT * 1024, "mvall")
            vd, vb = scr["MV"]
            kb.dma(vall.ap.rearrange("p (t d) -> p t d", d=1024), vd.rearrange("(t p) d -> p t d", p=128), [vb], [vall.b])
            Vb_cur[0] = vall.b
            vv = vall.ap.rearrange("p (t d) -> p t d", d=1024)
            KR = A.bf(SQ, "KR")
            ccm = build_mask(1)
            kb.dma(KR.ap[0:64, :], scr["KR"][0][:, :], [scr["KR"][1]], [KR.b])
            KN = [A.bf(SQ, "KN%d" % i) for i in range(2)]
            QN = [A.bf(SQ, "QN%d" % i) for i in range(2)]
            QR = [A.bf(SQ, "QR%d" % i) for i in range(2)]
            PT = [A.bf(512, "PT%d" % i) for i in range(3)]
            ost = [A.bf(512, "ost%d" % i) for i in range(2)]
            orec = [A.f32(512, "orec%d" % i) for i in range(2)]
            sc = float(192.0 ** -0.5)
            for h in range(8):
                kn = KN[h % 2]
                qn = QN[h % 2]
                qr = QR[h % 2]
                kb.dma(kn.ap, scr["KN%d" % h][0][:, :], [scr["KN%d" % h][1]], [kn.b])
                kb.dma(qn.ap, scr["QN%d" % h][0][:, :], [scr["QN%d" % h][1]], [qn.b])
                kb.dma(qr.ap[0:64, :], scr["QR%d" % h][0][:, :], [scr["QR%d" % h][1]], [qr.b])
                kq_b = Buf("kq")
                attn_head_mla(h, kn, qn, KR, qr, vv, PT, ost, orec, sc, ccm)

        def attn_head_mla(h, kn, qn, KR, qr, vv, PT, ost, orec, sc, ccm):
            mixd, mixb = scr["MIX"]
            for jb in range(NG):
                qs = slice(jb * 512, (jb + 1) * 512)
                nkt = 4 * jb + 4
                po = 3 + (jb % 2)
                pd = 5 + (jb % 2)
                pend = None
                for kt in range(nkt + 1):
                    if kt < nkt:
                        ps_ = kt % 3
                        ks = slice(kt * 128, (kt + 1) * 128)
                        diag = kt >= 4 * jb
                        kb.mm(PS[ps_][:, :], kn.ap[:, ks], qn.ap[:, qs], True, False, [kn.b, qn.b], [PB[ps_]])
                        kb.mm(PS[ps_][:, :], KR.ap[0:64, ks], qr.ap[0:64, qs], False, not diag, [KR.b, qr.b], [PB[ps_]])
                        if diag:
                            a = kt - 4 * jb
                            kb.mm(PS[ps_][:, :], ident_bf[:, :], ccm.ap[:, a * 512:(a + 1) * 512], False, True, [misc_b, ccm.b], [PB[ps_]])
                        pt_ = PT[kt % 3]
                        kb.act(pt_.ap, PS[ps_][:, :], AF.Exp, [PB[ps_], negB_b], [pt_.b], bias=negB[:, h:h + 1], scale=sc)
                    if pend is not None:
                        pk_, ppt = pend
                        kb.mm(PS[po][:, :], vv[:, pk_, h * 128:(h + 1) * 128], ppt.ap, pk_ == 0, pk_ == nkt - 1, [ppt.b, Vb_cur[0]], [PB[po]])
                        kb.mm(PS[pd][:, :], ones_bf[:, :], ppt.ap, pk_ == 0, pk_ == nkt - 1, [ppt.b, misc_b], [PB[pd]])
                    pend = (kt, PT[kt % 3]) if kt < nkt else None
                rc = orec[jb % 2]
                os_ = ost[jb % 2]
                kb.recip(rc.ap, PS[pd][:, :], [PB[pd]], [rc.b])
                kb.tt("dve", os_.ap, PS[po][:, :], rc.ap, ALU.mult, [PB[po], rc.b], [os_.b])
                kb.dma(mixd[h * 128:(h + 1) * 128, qs], os_.ap, [os_.b], [mixb])

        def final_phase():
            global A_sq, A_rs, A_tmp
            S.fence()
            A.reset()
            hg = A.f32(8 * 512, "hg")
            uo = [A.f32(8 * 512, "uo%d" % i) for i in range(2)]
            A_sq = A.bf(8 * 512, "sq")
            A_rs = A.f32(512, "rs")
            A_tmp = [A.f32(512, "tmp%d" % i) for i in range(2)]
            it = 0
            for s in range(NSEQ):
                for g in range(NG):
                    u_ = uo[it % 2]
                    it += 1
                    norm_group(hT[s][:, :], hT_b[s], s, 0, g, hg, u_, final=True)
                    dst = outT[s].rearrange("(c p) t -> p c t", p=128)[:, :, g * 512:(g + 1) * 512]
                    S.out_dma.append(kb.S.op("sp", (lambda dst=dst, u_=u_: lambda e: e.dma_start(out=dst, in_=u_.ap.rearrange("p (c t) -> p c t", c=8)))(),
                                             [u_.b], [], dma=True))

        steps = []
        for layer in range(DEPTH):
            steps.append(("mod%d" % layer, (lambda layer=layer: (compute_mod(layer), compute_abg(layer)))))
            steps.append(("ffn%d_0" % layer, (lambda layer=layer: ffn_phase(layer, 0))))
            for s in range(NSEQ):
                if layer % 2 == 0:
                    steps.append(("hproj%d_%d" % (layer, s), (lambda layer=layer, s=s: hyb_proj_phase(layer, s))))
                    steps.append(("fox%d_%d" % (layer, s), (lambda layer=layer, s=s: fox_attn_phase(s))))
                    steps.append(("dsa%d_%d" % (layer, s), (lambda layer=layer, s=s: dsa_phase(s))))
                    steps.append(("oproj%d_%d" % (layer, s), (lambda layer=layer, s=s: outproj_phase(layer, hyb_out[layer // 2], s))))
                else:
                    steps.append(("mproj%d_%d" % (layer, s), (lambda layer=layer, s=s: mla_proj_phase(layer, s))))
                    steps.append(("mattn%d_%d" % (layer, s), (lambda layer=layer, s=s: mla_attn_phase(s))))
                    steps.append(("oproj%d_%d" % (layer, s), (lambda layer=layer, s=s: outproj_phase(layer, mla_out[layer // 2], s))))
            steps.append(("ffn%d_1" % layer, (lambda layer=layer: ffn_phase(layer, 1))))
        steps.append(("final", final_phase))
        for name, fn in steps:
            fn()
            if cfg.get("stop") == name:
                break
        if cfg.get("dbg_sb"):
            S.fence()
            named = dict(modT=modT, ABG=ABG, condT=condT, stat=stat, negB=negB, iw_all=iw_all, ngT=ngT, smallf=smallf)
            for nm in cfg["dbg_sb"]:
                t_ = named[nm]
                dd_ = nc.dram_tensor("dbgsb_" + nm, list(t_.shape), F32, kind="ExternalOutput")
                kb.S.op("sp", (lambda dd_=dd_, t_=t_: lambda e: e.dma_start(out=dd_[:, :], in_=t_[:]))(), [], [], dma=True)
        S.emit()
    return nc


PERM64 = list(range(0, 8)) + list(range(16, 40)) + list(range(8, 16)) + list(range(40, 64))


def make_consts():
    c = np.zeros((128, 640), np.float32)
    p = np.arange(128)
    c[:, 0:512] = np.arange(512)[None, :]
    c[:, 512] = p
    for a in range(4):
        c[:, 513 + a] = 128 * a + p
        c[:, 517 + a] = 64 * ((128 * a + p) // 64)
        c[:, 521 + a] = 64 * ((128 * a + p) // 64 + 1)
    r = p % 64
    i16 = np.where(r < 8, r, np.where((r >= 32) & (r < 40), r - 32, 0))
    c[:, 525] = -np.log(500000.0) * 2.0 * i16 / 16.0
    i64 = np.where(r < 32, r, r - 32)
    c[:, 526] = -np.log(10000.0) * 2.0 * i64 / 64.0
    c[:, 527] = np.where((p % 32) == 1, -1.0, 0.0)
    return c


def lay_k(w, ncols_pad=None):
    K, N = w.shape
    kc = K // 128
    return np.ascontiguousarray(w.reshape(kc, 128, N).transpose(1, 0, 2).reshape(128, kc * N))


def gather_cols(w, idx):
    idx = np.asarray(idx)
    out = np.zeros((w.shape[0], len(idx)), w.dtype)
    m = idx >= 0
    out[:, m] = w[:, idx[m]]
    return out


def prep_shared(inp, DEPTH):
    f = {}
    n_even = (DEPTH + 1) // 2
    n_odd = DEPTH // 2
    f["consts"] = make_consts()
    f["ada_w"] = np.stack([lay_k(inp["ada_w"][i]) for i in range(DEPTH)])
    f["ada_b"] = np.ascontiguousarray(inp["ada_b"][:DEPTH].reshape(DEPTH, 1, 9216))
    ng = inp["norm_g"][:DEPTH].reshape(DEPTH * 3, 8, 128).transpose(0, 2, 1)
    f["norm_gT"] = np.ascontiguousarray(ng)
    f["final_gT"] = np.ascontiguousarray(inp["final_g"].reshape(8, 128).T)
    f["w_gate"] = np.stack([lay_k(inp["ffn_w_gate"][i, j]) for i in range(DEPTH) for j in range(2)])
    f["w_up"] = np.stack([lay_k(inp["ffn_w_up"][i, j]) for i in range(DEPTH) for j in range(2)])
    f["w_down"] = np.stack([lay_k(inp["ffn_w_down"][i, j]) for i in range(DEPTH) for j in range(2)])
    off = np.cumsum([0, 512, 512, 512, 8, 512, 512, 512, 256, 4, 64])
    o_fq, o_fk, o_fv, o_ff, o_dq, o_dk, o_dv, o_iq, o_iw, o_ik = off[:10]
    fm = []
    for h in range(8):
        blk = [-1] * 128
        blk[0:64] = list(range(o_fq + h * 64, o_fq + (h + 1) * 64))
        blk[64] = o_ff + h
        blk[65] = o_ff + h
        fm += blk
    for h in range(8):
        blk = [-1] * 128
        blk[0:64] = list(range(o_fk + h * 64, o_fk + (h + 1) * 64))
        fm += blk
    for base in (o_dq, o_dk):
        for h in range(8):
            fm += [base + h * 64 + d for d in PERM64]
    for h in range(4):
        fm += [o_iq + h * 64 + d for d in PERM64]
    fm += [o_ik + d for d in PERM64] * 2
    assert len(fm) == 3456
    tm = list(range(o_fv, o_fv + 512)) + list(range(o_dv, o_dv + 512)) + list(range(o_iw, o_iw + 4)) + [-1] * 4
    f["hyb_fm"] = np.stack([lay_k(gather_cols(inp["hyb_w_in"][j], fm)) for j in range(n_even)])
    f["hyb_tm"] = np.stack([lay_k(gather_cols(inp["hyb_w_in"][j], tm)) for j in range(n_even)])
    f["hyb_out"] = np.stack([lay_k(inp["hyb_w_out"][j]) for j in range(n_even)])
    f["fox_b"] = np.ascontiguousarray(np.broadcast_to(inp["fox_b_f"][:n_even, None, :], (n_even, 128, 8))).astype(np.float32)
    if n_odd:
        dn_idx = list(range(704)) + [-1] * 64
        f["mla_down"] = np.stack([lay_k(gather_cols(inp["mla_w_down"][j], dn_idx)) for j in range(n_odd)])
        f["mla_qn"] = np.ascontiguousarray(inp["mla_q_norm"][:n_odd].reshape(n_odd, 3, 128).transpose(0, 2, 1))
        f["mla_kvn"] = np.ascontiguousarray(inp["mla_kv_norm"][:n_odd].reshape(n_odd, 2, 128).transpose(0, 2, 1))
        f["mla_uq"] = np.stack([lay_k(inp["mla_w_uq"][j]) for j in range(n_odd)])
        kv_idx = [h * 256 + d for h in range(8) for d in range(128)] + [h * 256 + 128 + d for h in range(8) for d in range(128)]
        f["mla_ukv"] = np.stack([lay_k(gather_cols(inp["mla_w_ukv"][j], kv_idx)) for j in range(n_odd)])
        f["mla_out"] = np.stack([lay_k(inp["mla_w_out"][j]) for j in range(n_odd)])
    return f


def run_cfg(inp, cfg, n_cores):
    NSEQ = cfg["NSEQ"]
    DEPTH = cfg["DEPTH"]
    shared = prep_shared(inp, DEPTH)
    nc = build_program(cfg)
    in_maps = []
    for c in range(n_cores):
        sl = slice(c * NSEQ, (c + 1) * NSEQ)
        m = dict(shared)
        m["xT"] = np.ascontiguousarray(inp["x"][sl].transpose(0, 2, 1))
        cc = inp["c"][sl]
        m["cT"] = np.ascontiguousarray(cc.reshape(NSEQ, 8, 128).transpose(2, 1, 0).reshape(128, 8 * NSEQ))
        m["pos"] = np.ascontiguousarray(inp["positions"][sl]).astype(np.int32)
        in_maps.append(m)
    res = run_bass_kernel_spmd(nc, in_maps, core_ids=list(range(n_cores)))
    return res


def kernel(**inputs):
    inp = {k: np.asarray(v) for k, v in inputs.items()}
    cfg = dict(S=4096, NSEQ=2, DEPTH=4)
    res = run_cfg(inp, cfg, 8)
    outs = [np.asarray(r["outT"]).transpose(0, 2, 1) for r in res.results]
    return np.ascontiguousarray(np.concatenate(outs, axis=0)).astype(np.float32)
```

```python
import contextlib
import numpy as np
import ml_dtypes
import concourse.bass as bass
import concourse.mybir as mybir
from concourse.bass_utils import run_bass_kernel_spmd

F32 = mybir.dt.float32
BF16 = mybir.dt.bfloat16
I32 = mybir.dt.int32
ALU = mybir.AluOpType
AF = mybir.ActivationFunctionType
AX = mybir.AxisListType

STREAMS = ("pe", "act", "dve", "pool", "sp")
N_DMA_SEMS = 12
EPOCH = 20000

D = 1024
DFF = 2816
NFC = 22
NEGM = -30000.0


class Buf:
    __slots__ = ("name", "w", "r", "excl")

    def __init__(self, name, excl=False):
        self.name = name
        self.w = None
        self.r = []
        self.excl = excl


class Sched:
    def __init__(self, nc):
        self.nc = nc
        self.ops = []
        self.cnt = {(s, k): 0 for s in STREAMS for k in "cd"}
        self.seen = {s: {} for s in STREAMS}
        self.fence_deps = {s: set() for s in STREAMS}
        self.out_dma = []

    def fence(self):
        deps = set()
        for s in STREAMS:
            n = self.cnt[(s, "c")]
            if n > 0:
                deps.add((s, "c", n - 1))
            nd = self.cnt[(s, "d")]
            for j in range(max(0, nd - N_DMA_SEMS), nd):
                deps.add((s, "d", j))
        for s in STREAMS:
            self.fence_deps[s] = set(deps)
            self.seen[s] = {k: v for k, v in self.seen[s].items() if not isinstance(k, tuple)}

    def op(self, stream, fn, reads=(), writes=(), dma=False):
        kind = "d" if dma else "c"
        idx = self.cnt[(stream, kind)]
        self.cnt[(stream, kind)] += 1
        me = (stream, kind, idx)
        deps = set()
        if self.fence_deps[stream]:
            deps |= self.fence_deps[stream]
            self.fence_deps[stream] = set()
        for b in reads:
            if not b.excl and b.w is not None:
                deps.add(b.w)
        wl = list(writes) + [b for b in reads if b.excl]
        for b in wl:
            if b.w is not None:
                deps.add(b.w)
            deps.update(b.r)
        for b in reads:
            if not b.excl:
                if dma:
                    b.r.append(me)
                else:
                    b.r = [x for x in b.r if not (x[0] == stream and x[1] == "c")] + [me]
        for b in wl:
            b.w = me
            b.r = []
        fd = []
        seen = self.seen[stream]
        best = {}
        for d in deps:
            ps, pk, pi = d
            if d == me:
                continue
            if pk == "d":
                if d in seen:
                    continue
                seen[d] = True
                fd.append(d)
            else:
                if ps == stream and stream == "pe" and not dma:
                    continue
                if ps == stream and pi >= idx and not dma:
                    continue
                if seen.get(ps, -1) >= pi:
                    continue
                if best.get(ps, -1) < pi:
                    best[ps] = pi
        for ps, pi in best.items():
            seen[ps] = pi
            fd.append((ps, "c", pi))
        self.ops.append((stream, kind, idx, fn, fd))
        return me

    def emit(self):
        nc = self.nc
        needs = {s: [False] * self.cnt[(s, "c")] for s in STREAMS}
        for stream, kind, idx, fn, fd in self.ops:
            for (ps, pk, pi) in fd:
                if pk == "c":
                    needs[ps][pi] = True
        val = {}
        for s in STREAMS:
            c = 0
            v = []
            for n in needs[s]:
                if n:
                    c += 1
                v.append(c)
            val[s] = v
        per = {s: [] for s in STREAMS}
        for o in self.ops:
            per[o[0]].append(o)
        self.nwaits = 0
        with contextlib.ExitStack() as st:
            sems = {}
            for s in STREAMS:
                tot = val[s][-1] if val[s] else 0
                sems[s] = [st.enter_context(nc.semaphore("s_%s_%d" % (s, i))) for i in range(tot // EPOCH + 1)]
            dsems = {s: [st.enter_context(nc.semaphore("d_%s_%d" % (s, i))) for i in range(N_DMA_SEMS)]
                     for s in STREAMS if self.cnt[(s, "d")] > 0}
            block = st.enter_context(nc.Block())

            def semv(ps, c):
                ep = (c - 1) // EPOCH
                return sems[ps][ep], c - ep * EPOCH

            def run(s, e):
                for (stream, kind, idx, fn, fd) in per[s]:
                    for (ps, pk, pi) in fd:
                        self.nwaits += 1
                        if pk == "d":
                            e.wait_ge(dsems[ps][pi % N_DMA_SEMS], 16 * (pi // N_DMA_SEMS + 1))
                        else:
                            sm, v = semv(ps, val[ps][pi])
                            e.wait_ge(sm, v)
                    if kind == "d":
                        if idx >= N_DMA_SEMS:
                            e.wait_ge(dsems[s][idx % N_DMA_SEMS], 16 * (idx // N_DMA_SEMS))
                        fn(e).then_inc(dsems[s][idx % N_DMA_SEMS], 16)
                    else:
                        ins = fn(e)
                        if needs[s][idx]:
                            sm, v = semv(s, val[s][idx])
                            ins.then_inc(sm, 1)
                n = self.cnt[(s, "d")]
                for j in range(max(0, n - N_DMA_SEMS), n):
                    e.wait_ge(dsems[s][j % N_DMA_SEMS], 16 * (j // N_DMA_SEMS + 1))

            @block.tensor
            def _(e):
                run("pe", e)

            @block.scalar
            def _(e):
                run("act", e)

            @block.vector
            def _(e):
                run("dve", e)

            @block.gpsimd
            def _(e):
                run("pool", e)

            @block.sync
            def _(e):
                run("sp", e)


class Tl:
    __slots__ = ("ap", "b")

    def __init__(self, ap, b):
        self.ap = ap
        self.b = b


class KB:
    def __init__(self, nc, S, cfg):
        self.nc = nc
        self.S = S
        self.cfg = cfg
        self.st = contextlib.ExitStack()
        self.uid = 0
        self.rr = 0

    def mm(self, out, lhsT, rhs, start, stop, reads, writes):
        self.S.op("pe", lambda e: e.matmul(out, lhsT, rhs, start=start, stop=stop, skip_group_check=True), reads, writes)

    def act(self, out, in_, func, reads, writes, bias=0.0, scale=1.0, accum_out=None):
        if accum_out is None:
            self.S.op("act", lambda e: e.activation(out, in_, func, bias=bias, scale=scale), reads, writes)
        else:
            self.S.op("act", lambda e: e.activation(out, in_, func, bias=bias, scale=scale, accum_out=accum_out), reads, writes)

    def tt(self, eng, out, in0, in1, op, reads, writes):
        self.S.op(eng, lambda e: e.tensor_tensor(out, in0, in1, op), reads, writes)

    def ts(self, eng, out, in0, s1, s2, op0, op1, reads, writes, accum_out=None):
        if accum_out is None:
            if op1 is None:
                self.S.op(eng, lambda e: e.tensor_single_scalar(out, in0, s1, op0), reads, writes)
            else:
                self.S.op(eng, lambda e: e.tensor_scalar(out, in0, s1, s2, op0, op1), reads, writes)
        else:
            self.S.op(eng, lambda e: e.tensor_scalar(out, in0, s1, s2, op0, op1, accum_out=accum_out), reads, writes)

    def stt(self, eng, out, in0, scalar, in1, op0, op1, reads, writes):
        self.S.op(eng, lambda e: e.scalar_tensor_tensor(out, in0, scalar, in1, op0, op1), reads, writes)

    def cp(self, eng, out, in_, reads, writes):
        if eng == "act":
            self.S.op("act", lambda e: e.copy(out, in_), reads, writes)
        else:
            self.S.op(eng, lambda e: e.tensor_copy(out, in_), reads, writes)

    def memset(self, eng, ap, v, writes):
        self.S.op(eng, lambda e: e.memset(ap, v), (), writes)

    def dma(self, out, in_, reads, writes, slow=False):
        if slow:
            self.S.op("sp", lambda e: e.dma_start(out=out, in_=in_, allow_slow_non_contiguous=True), reads, writes, dma=True)
        else:
            self.S.op("sp", lambda e: e.dma_start(out=out, in_=in_), reads, writes, dma=True)

    def recip(self, out, in_, reads, writes):
        self.S.op("dve", lambda e: e.reciprocal(out, in_), reads, writes)

    def sb(self, name, shape, dt):
        t = self.st.enter_context(self.nc.sbuf_tensor(name, list(shape), dt))
        return t

    def psum(self, name, shape, dt):
        return self.st.enter_context(self.nc.psum_tensor(name, list(shape), dt))

    def dram(self, name, shape, dt, kind="Internal"):
        return self.nc.dram_tensor(name, list(shape), dt, kind=kind)


class Arena:
    def __init__(self, t_f32, nwords):
        self.t = t_f32
        self.n = nwords
        self.off = 0

    def reset(self):
        self.off = 0

    def f32(self, n, name="a"):
        o = self.off
        self.off += n
        assert self.off <= self.n, ("arena overflow", name, self.off, self.n)
        return Tl(self.t[:, o:o + n], Buf(name))

    def bf(self, n, name="a"):
        w = (n + 1) // 2
        o = self.off
        self.off += w
        assert self.off <= self.n, ("arena overflow", name, self.off, self.n)
        return Tl(self.t[:, o:o + w].bitcast(BF16), Buf(name))


def build_program(cfg):
    SQ = cfg["S"]
    NSEQ = cfg["NSEQ"]
    DEPTH = cfg["DEPTH"]
    NG = SQ // 512
    NKT = SQ // 128
    nc = bass.Bass("TRN2", target_bir_lowering=False)
    S = Sched(nc)
    kb = KB(nc, S, cfg)
    n_even = (DEPTH + 1) // 2
    n_odd = DEPTH // 2

    def din(name, shape, dt=F32):
        return nc.dram_tensor(name, list(shape), dt, kind="ExternalInput")

    xT = din("xT", [NSEQ, D, SQ])
    cT = din("cT", [128, 8 * NSEQ])
    pos = din("pos", [NSEQ, SQ], I32)
    consts = din("consts", [128, 640])
    ada_w = din("ada_w", [DEPTH, 128, 8 * 9216])
    ada_b = din("ada_b", [DEPTH, 1, 9216])
    norm_gT = din("norm_gT", [DEPTH * 3, 128, 8])
    final_gT = din("final_gT", [128, 8])
    w_gate = din("w_gate", [DEPTH * 2, 128, 8 * DFF])
    w_up = din("w_up", [DEPTH * 2, 128, 8 * DFF])
    w_down = din("w_down", [DEPTH * 2, 128, NFC * D])
    NHF = 3456
    NHT = 1032
    hyb_fm = din("hyb_fm", [n_even, 128, 8 * NHF])
    hyb_tm = din("hyb_tm", [n_even, 128, 8 * NHT])
    hyb_out = din("hyb_out", [n_even, 128, 8 * D])
    fox_b = din("fox_b", [n_even, 128, 8])
    if n_odd:
        mla_down = din("mla_down", [n_odd, 128, 8 * 768])
        mla_qn = din("mla_qn", [n_odd, 128, 3])
        mla_kvn = din("mla_kvn", [n_odd, 128, 2])
        mla_uq = din("mla_uq", [n_odd, 128, 3 * 1536])
        mla_ukv = din("mla_ukv", [n_odd, 128, 2 * 2048])
        mla_out = din("mla_out", [n_odd, 128, 8 * D])
    outT = nc.dram_tensor("outT", [NSEQ, D, SQ], F32, kind="ExternalOutput")

    hT = [nc.dram_tensor("hT%d" % s, [D, SQ], F32, kind=("ExternalOutput" if cfg.get("dbg_h") else "Internal")) for s in range(NSEQ)]
    hT_b = [[[Buf("hT%d_%d_%d" % (s, g, c)) for c in range(8)] for g in range(NG)] for s in range(NSEQ)]
    xT_b = [[Buf("xT%d_%d" % (g, c)) for c in range(8)] for g in range(NG)]
    scr = {}

    def scratch(name, shape, dt=BF16):
        kind = "ExternalOutput" if name in cfg.get("dbg", []) else "Internal"
        scr[name] = (nc.dram_tensor("scr_" + name, list(shape), dt, kind=kind), Buf("scr_" + name))
        return scr[name]

    for h in range(8):
        scratch("QA%d" % h, [128, SQ])
        scratch("KA%d" % h, [128, SQ])
    scratch("VF", [SQ, 1024])
    for c in range(4):
        scratch("DQ%d" % c, [128, SQ])
        scratch("DK%d" % c, [128, SQ])
    scratch("DV", [SQ, 512])
    for c in range(2):
        scratch("IQ%d" % c, [128, SQ])
    scratch("IK", [128, SQ])
    scratch("MIX", [D, SQ])
    if n_odd:
        for h in range(8):
            scratch("QN%d" % h, [128, SQ])
            scratch("QR%d" % h, [64, SQ])
            scratch("KN%d" % h, [128, SQ])
        scratch("KR", [64, SQ])
        scratch("MV", [SQ, 1024])
    with kb.st:
        ARENA_WORDS = 50600
        arena_t = kb.sb("arena", [128, ARENA_WORDS], F32)
        A = Arena(arena_t, ARENA_WORDS)
        cst = kb.sb("cst", [128, 640], F32)
        cst_b = Buf("cst")
        ident_bf = kb.sb("ident_bf", [128, 128], BF16)
        ones_bf = kb.sb("ones_bf", [128, 128], BF16)
        zeros_f = kb.sb("zeros_f", [128, 512], F32)
        misc_b = Buf("misc")
        condT = kb.sb("condT", [128, 8 * NSEQ], F32)
        cond_b = Buf("cond")
        modT = kb.sb("modT", [128, NSEQ * 72], F32)
        mod_b = Buf("mod")
        ngT = kb.sb("ngT", [128, DEPTH * 3 * 8 + 8], F32)
        ng_b = Buf("ng")
        ABG = kb.sb("ABG", [128, NSEQ * 3 * 24], F32)
        abg_b = Buf("abg")
        stat = kb.sb("stat", [128, 64], F32)
        stat_b = Buf("stat")
        negB = kb.sb("negB", [128, 16], F32)
        negB_b = Buf("negB")
        iw_all = kb.sb("iw_all", [128, NKT * 4], F32)
        iw_b = Buf("iw")
        iwabs = kb.sb("iwabs", [128, NKT * 4], F32)
        iwsgn = kb.sb("iwsgn", [128, NKT * 4], F32)
        smallf = kb.sb("smallf", [128, 64], F32)
        fb_t = kb.sb("fb_t", [128, 8], F32)
        fb_b = Buf("fb")
        qn_t = kb.sb("qn_t", [128, 8], F32)
        qn_b = Buf("qn")
        PS = [kb.psum("ps%d" % i, [128, 512], F32) for i in range(8)]
        PB = [Buf("ps%d" % i, excl=True) for i in range(8)]

        kb.dma(cst[:], consts[:, :], [], [cst_b])
        IOTA = cst[:, 0:512]
        kb.memset("pool", zeros_f[:], 0.0, [misc_b])
        kb.memset("pool", ones_bf[:], 1.0, [misc_b])
        kb.ts("dve", ident_bf[:], cst[:, 0:128], cst[:, 512:513], None, ALU.is_equal, None, [cst_b], [misc_b])

        def build_mask(kind):
            m = A.bf(4 * 512, "mask%d" % kind)
            for a in range(4):
                if kind == 0:
                    kb.ts("dve", m.ap[:, a * 512:(a + 1) * 512], IOTA, cst[:, 513 + a:514 + a], NEGM, ALU.is_lt, ALU.mult, [cst_b], [m.b])
                elif kind == 1:
                    kb.ts("dve", m.ap[:, a * 512:(a + 1) * 512], IOTA, cst[:, 517 + a:518 + a], NEGM, ALU.is_lt, ALU.mult, [cst_b], [m.b])
                else:
                    kb.ts("dve", m.ap[:, a * 512:(a + 1) * 512], IOTA, cst[:, 521 + a:522 + a], -1e30, ALU.is_ge, ALU.mult, [cst_b], [m.b])
            return m
        kb.dma(condT[:], cT[:, :], [], [cond_b])
        kb.act(condT[:], condT[:], AF.Silu, [cond_b], [cond_b])
        for i in range(DEPTH * 3):
            kb.dma(ngT[:, i * 8:(i + 1) * 8], norm_gT[i, :, :], [], [ng_b])
        kb.dma(ngT[:, DEPTH * 24:DEPTH * 24 + 8], final_gT[:, :], [], [ng_b])

        def load_w_bf(dst_bf_ap, src_dram_ap, ncols, name):
            CH = 2048
            nst = 3
            stg = [A.f32(CH, "stg%d" % i) for i in range(nst)]
            dstb = Buf(name)
            i = 0
            engs = ("pool", "act", "dve")
            for o in range(0, ncols, CH):
                n = min(CH, ncols - o)
                s_ = stg[i % nst]
                kb.dma(s_.ap[:, 0:n], src_dram_ap[:, o:o + n], [], [s_.b])
                kb.cp(("dve", "act", "dve", "act", "pool")[i % 5], dst_bf_ap[:, o:o + n], s_.ap[:, 0:n], [s_.b], [dstb])
                i += 1
            S.fence()
            return dstb

        def compute_mod(layer):
            A.reset()
            PIECE = 512
            wst = [A.f32(8 * PIECE, "adaw%d" % i) for i in range(2)]
            bst = [A.f32(PIECE, "adab%d" % i) for i in range(2)]
            onesf = A.f32(8, "onesf")
            kb.memset("pool", onesf.ap[0:1, 0:NSEQ], 1.0, [onesf.b])
            npiece = 9216 // PIECE
            for pi in range(npiece):
                w_ = wst[pi % 2]
                b_ = bst[pi % 2]
                src = ada_w[layer].rearrange("p (k n) -> p k n", k=8)[:, :, pi * PIECE:(pi + 1) * PIECE]
                kb.dma(w_.ap.rearrange("p (k n) -> p k n", k=8), src, [], [w_.b])
                kb.dma(b_.ap[0:1, :], ada_b[layer, :, pi * PIECE:(pi + 1) * PIECE], [], [b_.b])
                for cc in range(PIECE // 128):
                    col = pi * (PIECE // 128) + cc
                    for k in range(8):
                        kb.mm(PS[0][:, col * NSEQ:(col + 1) * NSEQ], w_.ap[:, k * PIECE + cc * 128:k * PIECE + (cc + 1) * 128],
                              condT[:, k * NSEQ:(k + 1) * NSEQ], k == 0, False, [w_.b, cond_b], [PB[0]])
                    kb.mm(PS[0][:, col * NSEQ:(col + 1) * NSEQ], b_.ap[0:1, cc * 128:(cc + 1) * 128],
                          onesf.ap[0:1, 0:NSEQ], False, True, [b_.b, onesf.b], [PB[0]])
            for s in range(NSEQ):
                src = PS[0][:, 0:72 * NSEQ].rearrange("p (c s) -> p c s", s=NSEQ)[:, :, s]
                kb.cp("dve", modT[:, s * 72:(s + 1) * 72], src, [PB[0]], [mod_b])

        def compute_abg(layer):
            for s in range(NSEQ):
                for sub in range(3):
                    o = (s * 3 + sub) * 24
                    m = s * 72 + sub * 24
                    g = ngT[:, (layer * 3 + sub) * 8:(layer * 3 + sub) * 8 + 8]
                    kb.ts("dve", ABG[:, o:o + 8], modT[:, m + 8:m + 16], 1.0, None, ALU.add, None, [mod_b], [abg_b])
                    kb.tt("dve", ABG[:, o:o + 8], ABG[:, o:o + 8], g, ALU.mult, [abg_b, ng_b], [abg_b])
                    kb.ts("dve", ABG[:, o:o + 8], ABG[:, o:o + 8], 32.0, None, ALU.mult, None, [abg_b], [abg_b])
                    kb.cp("dve", ABG[:, o + 8:o + 16], modT[:, m:m + 8], [mod_b], [abg_b])
                    gs = 1.0 if sub == 1 else 0.5
                    kb.ts("dve", ABG[:, o + 16:o + 24], modT[:, m + 16:m + 24], gs, None, ALU.mult, None, [mod_b], [abg_b])

        def h_src(layer, sub, s):
            if layer == 0 and sub == 0:
                return xT[s], xT_b
            return hT[s][:, :], hT_b[s]

        def norm_group(hsrc, hsrc_b, s, sub, g, hg, uT, extra_scale=None, final=False, layer=0):
            sq = A_sq
            rs = A_rs
            tmp = A_tmp
            src = hsrc.rearrange("(c p) t -> p c t", p=128)[:, :, g * 512:(g + 1) * 512]
            kb.dma(hg.ap.rearrange("p (c t) -> p c t", c=8), src, list(hsrc_b[g]), [hg.b])
            for c in range(8):
                kb.act(sq.ap[:, c * 512:(c + 1) * 512], hg.ap[:, c * 512:(c + 1) * 512], AF.Square, [hg.b], [sq.b])
            for c in range(8):
                kb.mm(PS[7][:, :], ones_bf[:, :], sq.ap[:, c * 512:(c + 1) * 512], c == 0, c == 7, [sq.b, misc_b], [PB[7]])
            kb.act(rs.ap, PS[7][:, :], AF.Sqrt, [PB[7]], [rs.b], bias=epsb[:, 0:1], scale=1.0)
            kb.recip(rs.ap, rs.ap, [rs.b], [rs.b])
            o = (s * 3 + sub) * 24
            for c in range(8):
                t_ = tmp[c % 2]
                if final:
                    kb.stt("dve", t_.ap, hg.ap[:, c * 512:(c + 1) * 512], fg32[:, c:c + 1], rs.ap, ALU.mult, ALU.mult,
                           [hg.b, rs.b, abg_b], [t_.b])
                    kb.cp("act", uT.ap[:, c * 512:(c + 1) * 512], t_.ap, [t_.b], [uT.b])
                else:
                    kb.stt("dve", t_.ap, hg.ap[:, c * 512:(c + 1) * 512], ABG[:, o + c:o + c + 1], rs.ap, ALU.mult, ALU.mult,
                           [hg.b, rs.b, abg_b], [t_.b])
                    kb.act(uT.ap[:, c * 512:(c + 1) * 512], t_.ap, AF.Identity, [t_.b, abg_b], [uT.b],
                           bias=ABG[:, o + 8 + c:o + 9 + c], scale=1.0)

        epsb = smallf[:, 0:1]
        kb.memset("pool", smallf[:, 0:1], float(D) * 1e-6, [misc_b])
        fg32 = smallf[:, 8:16]
        kb.ts("dve", fg32, ngT[:, DEPTH * 24:DEPTH * 24 + 8], 32.0, None, ALU.mult, None, [ng_b], [abg_b])

        def ffn_phase(layer, which):
            global A_sq, A_rs, A_tmp
            S.fence()
            A.reset()
            sub = 0 if which == 0 else 2
            wi = layer * 2 + which
            wg = A.bf(8 * DFF, "wg")
            wu = A.bf(8 * DFF, "wu")
            wd = A.bf(NFC * D, "wd")
            mark = A.off
            wg.b = load_w_bf(wg.ap, w_gate[wi], 8 * DFF, "wg")
            A.off = mark
            wu.b = load_w_bf(wu.ap, w_up[wi], 8 * DFF, "wu")
            A.off = mark
            wd.b = load_w_bf(wd.ap, w_down[wi], NFC * D, "wd")
            A.off = mark
            S.fence()
            hg = A.f32(8 * 512, "hg")
            uT = A.bf(8 * 512, "uT")
            actT = A.bf(NFC * 512, "actT")
            sq = Tl(uT.ap, uT.b)
            rs = A.f32(512, "rs")
            tmp = [A.f32(512, "tmp%d" % i) for i in range(2)]
            sg = [A.bf(512, "sg%d" % i) for i in range(2)]
            hr = [A.f32(512, "hr%d" % i) for i in range(2)]
            items = [(s, g) for s in range(NSEQ) for g in range(NG)]

            def n_load(i):
                s, g = items[i]
                hsrc, hsrc_b = h_src(layer, sub, s)
                src = hsrc.rearrange("(c p) t -> p c t", p=128)[:, :, g * 512:(g + 1) * 512]
                kb.dma(hg.ap.rearrange("p (c t) -> p c t", c=8), src, list(hsrc_b[g]), [hg.b])

            def n_sq(i):
                for c in range(8):
                    kb.act(sq.ap[:, c * 512:(c + 1) * 512], hg.ap[:, c * 512:(c + 1) * 512], AF.Square, [hg.b], [sq.b])

            def n_stats(i):
                for c in range(8):
                    kb.mm(PS[7][:, :], ones_bf[:, :], sq.ap[:, c * 512:(c + 1) * 512], c == 0, c == 7, [sq.b, misc_b], [PB[7]])
                kb.act(rs.ap, PS[7][:, :], AF.Sqrt, [PB[7]], [rs.b], bias=epsb[:, 0:1], scale=1.0)
                kb.recip(rs.ap, rs.ap, [rs.b], [rs.b])

            def n_u(i, c):
                s, g = items[i]
                o = (s * 3 + sub) * 24
                t_ = tmp[c % 2]
                kb.stt("dve", t_.ap, hg.ap[:, c * 512:(c + 1) * 512], ABG[:, o + c:o + c + 1], rs.ap, ALU.mult, ALU.mult,
                       [hg.b, rs.b, abg_b], [t_.b])
                kb.act(uT.ap[:, c * 512:(c + 1) * 512], t_.ap, AF.Identity, [t_.b, abg_b], [uT.b],
                       bias=ABG[:, o + 8 + c:o + 9 + c], scale=1.0)

            def r_load(i, c):
                s, g = items[i]
                hsrc, hsrc_b = h_src(layer, sub, s)
                kb.dma(hr[c % 2].ap, hsrc[c * 128:(c + 1) * 128, g * 512:(g + 1) * 512], [hsrc_b[g][c]], [hr[c % 2].b])

            def down(i, c):
                s, g = items[i]
                o = (s * 3 + sub) * 24
                pd = 4 + (c % 2)
                for f in range(NFC):
                    kb.mm(PS[pd][:, :], wd.ap[:, f * D + c * 128:f * D + (c + 1) * 128], actT.ap[:, f * 512:(f + 1) * 512],
                          f == 0, f == NFC - 1, [wd.b, actT.b], [PB[pd]])
                h_ = hr[c % 2]
                kb.stt("dve", h_.ap, PS[pd][:, :], ABG[:, o + 16 + c:o + 17 + c], h_.ap, ALU.mult, ALU.add, [PB[pd], abg_b, h_.b], [h_.b])
                kb.dma(hT[s][c * 128:(c + 1) * 128, g * 512:(g + 1) * 512], h_.ap, [h_.b], [hT_b[s][g][c]])
                if c + 2 < 8:
                    r_load(i, c + 2)

            n_load(0)
            n_sq(0)
            n_stats(0)
            for c in range(8):
                n_u(0, c)
            for i in range(len(items)):
                s, g = items[i]
                nxt = i + 1 < len(items)
                if cfg.get("dbg_ffn") and i == 0 and layer == 0 and which == 0:
                    du = nc.dram_tensor("dbg_uT", [128, 4096], BF16, kind="ExternalOutput")
                    kb.S.op("sp", (lambda du=du, uT=uT: lambda e: e.dma_start(out=du[:, :], in_=uT.ap))(), [uT.b], [], dma=True)
                r_load(i, 0)
                r_load(i, 1)
                if nxt:
                    n_load(i + 1)
                for f in range(NFC):
                    pg = 0 + (f % 2) * 2
                    pu = 1 + (f % 2) * 2
                    for k in range(8):
                        kb.mm(PS[pg][:, :], wg.ap[:, k * DFF + f * 128:k * DFF + (f + 1) * 128], uT.ap[:, k * 512:(k + 1) * 512],
                              k == 0, k == 7, [wg.b, uT.b], [PB[pg]])
                    for k in range(8):
                        kb.mm(PS[pu][:, :], wu.ap[:, k * DFF + f * 128:k * DFF + (f + 1) * 128], uT.ap[:, k * 512:(k + 1) * 512],
                              k == 0, k == 7, [wu.b, uT.b], [PB[pu]])
                    sg_ = sg[f % 2]
                    kb.act(sg_.ap, PS[pg][:, :], AF.Silu, [PB[pg]], [sg_.b])
                    kb.tt("dve", actT.ap[:, f * 512:(f + 1) * 512], sg_.ap, PS[pu][:, :], ALU.mult, [sg_.b, PB[pu]], [actT.b])
                if cfg.get("dbg_ffn") and i == 0 and layer == 0 and which == 0:
                    da = nc.dram_tensor("dbg_actT", [128, NFC * 512], BF16, kind="ExternalOutput")
                    kb.S.op("sp", (lambda da=da, actT=actT: lambda e: e.dma_start(out=da[:, :], in_=actT.ap))(), [actT.b], [], dma=True)
                if nxt:
                    n_sq(i + 1)
                down(i, 0)
                down(i, 1)
                if nxt:
                    n_stats(i + 1)
                for c in range(2, 8):
                    down(i, c)
                    if nxt:
                        n_u(i + 1, c - 2)
                if nxt:
                    n_u(i + 1, 6)
                    n_u(i + 1, 7)

        def outproj_phase(layer, w_dram, s):
            S.fence()
            A.reset()
            wo = A.bf(8 * D, "wo")
            mark = A.off
            wo.b = load_w_bf(wo.ap, w_dram, 8 * D, "wo")
            A.off = mark
            S.fence()
            mx = [A.bf(8 * 512, "mx%d" % i) for i in range(2)]
            hg = [A.f32(8 * 512, "hgo%d" % i) for i in range(2)]
            ho = [A.f32(512, "hoo%d" % i) for i in range(2)]
            o = (s * 3 + 1) * 24
            mixd, mixb = scr["MIX"]
            for g in range(NG):
                m_ = mx[g % 2]
                h_ = hg[g % 2]
                kb.dma(m_.ap.rearrange("p (c t) -> p c t", c=8),
                       mixd.rearrange("(c p) t -> p c t", p=128)[:, :, g * 512:(g + 1) * 512], [mixb], [m_.b])
                kb.dma(h_.ap.rearrange("p (c t) -> p c t", c=8),
                       hT[s].rearrange("(c p) t -> p c t", p=128)[:, :, g * 512:(g + 1) * 512], list(hT_b[s][g]), [h_.b])
                for c in range(8):
                    pd = c % 2
                    for k in range(8):
                        kb.mm(PS[pd][:, :], wo.ap[:, k * D + c * 128:k * D + (c + 1) * 128], m_.ap[:, k * 512:(k + 1) * 512],
                              k == 0, k == 7, [wo.b, m_.b], [PB[pd]])
                    ho_ = ho[c % 2]
                    kb.stt("dve", ho_.ap, PS[pd][:, :], ABG[:, o + 16 + c:o + 17 + c], h_.ap[:, c * 512:(c + 1) * 512],
                           ALU.mult, ALU.add, [PB[pd], abg_b, h_.b], [ho_.b])
                    kb.dma(hT[s][c * 128:(c + 1) * 128, g * 512:(g + 1) * 512], ho_.ap, [ho_.b], [hT_b[s][g][c]])

        def rope_tables(s, col_scale, cosT, sinT):
            posi = A.f32(SQ, "posi")
            pi_ap = posi.ap.bitcast(I32)
            kb.dma(pi_ap, pos[s:s + 1, :].partition_broadcast(128), [], [posi.b])
            invf = smallf[:, 20:21]
            kb.act(invf, cst[:, col_scale:col_scale + 1], AF.Exp, [cst_b], [misc_b])
            ang = cosT
            kb.cp("dve", ang.ap, pi_ap, [posi.b], [ang.b])
            kb.ts("dve", ang.ap, ang.ap, invf, None, ALU.mult, None, [ang.b, misc_b], [ang.b])
            C1 = 6.28125
            C2 = float(2.0 * np.pi - 6.28125)
            ki = A.f32(SQ, "ki")
            kf = A.f32(SQ, "kf")

            def reduce_to(dst, shift):
                kb.ts("dve", kf.ap, ang.ap, shift, float(1.0 / (2.0 * np.pi)), ALU.add, ALU.mult, [ang.b], [kf.b])
                kb.cp("dve", ki.ap.bitcast(I32), kf.ap, [kf.b], [ki.b])
                kb.cp("dve", kf.ap, ki.ap.bitcast(I32), [ki.b], [kf.b])
                kb.stt("dve", dst.ap, kf.ap, -C1, ang.ap, ALU.mult, ALU.add, [kf.b, ang.b], [dst.b])
                kb.stt("dve", dst.ap, kf.ap, -C2, dst.ap, ALU.mult, ALU.add, [kf.b, dst.b], [dst.b])
                if shift != 0.0:
                    kb.ts("dve", dst.ap, dst.ap, shift, None, ALU.add, None, [dst.b], [dst.b])
                kb.ts("dve", kf.ap, dst.ap, float(np.pi), float(-2.0 * np.pi), ALU.is_gt, ALU.mult, [dst.b], [kf.b])
                kb.tt("dve", dst.ap, dst.ap, kf.ap, ALU.add, [dst.b, kf.b], [dst.b])
                kb.ts("dve", kf.ap, dst.ap, float(-np.pi), float(2.0 * np.pi), ALU.is_lt, ALU.mult, [dst.b], [kf.b])
                kb.tt("dve", dst.ap, dst.ap, kf.ap, ALU.add, [dst.b, kf.b], [dst.b])
                kb.ts("dve", dst.ap, dst.ap, float(np.pi), -float(np.pi), ALU.min, ALU.max, [dst.b], [dst.b])

            reduce_to(sinT, 0.0)
            tmpc = A.f32(SQ, "tmpc")
            reduce_to(tmpc, float(0.5 * np.pi))
            kb.cp("dve", cosT.ap, tmpc.ap, [tmpc.b], [cosT.b])
            kb.act(sinT.ap, sinT.ap, AF.Sin, [sinT.b], [sinT.b])
            kb.act(cosT.ap, cosT.ap, AF.Sin, [cosT.b], [cosT.b])

        def rope_block(y_ps, pb, b0, half, g, cosT, sinT, dst, t1, t2, eng="dve"):
            tk = slice(g * 512, (g + 1) * 512)
            r1 = slice(b0, b0 + half)
            r2 = slice(b0 + 32, b0 + 32 + half)
            kb.tt(eng, t1.ap[r1, :], y_ps[r2, :], sinT.ap[r2, tk], ALU.mult, [pb, sinT.b], [t1.b])
            kb.tt(eng, t2.ap[r2, :], y_ps[r1, :], sinT.ap[r1, tk], ALU.mult, [pb, sinT.b], [t2.b])
            kb.tt(eng, t2.ap[r1, :], y_ps[r1, :], cosT.ap[r1, tk], ALU.mult, [pb, cosT.b], [t2.b])
            kb.tt(eng, t1.ap[r2, :], y_ps[r2, :], cosT.ap[r2, tk], ALU.mult, [pb, cosT.b], [t1.b])
            kb.tt(eng, dst[r1, :], t2.ap[r1, :], t1.ap[r1, :], ALU.subtract, [t1.b, t2.b], [dstb_cur[0]])
            kb.tt(eng, dst[r2, :], t1.ap[r2, :], t2.ap[r2, :], ALU.add, [t1.b, t2.b], [dstb_cur[0]])

        dstb_cur = [None]

        deferred = []

        def flush_deferred(keep=0):
            while len(deferred) > keep:
                deferred.pop(0)()

        def bound_update(sq_ap, sq_b, rows, col):
            deferred.append(lambda: bound_update_now(sq_ap, sq_b, rows, col))

        def bound_update_now(sq_ap, sq_b, rows, col):
            kb.mm(PS[6][:, :], ones_bf[rows, :], sq_ap[rows, :], True, True, [sq_b, misc_b], [PB[6]])
            kb.S.op("dve", lambda e: e.tensor_reduce(smallf[:, 32:33], PS[6][:, :], AX.X, ALU.max), [PB[6]], [misc_b])
            kb.tt("dve", stat[:, col:col + 1], stat[:, col:col + 1], smallf[:, 32:33], ALU.max, [misc_b, stat_b], [stat_b])

        def finish_bounds(nh, scale):
            kb.tt("dve", negB[:, 0:nh], stat[:, 0:nh], stat[:, 16:16 + nh], ALU.mult, [stat_b], [negB_b])
            kb.act(negB[:, 0:nh], negB[:, 0:nh], AF.Sqrt, [negB_b], [negB_b], scale=scale * scale)
            kb.ts("dve", negB[:, 0:nh], negB[:, 0:nh], -1.0, None, ALU.mult, None, [negB_b], [negB_b])

        def hyb_proj_phase(layer, s):
            global A_sq, A_rs, A_tmp
            j = layer // 2
            S.fence()
            A.reset()
            wf = A.bf(8 * NHF, "wf")
            wt = A.bf(8 * NHT, "wt")
            mark = A.off
            wf.b = load_w_bf(wf.ap, hyb_fm[j], 8 * NHF, "wf")
            A.off = mark
            wt.b = load_w_bf(wt.ap, hyb_tm[j], 8 * NHT, "wt")
            A.off = mark
            kb.dma(fb_t[:], fox_b[j, :, :], [], [fb_b])
            kb.ts("dve", fb_t[:], fb_t[:], -1.0, None, ALU.mult, None, [fb_b], [fb_b])
            S.fence()
            cos16 = A.f32(SQ, "cos16")
            sin16 = A.f32(SQ, "sin16")
            mk2 = A.off
            rope_tables(s, 525, cos16, sin16)
            A.off = mk2
            S.fence()
            hg = A.f32(8 * 512, "hg")
            uT = A.bf(8 * 512, "uT")
            A_sq = A.bf(8 * 512, "sq")
            A_rs = A.f32(512, "rs")
            A_tmp = [A.f32(512, "tmp%d" % i) for i in range(2)]
            qst = [A.bf(512, "qst%d" % i) for i in range(2)]
            kst = [A.bf(512, "kst%d" % i) for i in range(2)]
            gst = [A.bf(512, "gst%d" % i) for i in range(3)]
            sqs = [A.bf(512, "sqs%d" % i) for i in range(2)]
            t1 = A.f32(512, "t1")
            t2 = A.f32(512, "t2")
            ge = A.f32(512, "ge")
            gc = A.f32(512, "gc")
            gh = A.f32(512, "gh")
            ghb = A.bf(512, "ghb")
            vst = [A.bf(1024, "vstf"), A.bf(512, "vstd")]
            kb.memset("pool", vst[0].ap[:, :], 1.0, [vst[0].b])
            carry = smallf[:, 34:42]
            kb.memset("pool", carry, 0.0, [misc_b])
            kb.memset("pool", stat[:, 0:32], 0.0, [stat_b])
            for q_ in qst:
                kb.memset("pool", q_.ap[:, :], 0.0, [q_.b])
                kb.memset("pool", q_.ap[96:98, :], 1.0, [q_.b])
            for k_ in kst:
                kb.memset("pool", k_.ap[:, :], 0.0, [k_.b])
                kb.memset("pool", k_.ap[64:66, :], 1.0, [k_.b])
            hsrc, hsrc_b = hT[s][:, :], hT_b[s]
            it = 0
            for g in range(NG):
                tk = slice(g * 512, (g + 1) * 512)
                norm_group(hsrc, hsrc_b, s, 1, g, hg, uT)

                def proj_fm(chunk, pbank):
                    for k in range(8):
                        kb.mm(PS[pbank][:, :], wf.ap[:, k * NHF + chunk * 128:k * NHF + (chunk + 1) * 128],
                              uT.ap[:, k * 512:(k + 1) * 512], k == 0, k == 7, [wf.b, uT.b], [PB[pbank]])

                for h in range(8):
                    pq = (h % 2) * 2
                    pk = (h % 2) * 2 + 1
                    proj_fm(h, pq)
                    proj_fm(8 + h, pk)
                    flush_deferred()
                    q_ = qst[h % 2]
                    k_ = kst[h % 2]
                    sq_ = sqs[h % 2]
                    kb.cp("act", q_.ap[0:64, :], PS[pq][0:64, :], [PB[pq]], [q_.b])
                    kb.act(sq_.ap[0:64, :], PS[pq][0:64, :], AF.Square, [PB[pq]], [sq_.b])
                    bound_update(sq_.ap, sq_.b, slice(0, 64), h)
                    kb.cp("act", k_.ap[0:64, :], PS[pk][0:64, :], [PB[pk]], [k_.b])
                    kb.act(sq_.ap[64:128, :], PS[pk][0:64, :], AF.Square, [PB[pk]], [sq_.b])
                    bound_update(sq_.ap, sq_.b, slice(64, 128), 16 + h)
                    R = slice(64, 66)
                    kb.act(ge.ap[R, :], PS[pq][R, :], AF.Exp, [PB[pq], fb_b], [ge.b], bias=fb_t[R, h:h + 1], scale=-1.0)
                    kb.act(ge.ap[R, :], ge.ap[R, :], AF.Ln, [ge.b], [ge.b], bias=1.0, scale=1.0)
                    kb.ts("dve", ge.ap[R, :], ge.ap[R, :], -8.0, None, ALU.mult, None, [ge.b], [ge.b])
                    kb.S.op("dve", (lambda R=R, h=h: lambda e: e.tensor_tensor_scan(gc.ap[R, :], ge.ap[R, :], zeros_f[R, :], carry[R, h:h + 1], ALU.add, ALU.add))(),
                            [ge.b, misc_b], [gc.b])
                    kb.cp("dve", carry[R, h:h + 1], gc.ap[R, 511:512], [gc.b], [misc_b])
                    kb.cp("dve", ghb.ap[R, :], gc.ap[R, :], [gc.b], [ghb.b])
                    kb.cp("dve", gh.ap[R, :], ghb.ap[R, :], [ghb.b], [gh.b])
                    kb.stt("dve", gh.ap[R, :], gh.ap[R, :], cst[R, 527:528], gc.ap[R, :], ALU.mult, ALU.add, [gh.b, gc.b, cst_b], [gh.b])
                    kb.cp("dve", q_.ap[R, :], gh.ap[R, :], [gh.b], [q_.b])
                    kb.S.op("act", (lambda q_=q_, k_=k_: lambda e: e.mul(k_.ap[96:98, :], q_.ap[64:66, :], -1.0))(), [q_.b], [k_.b])
                    kb.dma(scr["QA%d" % h][0][:, tk], q_.ap, [q_.b], [scr["QA%d" % h][1]])
                    kb.dma(scr["KA%d" % h][0][:, tk], k_.ap, [k_.b], [scr["KA%d" % h][1]])
                fm_list = [("DQ%d" % c, 16 + c, 32 + 2 * c) for c in range(4)] + [("DK%d" % c, 20 + c, 48 + 2 * c) for c in range(4)] + \
                          [("IQ%d" % c, 24 + c, None) for c in range(2)] + [("IK", 26, None)]
                for n_i, (nm, chunk, scol) in enumerate(fm_list):
                    pbk = 4 + (n_i % 2)
                    proj_fm(chunk, pbk)
                    flush_deferred()
                    d_ = gst[n_i % 3]
                    kb.cp("act", d_.ap[:, :], PS[pbk][:, :], [PB[pbk]], [d_.b])
                    dstb_cur[0] = d_.b
                    for b0 in (0, 64):
                        rope_block(PS[pbk], PB[pbk], b0, 8, g, cos16, sin16, d_.ap, t1, t2)
                    if scol is not None:
                        sq_ = sqs[n_i % 2]
                        kb.act(sq_.ap[:, :], d_.ap[:, :], AF.Square, [d_.b], [sq_.b])
                        hb = (chunk - 16) * 2 if chunk < 20 else (chunk - 20) * 2
                        base = 8 if chunk < 20 else 24
                        bound_update(sq_.ap, sq_.b, slice(0, 64), base + hb)
                        bound_update(sq_.ap, sq_.b, slice(64, 128), base + hb + 1)
                    kb.dma(scr[nm][0][:, tk], d_.ap, [d_.b], [scr[nm][1]])
                for tt_ in range(4):
                    if tt_ == 1:
                        flush_deferred()
                    tok = slice(tt_ * 128, (tt_ + 1) * 128)
                    gt = g * 4 + tt_
                    for vi, nm in enumerate(("VF", "DV")):
                        pbk = 4 + vi
                        for k in range(8):
                            kb.mm(PS[pbk][:, :], uT.ap[:, k * 512 + tt_ * 128:k * 512 + (tt_ + 1) * 128],
                                  wt.ap[:, k * NHT + vi * 512:k * NHT + (vi + 1) * 512], k == 0, k == 7, [wt.b, uT.b], [PB[pbk]])
                        v_ = vst[vi]
                        if vi == 0:
                            kb.cp("act", v_.ap.rearrange("p (h e) -> p h e", e=128)[:, :, 0:64],
                                  PS[pbk][:, :].rearrange("p (h d) -> p h d", d=64), [PB[pbk]], [v_.b])
                        else:
                            kb.cp("act", v_.ap[:, :], PS[pbk][:, :], [PB[pbk]], [v_.b])
                        kb.dma(scr[nm][0][gt * 128:(gt + 1) * 128, :], v_.ap, [v_.b], [scr[nm][1]])
                    for k in range(8):
                        kb.mm(PS[0][:, 0:4], uT.ap[:, k * 512 + tt_ * 128:k * 512 + (tt_ + 1) * 128],
                              wt.ap[:, k * NHT + 1024:k * NHT + 1028], k == 0, k == 7, [wt.b, uT.b], [PB[0]])
                    kb.ts("dve", iw_all[:, gt * 4:(gt + 1) * 4], PS[0][:, 0:4], 1.0 / 16.0, None, ALU.mult, None, [PB[0]], [iw_b])
            flush_deferred()
            finish_bounds(16, 0.125)
            kb.act(iwabs[:, :], iw_all[:, :], AF.Abs, [iw_b], [iw_b])
            kb.act(iwsgn[:, :], iw_all[:, :], AF.Sign, [iw_b], [iw_b])

        def attn_head(j_blocks, Kt, Kb_, Qt, Qb_, K2, Q2, Vfn, mask_tiles, mask_b, dsa_masks, scale, negcol, dv, out_rows, PT, ost, orec):
            mixd, mixb = scr["MIX"]
            for jb in j_blocks:
                qs = slice(jb * 512, (jb + 1) * 512)
                nkt = 4 * jb + 4
                po = 3 + (jb % 2)
                pd = 5 + (jb % 2)
                pend = None
                for kt in range(nkt + 1):
                    if kt < nkt:
                        ps_ = kt % 3
                        ks = slice(kt * 128, (kt + 1) * 128)
                        diag = kt >= 4 * jb
                        last_plain = (K2 is None) and not (diag and mask_tiles is not None) and dsa_masks is None
                        kb.mm(PS[ps_][:, :], Kt[:, ks], Qt[:, qs], True, last_plain, [Kb_, Qb_], [PB[ps_]])
                        if K2 is not None:
                            kb.mm(PS[ps_][:, :], K2[:, ks], Q2[:, qs], False, not (diag and mask_tiles is not None), [Kb_, Qb_], [PB[ps_]])
                        if diag and mask_tiles is not None:
                            a = kt - 4 * jb
                            kb.mm(PS[ps_][:, :], ident_bf[:, :], mask_tiles[:, a * 512:(a + 1) * 512], False, True, [misc_b, mask_b], [PB[ps_]])
                        if dsa_masks is not None:
                            for a in range(4):
                                mb_ = dsa_masks[a]
                                kb.mm(PS[ps_][:, a * 128:(a + 1) * 128], mb_.ap[:, ks], ident_bf[:, :], False, a == 3, [mb_.b, misc_b], [PB[ps_]])
                        pt_ = PT[kt % 3]
                        kb.act(pt_.ap, PS[ps_][:, :], AF.Exp, [PB[ps_], negB_b], [pt_.b], bias=negB[:, negcol:negcol + 1], scale=scale)
                    if pend is not None:
                        pk_, ppt = pend
                        kb.mm(PS[po][:, :], Vfn(pk_), ppt.ap, pk_ == 0, pk_ == nkt - 1, [ppt.b, Vb_cur[0]], [PB[po]])
                    pend = (kt, PT[kt % 3]) if kt < nkt else None
                rc = orec[jb % 2]
                os_ = ost[jb % 2]
                kb.recip(rc.ap[0:dv, :], PS[po][64:128, :], [PB[po]], [rc.b])
                kb.tt("dve", os_.ap[0:dv, :], PS[po][0:dv, :], rc.ap[0:dv, :], ALU.mult, [PB[po], rc.b], [os_.b])
                kb.dma(mixd[out_rows, qs], os_.ap[0:dv, :], [os_.b], [mixb])

        Vb_cur = [None]

        def fox_attn_phase(s):
            S.fence()
            A.reset()
            vall = A.bf(NKT * 1024, "vall")
            vd, vb = scr["VF"]
            kb.dma(vall.ap.rearrange("p (t d) -> p t d", d=1024), vd.rearrange("(t p) d -> p t d", p=128), [vb], [vall.b])
            Vb_cur[0] = vall.b
            cm = build_mask(0)
            KA = [A.bf(SQ, "KA%d" % i) for i in range(2)]
            QA = [A.bf(SQ, "QA%d" % i) for i in range(2)]
            PT = [A.bf(512, "PT%d" % i) for i in range(3)]
            ost = [A.bf(512, "ost%d" % i) for i in range(2)]
            orec = [A.f32(512, "orec%d" % i) for i in range(2)]
            for h in range(8):
                k_ = KA[h % 2]
                q_ = QA[h % 2]
                kb.dma(k_.ap, scr["KA%d" % h][0][:, :], [scr["KA%d" % h][1]], [k_.b])
                kb.dma(q_.ap, scr["QA%d" % h][0][:, :], [scr["QA%d" % h][1]], [q_.b])
                vv = vall.ap.rearrange("p (t d) -> p t d", d=1024)
                attn_head(range(NG), k_.ap, k_.b, q_.ap, q_.b, None, None,
                          (lambda h: lambda kt: vv[:, kt, h * 128:(h + 1) * 128])(h),
                          cm.ap, cm.b, None, 0.125, h, 64, slice(h * 64, (h + 1) * 64), PT, ost, orec)

        def dsa_phase(s):
            S.fence()
            A.reset()
            vall = A.bf(NKT * 512, "dvall")
            vd, vb = scr["DV"]
            kb.dma(vall.ap.rearrange("p (t d) -> p t d", d=512), vd.rearrange("(t p) d -> p t d", p=128), [vb], [vall.b])
            Vb_cur[0] = vall.b
            vv = vall.ap.rearrange("p (t d) -> p t d", d=512)
            DKs = []
            for c in range(4):
                t_ = A.bf(SQ, "DK%d" % c)
                kb.dma(t_.ap, scr["DK%d" % c][0][:, :], [scr["DK%d" % c][1]], [t_.b])
                DKs.append(t_)
            IK = A.bf(SQ, "IK")
            kb.dma(IK.ap, scr["IK"][0][:, :], [scr["IK"][1]], [IK.b])
            DQb = [[A.bf(512, "DQ%d_%d" % (c, i)) for c in range(4)] for i in range(2)]
            IQb = [[A.bf(512, "IQ%d_%d" % (c, i)) for c in range(2)] for i in range(2)]
            Sc = A.f32(SQ, "Sc")
            ccmq = build_mask(2)
            junk = A.bf(SQ, "junk")
            maskb = [[A.bf(SQ, "maskb%d_%d" % (i, a)) for a in range(4)] for i in range(2)]
            rr_ = [A.f32(512, "rr%d" % i) for i in range(2)]
            PT = [A.bf(512, "PT%d" % i) for i in range(3)]
            ost = [A.bf(512, "ost%d" % i) for i in range(2)]
            orec = [A.f32(512, "orec%d" % i) for i in range(2)]
            osb = [A.f32(512, "osb%d" % i) for i in range(2)]
            bs = smallf[:, 44:60]
            IBANK = (4, 6, 7)
            ictr = [0]

            def load_block(jb):
                qs = slice(jb * 512, (jb + 1) * 512)
                for c in range(4):
                    kb.dma(DQb[jb % 2][c].ap, scr["DQ%d" % c][0][:, qs], [scr["DQ%d" % c][1]], [DQb[jb % 2][c].b])
                for c in range(2):
                    kb.dma(IQb[jb % 2][c].ap, scr["IQ%d" % c][0][:, qs], [scr["IQ%d" % c][1]], [IQb[jb % 2][c].b])

            def idx_tile(jb, a):
                iq = IQb[jb % 2]
                nk = 512 * (jb + 1)
                qt = jb * 4 + a
                mb_out = maskb[jb % 2][a]
                for kbk in range(jb + 1):
                    kk = slice(kbk * 512, (kbk + 1) * 512)
                    for ih in range(4):
                        rows = slice((ih % 2) * 64, (ih % 2) * 64 + 64)
                        bank = IBANK[ictr[0] % 3]
                        ictr[0] += 1
                        kb.mm(PS[bank][:, :], iq[ih // 2].ap[rows, a * 128:(a + 1) * 128], IK.ap[rows, kk], True, True,
                              [iq[ih // 2].b, IK.b], [PB[bank]])
                        r_ = rr_[ih % 2]
                        kb.act(r_.ap, PS[bank][:, :], AF.Relu, [PB[bank], iw_b], [r_.b], scale=iwabs[:, qt * 4 + ih:qt * 4 + ih + 1])
                        if ih == 0:
                            kb.ts("dve", Sc.ap[:, kk], r_.ap, iwsgn[:, qt * 4:qt * 4 + 1], None, ALU.mult, None, [r_.b, iw_b], [Sc.b])
                        else:
                            kb.stt("dve", Sc.ap[:, kk], r_.ap, iwsgn[:, qt * 4 + ih:qt * 4 + ih + 1], Sc.ap[:, kk], ALU.mult, ALU.add,
                                   [r_.b, iw_b, Sc.b], [Sc.b])
                lo = bs[:, 0:1]
                hi = bs[:, 1:2]
                mid = bs[:, 2:3]
                cnt = bs[:, 3:4]
                cond = bs[:, 4:5]
                dd = bs[:, 5:6]
                bsb = Buf("bs")
                if qt >= 2:
                    kb.S.op("dve", (lambda nk=nk: lambda e: e.tensor_reduce(hi, Sc.ap[:, 0:nk], AX.X, ALU.max))(), [Sc.b], [bsb])
                    kb.S.op("dve", (lambda nk=nk: lambda e: e.tensor_reduce(lo, Sc.ap[:, 0:nk], AX.X, ALU.min))(), [Sc.b], [bsb])
                    kb.tt("dve", dd, hi, lo, ALU.subtract, [bsb], [bsb])
                    kb.ts("dve", dd, dd, 1.001, 1e-6, ALU.mult, ALU.add, [bsb], [bsb])
                dk_ = slice(jb * 512, (jb + 1) * 512)
                kb.tt("dve", Sc.ap[:, dk_], Sc.ap[:, dk_], ccmq.ap[:, a * 512:(a + 1) * 512], ALU.add, [Sc.b, ccmq.b], [Sc.b])
                if qt >= 2:
                    for itn in range(13):
                        kb.ts("dve", dd, dd, 0.5, None, ALU.mult, None, [bsb], [bsb])
                        kb.tt("dve", mid, lo, dd, ALU.add, [bsb], [bsb])
                        kb.ts("dve", junk.ap[:, 0:nk], Sc.ap[:, 0:nk], mid, 0.0, ALU.is_ge, ALU.add, [Sc.b, bsb], [junk.b, bsb], accum_out=cnt)
                        kb.stt("dve", cond, cnt, 255.5, dd, ALU.is_ge, ALU.mult, [bsb], [bsb])
                        kb.tt("dve", lo, lo, cond, ALU.add, [bsb], [bsb])
                else:
                    kb.memset("dve", lo, -1e29, [bsb])
                kb.ts("dve", mb_out.ap[:, 0:nk], Sc.ap[:, 0:nk], lo, NEGM, ALU.is_lt, ALU.mult, [Sc.b, bsb], [mb_out.b])

            def attn_head_dsa(jb, h):
                c = h // 2
                rows = slice((h % 2) * 64, (h % 2) * 64 + 64)
                DKc = DKs[c]
                dqc = DQb[jb % 2][c]
                mbs = maskb[jb % 2]
                mixd, mixb = scr["MIX"]
                nkt = 4 * jb + 4
                po = 3
                pd = 5
                pend = None
                qs = slice(jb * 512, (jb + 1) * 512)
                for kt in range(nkt + 1):
                    if kt < nkt:
                        ps_ = kt % 3
                        ks = slice(kt * 128, (kt + 1) * 128)
                        kb.mm(PS[ps_][:, :], DKc.ap[rows, ks], dqc.ap[rows, :], True, False, [DKc.b, dqc.b], [PB[ps_]])
                        for a in range(4):
                            mb_ = mbs[a]
                            kb.mm(PS[ps_][:, a * 128:(a + 1) * 128], mb_.ap[:, ks], ident_bf[:, :], False, a == 3, [mb_.b, misc_b], [PB[ps_]])
                        pt_ = PT[kt % 3]
                        kb.act(pt_.ap, PS[ps_][:, :], AF.Exp, [PB[ps_], negB_b], [pt_.b], bias=negB[:, 8 + h:9 + h], scale=0.125)
                    if pend is not None:
                        pk_, ppt = pend
                        kb.mm(PS[po][0:64, :], vv[:, pk_, h * 64:(h + 1) * 64], ppt.ap, pk_ == 0, pk_ == nkt - 1, [ppt.b, Vb_cur[0]], [PB[po]])
                        kb.mm(PS[pd][0:64, :], ones_bf[:, 0:64], ppt.ap, pk_ == 0, pk_ == nkt - 1, [ppt.b, misc_b], [PB[pd]])
                    pend = (kt, PT[kt % 3]) if kt < nkt else None
                rc = orec[h % 2]
                os_ = ost[h % 2]
                ob_ = osb[h % 2]
                kb.act(rc.ap[0:64, :], PS[pd][0:64, :], AF.Ln, [PB[pd]], [rc.b])
                kb.act(rc.ap[0:64, :], rc.ap[0:64, :], AF.Exp, [rc.b], [rc.b], scale=-1.0)
                kb.cp("act", ob_.ap[0:64, :], PS[po][0:64, :], [PB[po]], [ob_.b])
                kb.tt("pool", os_.ap[0:64, :], ob_.ap[0:64, :], rc.ap[0:64, :], ALU.mult, [ob_.b, rc.b], [os_.b])
                kb.dma(mixd[512 + h * 64:512 + (h + 1) * 64, qs], os_.ap[0:64, :], [os_.b], [mixb])

            load_block(0)
            for a in range(4):
                idx_tile(0, a)
            for jb in range(NG):
                if jb + 1 < NG:
                    load_block(jb + 1)
                for a in range(4):
                    if jb + 1 < NG:
                        idx_tile(jb + 1, a)
                    attn_head_dsa(jb, 2 * a)
                    attn_head_dsa(jb, 2 * a + 1)

        def mla_proj_phase(layer, s):
            global A_sq, A_rs, A_tmp
            j = layer // 2
            S.fence()
            A.reset()
            wdn = A.bf(8 * 768, "wdn")
            wuq = A.bf(3 * 1536, "wuq")
            wukv = A.bf(2 * 2048, "wukv")
            mark = A.off
            wdn.b = load_w_bf(wdn.ap, mla_down[j], 8 * 768, "wdn")
            A.off = mark
            wuq.b = load_w_bf(wuq.ap, mla_uq[j], 3 * 1536, "wuq")
            A.off = mark
            wukv.b = load_w_bf(wukv.ap, mla_ukv[j], 2 * 2048, "wukv")
            A.off = mark
            kb.dma(qn_t[:, 0:3], mla_qn[j, :, :], [], [qn_b])
            kb.dma(qn_t[:, 3:5], mla_kvn[j, :, :], [], [qn_b])
            S.fence()
            cos64 = A.f32(SQ, "cos64")
            sin64 = A.f32(SQ, "sin64")
            mk2 = A.off
            rope_tables(s, 526, cos64, sin64)
            A.off = mk2
            S.fence()
            hg = A.f32(8 * 512, "hg")
            uT = A.bf(8 * 512, "uT")
            A_sq = A.bf(8 * 512, "sq")
            A_rs = A.f32(512, "rs")
            A_tmp = [A.f32(512, "tmp%d" % i) for i in range(2)]
            cl = A.f32(6 * 512, "cl")
            cn = A.bf(5 * 512, "cn")
            sq2 = A.bf(5 * 512, "sq2")
            rs2 = A.f32(1024, "rs2")
            gst = [A.bf(512, "gst%d" % i) for i in range(3)]
            sqs = [A.bf(512, "sqs%d" % i) for i in range(2)]
            t1 = A.f32(512, "t1")
            t2 = A.f32(512, "t2")
            vst = [A.bf(512, "vst%d" % i) for i in range(2)]
            kb.memset("pool", stat[:, 0:32], 0.0, [stat_b])
            epsq = smallf[:, 2:3]
            epsk = smallf[:, 3:4]
            kb.memset("pool", epsq, 384.0 * 1e-6, [misc_b])
            kb.memset("pool", epsk, 256.0 * 1e-6, [misc_b])
            kb.ts("dve", qn_t[:, 0:3], qn_t[:, 0:3], float(np.sqrt(384.0)), None, ALU.mult, None, [qn_b], [qn_b])
            kb.ts("dve", qn_t[:, 3:5], qn_t[:, 3:5], 16.0, None, ALU.mult, None, [qn_b], [qn_b])
            hsrc, hsrc_b = hT[s][:, :], hT_b[s]
            for g in range(NG):
                tk = slice(g * 512, (g + 1) * 512)
                norm_group(hsrc, hsrc_b, s, 1, g, hg, uT)
                for c in range(6):
                    pbk = c % 2
                    for k in range(8):
                        kb.mm(PS[pbk][:, :], wdn.ap[:, k * 768 + c * 128:k * 768 + (c + 1) * 128], uT.ap[:, k * 512:(k + 1) * 512],
                              k == 0, k == 7, [wdn.b, uT.b], [PB[pbk]])
                    if c < 5:
                        kb.cp("act", cl.ap[:, c * 512:(c + 1) * 512], PS[pbk][:, :], [PB[pbk]], [cl.b])
                        kb.act(sq2.ap[:, c * 512:(c + 1) * 512], PS[pbk][:, :], AF.Square, [PB[pbk]], [sq2.b])
                    else:
                        d_ = gst[0]
                        dstb_cur[0] = d_.b
                        rope_block(PS[pbk], PB[pbk], 0, 32, g, cos64, sin64, d_.ap, t1, t2)
                        kb.act(sqs[0].ap[0:64, :], d_.ap[0:64, :], AF.Square, [d_.b], [sqs[0].b])
                        kb.mm(PS[2][:, :], ones_bf[0:64, :], sqs[0].ap[0:64, :], True, True, [sqs[0].b, misc_b], [PB[2]])
                        kb.cp("dve", rs2.ap[:, 512:1024], PS[2][:, :], [PB[2]], [rs2.b])
                        kb.dma(scr["KR"][0][:, tk], d_.ap[0:64, :], [d_.b], [scr["KR"][1]])
                for (c0, c1, epsc, col) in ((0, 3, epsq, 0), (3, 5, epsk, 1)):
                    for c in range(c0, c1):
                        kb.mm(PS[3][:, :], ones_bf[:, :], sq2.ap[:, c * 512:(c + 1) * 512], c == c0, c == c1 - 1, [sq2.b, misc_b], [PB[3]])
                    kb.act(A_rs.ap, PS[3][:, :], AF.Sqrt, [PB[3]], [A_rs.b], bias=epsc, scale=1.0)
                    kb.recip(A_rs.ap, A_rs.ap, [A_rs.b], [A_rs.b])
                    for c in range(c0, c1):
                        kb.stt("dve", cn.ap[:, c * 512:(c + 1) * 512], cl.ap[:, c * 512:(c + 1) * 512], qn_t[:, c:c + 1], A_rs.ap,
                               ALU.mult, ALU.mult, [cl.b, A_rs.b, qn_b], [cn.b])
                for h in range(8):
                    pn = (h % 2) * 2
                    pr = (h % 2) * 2 + 1
                    for k in range(3):
                        kb.mm(PS[pn][:, :], wuq.ap[:, k * 1536 + h * 192:k * 1536 + h * 192 + 128], cn.ap[:, k * 512:(k + 1) * 512],
                              k == 0, k == 2, [wuq.b, cn.b], [PB[pn]])
                    for k in range(3):
                        kb.mm(PS[pr][0:64, :], wuq.ap[:, k * 1536 + h * 192 + 128:k * 1536 + h * 192 + 192], cn.ap[:, k * 512:(k + 1) * 512],
                              k == 0, k == 2, [wuq.b, cn.b], [PB[pr]])
                    dn = gst[1]
                    dr = gst[2]
                    kb.cp("act", dn.ap[:, :], PS[pn][:, :], [PB[pn]], [dn.b])
                    dstb_cur[0] = dr.b
                    rope_block(PS[pr], PB[pr], 0, 32, g, cos64, sin64, dr.ap, t1, t2)
                    kb.act(sqs[0].ap[:, :], dn.ap[:, :], AF.Square, [dn.b], [sqs[0].b])
                    kb.act(sqs[1].ap[0:64, :], dr.ap[0:64, :], AF.Square, [dr.b], [sqs[1].b])
                    kb.mm(PS[6][:, :], ones_bf[:, :], sqs[0].ap[:, :], True, False, [sqs[0].b, misc_b], [PB[6]])
                    kb.mm(PS[6][:, :], ones_bf[0:64, :], sqs[1].ap[0:64, :], False, True, [sqs[1].b, misc_b], [PB[6]])
                    kb.S.op("dve", lambda e: e.tensor_reduce(smallf[:, 32:33], PS[6][:, :], AX.X, ALU.max), [PB[6]], [misc_b])
                    kb.tt("dve", stat[:, h:h + 1], stat[:, h:h + 1], smallf[:, 32:33], ALU.max, [misc_b, stat_b], [stat_b])
                    kb.dma(scr["QN%d" % h][0][:, tk], dn.ap, [dn.b], [scr["QN%d" % h][1]])
                    kb.dma(scr["QR%d" % h][0][:, tk], dr.ap[0:64, :], [dr.b], [scr["QR%d" % h][1]])
                for h in range(8):
                    pn = 4 + (h % 2)
                    for k in range(2):
                        kb.mm(PS[pn][:, :], wukv.ap[:, k * 2048 + h * 128:k * 2048 + (h + 1) * 128], cn.ap[:, (3 + k) * 512:(4 + k) * 512],
                              k == 0, k == 1, [wukv.b, cn.b], [PB[pn]])
                    dn = gst[h % 2]
                    kb.cp("act", dn.ap[:, :], PS[pn][:, :], [PB[pn]], [dn.b])
                    kb.act(sqs[h % 2].ap[:, :], PS[pn][:, :], AF.Square, [PB[pn]], [sqs[h % 2].b])
                    kb.mm(PS[6][:, :], ones_bf[:, :], sqs[h % 2].ap[:, :], True, True, [sqs[h % 2].b, misc_b], [PB[6]])
                    kb.tt("dve", t1.ap, PS[6][:, :], rs2.ap[:, 512:1024], ALU.add, [PB[6], rs2.b], [t1.b])
                    kb.S.op("dve", lambda e: e.tensor_reduce(smallf[:, 32:33], t1.ap, AX.X, ALU.max), [t1.b], [misc_b])
                    kb.tt("dve", stat[:, 16 + h:17 + h], stat[:, 16 + h:17 + h], smallf[:, 32:33], ALU.max, [misc_b, stat_b], [stat_b])
                    kb.dma(scr["KN%d" % h][0][:, tk], dn.ap, [dn.b], [scr["KN%d" % h][1]])
                for tt_ in range(4):
                    gt = g * 4 + tt_
                    for half in range(2):
                        pbk = 2 + half
                        for k in range(2):
                            kb.mm(PS[pbk][:, :], cn.ap[:, (3 + k) * 512 + tt_ * 128:(3 + k) * 512 + (tt_ + 1) * 128],
                                  wukv.ap[:, k * 2048 + 1024 + half * 512:k * 2048 + 1024 + (half + 1) * 512], k == 0, k == 1,
                                  [wukv.b, cn.b], [PB[pbk]])
                        v_ = vst[half]
                        kb.cp("act", v_.ap[:, :], PS[pbk][:, :], [PB[pbk]], [v_.b])
                        kb.dma(scr["MV"][0][gt * 128:(gt + 1) * 128, half * 512:(half + 1) * 512], v_.ap, [v_.b], [scr["MV"][1]])
            finish_bounds(8, float(192.0 ** -0.5))

        def mla_attn_phase(s):
            S.fence()
            A.reset()
            vall = A.bf(NKT * 1024, "mvall")
            vd, vb = scr["MV"]
            kb.dma(vall.ap.rearrange("p (t d) -> p t d", d=1024), vd.rearrange("(t p) d -> p t d", p=128), [vb], [vall.b])
            Vb_cur[0] = vall.b
            vv = vall.ap.rearrange("p (t d) -> p t d", d=1024)
            KR = A.bf(SQ, "KR")
            ccm = build_mask(1)
            kb.dma(KR.ap[0:64, :], scr["KR"][0][:, :], [scr["KR"][1]], [KR.b])
            KN = [A.bf(SQ, "KN%d" % i) for i in range(2)]
            QN = [A.bf(SQ, "QN%d" % i) for i in range(2)]
            QR = [A.bf(SQ, "QR%d" % i) for i in range(2)]
            PT = [A.bf(512, "PT%d" % i) for i in range(3)]
            ost = [A.bf(512, "ost%d" % i) for i in range(2)]
            orec = [A.f32(512, "orec%d" % i) for i in range(2)]
            sc = float(192.0 ** -0.5)
            for h in range(8):
                kn = KN[h % 2]
                qn = QN[h % 2]
                qr = QR[h % 2]
                kb.dma(kn.ap, scr["KN%d" % h][0][:, :], [scr["KN%d" % h][1]], [kn.b])
                kb.dma(qn.ap, scr["QN%d" % h][0][:, :], [scr["QN%d" % h][1]], [qn.b])
                kb.dma(qr.ap[0:64, :], scr["QR%d" % h][0][:, :], [scr["QR%d" % h][1]], [qr.b])
                kq_b = Buf("kq")
                attn_head_mla(h, kn, qn, KR, qr, vv, PT, ost, orec, sc, ccm)

        def attn_head_mla(h, kn, qn, KR, qr, vv, PT, ost, orec, sc, ccm):
            mixd, mixb = scr["MIX"]
            for jb in range(NG):
                qs = slice(jb * 512, (jb + 1) * 512)
                nkt = 4 * jb + 4
                po = 3 + (jb % 2)
                pd = 5 + (jb % 2)
                pend = None
                for kt in range(nkt + 1):
                    if kt < nkt:
                        ps_ = kt % 3
                        ks = slice(kt * 128, (kt + 1) * 128)
                        diag = kt >= 4 * jb
                        kb.mm(PS[ps_][:, :], kn.ap[:, ks], qn.ap[:, qs], True, False, [kn.b, qn.b], [PB[ps_]])
                        kb.mm(PS[ps_][:, :], KR.ap[0:64, ks], qr.ap[0:64, qs], False, not diag, [KR.b, qr.b], [PB[ps_]])
                        if diag:
                            a = kt - 4 * jb
                            kb.mm(PS[ps_][:, :], ident_bf[:, :], ccm.ap[:, a * 512:(a + 1) * 512], False, True, [misc_b, ccm.b], [PB[ps_]])
                        pt_ = PT[kt % 3]
                        kb.act(pt_.ap, PS[ps_][:, :], AF.Exp, [PB[ps_], negB_b], [pt_.b], bias=negB[:, h:h + 1], scale=sc)
                    if pend is not None:
                        pk_, ppt = pend
                        kb.mm(PS[po][:, :], vv[:, pk_, h * 128:(h + 1) * 128], ppt.ap, pk_ == 0, pk_ == nkt - 1, [ppt.b, Vb_cur[0]], [PB[po]])
                        kb.mm(PS[pd][:, :], ones_bf[:, :], ppt.ap, pk_ == 0, pk_ == nkt - 1, [ppt.b, misc_b], [PB[pd]])
                    pend = (kt, PT[kt % 3]) if kt < nkt else None
                rc = orec[jb % 2]
                os_ = ost[jb % 2]
                kb.recip(rc.ap, PS[pd][:, :], [PB[pd]], [rc.b])
                kb.tt("dve", os_.ap, PS[po][:, :], rc.ap, ALU.mult, [PB[po], rc.b], [os_.b])
                kb.dma(mixd[h * 128:(h + 1) * 128, qs], os_.ap, [os_.b], [mixb])

        def final_phase():
            global A_sq, A_rs, A_tmp
            S.fence()
            A.reset()
            hg = A.f32(8 * 512, "hg")
            uo = [A.f32(8 * 512, "uo%d" % i) for i in range(2)]
            A_sq = A.bf(8 * 512, "sq")
            A_rs = A.f32(512, "rs")
            A_tmp = [A.f32(512, "tmp%d" % i) for i in range(2)]
            it = 0
            for s in range(NSEQ):
                for g in range(NG):
                    u_ = uo[it % 2]
                    it += 1
                    norm_group(hT[s][:, :], hT_b[s], s, 0, g, hg, u_, final=True)
                    dst = outT[s].rearrange("(c p) t -> p c t", p=128)[:, :, g * 512:(g + 1) * 512]
                    S.out_dma.append(kb.S.op("sp", (lambda dst=dst, u_=u_: lambda e: e.dma_start(out=dst, in_=u_.ap.rearrange("p (c t) -> p c t", c=8)))(),
                                             [u_.b], [], dma=True))

        steps = []
        for layer in range(DEPTH):
            steps.append(("mod%d" % layer, (lambda layer=layer: (compute_mod(layer), compute_abg(layer)))))
            steps.append(("ffn%d_0" % layer, (lambda layer=layer: ffn_phase(layer, 0))))
            for s in range(NSEQ):
                if layer % 2 == 0:
                    steps.append(("hproj%d_%d" % (layer, s), (lambda layer=layer, s=s: hyb_proj_phase(layer, s))))
                    steps.append(("fox%d_%d" % (layer, s), (lambda layer=layer, s=s: fox_attn_phase(s))))
                    steps.append(("dsa%d_%d" % (layer, s), (lambda layer=layer, s=s: dsa_phase(s))))
                    steps.append(("oproj%d_%d" % (layer, s), (lambda layer=layer, s=s: outproj_phase(layer, hyb_out[layer // 2], s))))
                else:
                    steps.append(("mproj%d_%d" % (layer, s), (lambda layer=layer, s=s: mla_proj_phase(layer, s))))
                    steps.append(("mattn%d_%d" % (layer, s), (lambda layer=layer, s=s: mla_attn_phase(s))))
                    steps.append(("oproj%d_%d" % (layer, s), (lambda layer=layer, s=s: outproj_phase(layer, mla_out[layer // 2], s))))
            steps.append(("ffn%d_1" % layer, (lambda layer=layer: ffn_phase(layer, 1))))
        steps.append(("final", final_phase))
        for name, fn in steps:
            fn()
            if cfg.get("stop") == name:
                break
        if cfg.get("dbg_sb"):
            S.fence()
            named = dict(modT=modT, ABG=ABG, condT=condT, stat=stat, negB=negB, iw_all=iw_all, ngT=ngT, smallf=smallf)
            for nm in cfg["dbg_sb"]:
                t_ = named[nm]
                dd_ = nc.dram_tensor("dbgsb_" + nm, list(t_.shape), F32, kind="ExternalOutput")
                kb.S.op("sp", (lambda dd_=dd_, t_=t_: lambda e: e.dma_start(out=dd_[:, :], in_=t_[:]))(), [], [], dma=True)
        S.emit()
    return nc


PERM64 = list(range(0, 8)) + list(range(16, 40)) + list(range(8, 16)) + list(range(40, 64))


def make_consts():
    c = np.zeros((128, 640), np.float32)
    p = np.arange(128)
    c[:, 0:512] = np.arange(512)[None, :]
    c[:, 512] = p
    for a in range(4):
        c[:, 513 + a] = 128 * a + p
        c[:, 517 + a] = 64 * ((128 * a + p) // 64)
        c[:, 521 + a] = 64 * ((128 * a + p) // 64 + 1)
    r = p % 64
    i16 = np.where(r < 8, r, np.where((r >= 32) & (r < 40), r - 32, 0))
    c[:, 525] = -np.log(500000.0) * 2.0 * i16 / 16.0
    i64 = np.where(r < 32, r, r - 32)
    c[:, 526] = -np.log(10000.0) * 2.0 * i64 / 64.0
    c[:, 527] = np.where((p % 32) == 1, -1.0, 0.0)
    return c


def lay_k(w, ncols_pad=None):
    K, N = w.shape
    kc = K // 128
    return np.ascontiguousarray(w.reshape(kc, 128, N).transpose(1, 0, 2).reshape(128, kc * N))


def gather_cols(w, idx):
    idx = np.asarray(idx)
    out = np.zeros((w.shape[0], len(idx)), w.dtype)
    m = idx >= 0
    out[:, m] = w[:, idx[m]]
    return out


def prep_shared(inp, DEPTH):
    f = {}
    n_even = (DEPTH + 1) // 2
    n_odd = DEPTH // 2
    f["consts"] = make_consts()
    f["ada_w"] = np.stack([lay_k(inp["ada_w"][i]) for i in range(DEPTH)])
    f["ada_b"] = np.ascontiguousarray(inp["ada_b"][:DEPTH].reshape(DEPTH, 1, 9216))
    ng = inp["norm_g"][:DEPTH].reshape(DEPTH * 3, 8, 128).transpose(0, 2, 1)
    f["norm_gT"] = np.ascontiguousarray(ng)
    f["final_gT"] = np.ascontiguousarray(inp["final_g"].reshape(8, 128).T)
    f["w_gate"] = np.stack([lay_k(inp["ffn_w_gate"][i, j]) for i in range(DEPTH) for j in range(2)])
    f["w_up"] = np.stack([lay_k(inp["ffn_w_up"][i, j]) for i in range(DEPTH) for j in range(2)])
    f["w_down"] = np.stack([lay_k(inp["ffn_w_down"][i, j]) for i in range(DEPTH) for j in range(2)])
    off = np.cumsum([0, 512, 512, 512, 8, 512, 512, 512, 256, 4, 64])
    o_fq, o_fk, o_fv, o_ff, o_dq, o_dk, o_dv, o_iq, o_iw, o_ik = off[:10]
    fm = []
    for h in range(8):
        blk = [-1] * 128
        blk[0:64] = list(range(o_fq + h * 64, o_fq + (h + 1) * 64))
        blk[64] = o_ff + h
        blk[65] = o_ff + h
        fm += blk
    for h in range(8):
        blk = [-1] * 128
        blk[0:64] = list(range(o_fk + h * 64, o_fk + (h + 1) * 64))
        fm += blk
    for base in (o_dq, o_dk):
        for h in range(8):
            fm += [base + h * 64 + d for d in PERM64]
    for h in range(4):
        fm += [o_iq + h * 64 + d for d in PERM64]
    fm += [o_ik + d for d in PERM64] * 2
    assert len(fm) == 3456
    tm = list(range(o_fv, o_fv + 512)) + list(range(o_dv, o_dv + 512)) + list(range(o_iw, o_iw + 4)) + [-1] * 4
    f["hyb_fm"] = np.stack([lay_k(gather_cols(inp["hyb_w_in"][j], fm)) for j in range(n_even)])
    f["hyb_tm"] = np.stack([lay_k(gather_cols(inp["hyb_w_in"][j], tm)) for j in range(n_even)])
    f["hyb_out"] = np.stack([lay_k(inp["hyb_w_out"][j]) for j in range(n_even)])
    f["fox_b"] = np.ascontiguousarray(np.broadcast_to(inp["fox_b_f"][:n_even, None, :], (n_even, 128, 8))).astype(np.float32)
    if n_odd:
        dn_idx = list(range(704)) + [-1] * 64
        f["mla_down"] = np.stack([lay_k(gather_cols(inp["mla_w_down"][j], dn_idx)) for j in range(n_odd)])
        f["mla_qn"] = np.ascontiguousarray(inp["mla_q_norm"][:n_odd].reshape(n_odd, 3, 128).transpose(0, 2, 1))
        f["mla_kvn"] = np.ascontiguousarray(inp["mla_kv_norm"][:n_odd].reshape(n_odd, 2, 128).transpose(0, 2, 1))
        f["mla_uq"] = np.stack([lay_k(inp["mla_w_uq"][j]) for j in range(n_odd)])
        kv_idx = [h * 256 + d for h in range(8) for d in range(128)] + [h * 256 + 128 + d for h in range(8) for d in range(128)]
        f["mla_ukv"] = np.stack([lay_k(gather_cols(inp["mla_w_ukv"][j], kv_idx)) for j in range(n_odd)])
        f["mla_out"] = np.stack([lay_k(inp["mla_w_out"][j]) for j in range(n_odd)])
    return f


def run_cfg(inp, cfg, n_cores):
    NSEQ = cfg["NSEQ"]
    DEPTH = cfg["DEPTH"]
    shared = prep_shared(inp, DEPTH)
    nc = build_program(cfg)
    in_maps = []
    for c in range(n_cores):
        sl = slice(c * NSEQ, (c + 1) * NSEQ)
        m = dict(shared)
        m["xT"] = np.ascontiguousarray(inp["x"][sl].transpose(0, 2, 1))
        cc = inp["c"][sl]
        m["cT"] = np.ascontiguousarray(cc.reshape(NSEQ, 8, 128).transpose(2, 1, 0).reshape(128, 8 * NSEQ))
        m["pos"] = np.ascontiguousarray(inp["positions"][sl]).astype(np.int32)
        in_maps.append(m)
    res = run_bass_kernel_spmd(nc, in_maps, core_ids=list(range(n_cores)))
    return res


def kernel(**inputs):
    inp = {k: np.asarray(v) for k, v in inputs.items()}
    cfg = dict(S=4096, NSEQ=2, DEPTH=4)
    res = run_cfg(inp, cfg, 8)
    outs = [np.asarray(r["outT"]).transpose(0, 2, 1) for r in res.results]
    return np.ascontiguousarray(np.concatenate(outs, axis=0)).astype(np.float32)
```
